# Optimizing a Trainium2 kernel written in Bass

```python
import math
import jax, jax.numpy as jnp
from jax import lax
import numpy as np

D_MODEL = 1024
BATCH = 8
SEQ = 4096
DEPTH = 1

GRID_W = 64
HEAD_DIM = 64
N_Q_HEADS = 8
N_KV_HEADS = 2
GQA_GROUP = N_Q_HEADS // N_KV_HEADS
ATTN_W = N_Q_HEADS * HEAD_DIM
KV_W = N_KV_HEADS * HEAD_DIM
Q_BLOCK = 128
ROPE_THETA = 10000.0
SSD_HEAD_DIM = 64
SSD_HEADS = 8
SSD_W = SSD_HEADS * SSD_HEAD_DIM
SSD_GROUPS = 2
HEADS_PER_GROUP = SSD_HEADS // SSD_GROUPS
D_STATE = 64
D_CONV = 7
CONV_CH = SSD_W + 2 * SSD_GROUPS * D_STATE
CHUNK = 128
SPLIT_SIZES = (ATTN_W, KV_W, KV_W, SSD_W, CONV_CH, 2 * SSD_HEADS, 2 * D_MODEL)
IN_PROJ = sum(SPLIT_SIZES)
N_EXPERT_GROUPS = 4
EXPERTS_PER_GROUP = 4
N_EXPERTS = N_EXPERT_GROUPS * EXPERTS_PER_GROUP
TOP_K_IN_GROUP = 2
D_EXPERT = 512
EPS = 1e-6

kernel_name = "hybrid_gqa_ssd_hiermoe_block"


def rmsnorm(x, w):
    xf = x.astype(jnp.float32)
    xf = xf * lax.rsqrt(jnp.mean(xf * xf, axis=-1, keepdims=True) + EPS)
    return xf.astype(x.dtype) * w


def rotate_half(u):
    u1, u2 = jnp.split(u, 2, axis=-1)
    return jnp.concatenate([-u2, u1], axis=-1)


def axial_rope_tables(seq_len, dtype):
    rows = seq_len // GRID_W
    row = jnp.repeat(jnp.arange(rows, dtype=jnp.int32), GRID_W)
    col = jnp.tile(jnp.arange(GRID_W, dtype=jnp.int32), rows)
    half = HEAD_DIM // 2
    inv_freq = ROPE_THETA ** (-jnp.arange(0, half, 2, dtype=jnp.float32) / half)
    ang_r = row.astype(jnp.float32)[:, None] * inv_freq[None, :]
    ang_c = col.astype(jnp.float32)[:, None] * inv_freq[None, :]
    cos = jnp.concatenate([jnp.cos(ang_r), jnp.cos(ang_r), jnp.cos(ang_c), jnp.cos(ang_c)], axis=-1)
    sin = jnp.concatenate([jnp.sin(ang_r), jnp.sin(ang_r), jnp.sin(ang_c), jnp.sin(ang_c)], axis=-1)
    return cos.astype(dtype), sin.astype(dtype)


def apply_axial_rope(x, cos, sin):
    xr, xc = jnp.split(x, 2, axis=-1)
    rot = jnp.concatenate([rotate_half(xr), rotate_half(xc)], axis=-1)
    return x * cos[None, :, None, :] + rot * sin[None, :, None, :]


def gqa_attention(q, k, v, q_norm_w, k_norm_w, cos, sin):
    b, s = q.shape[0], q.shape[1]
    q = apply_axial_rope(rmsnorm(q, q_norm_w), cos, sin)
    k = apply_axial_rope(rmsnorm(k, k_norm_w), cos, sin)
    scale = 1.0 / math.sqrt(HEAD_DIM)
    nb = s // Q_BLOCK
    qb = jnp.moveaxis(q.reshape(b, nb, Q_BLOCK, N_KV_HEADS, GQA_GROUP, HEAD_DIM), 1, 0)

    def one_block(qblk):
        scores = jnp.einsum('bqkgd,bskd->bkgqs', qblk, k).astype(jnp.float32) * scale
        p = jax.nn.softmax(scores, axis=-1).astype(v.dtype)
        return jnp.einsum('bkgqs,bskd->bqkgd', p, v)

    o = lax.map(one_block, qb)
    return jnp.moveaxis(o, 0, 1).reshape(b, s, ATTN_W)


def depthwise_centred_conv(u, w, bias):
    pad = (D_CONV - 1) // 2
    y = lax.conv_general_dilated(u, w[:, None, :], window_strides=(1,), padding=[(pad, pad)],
                                 dimension_numbers=('NWC', 'WIO', 'NWC'),
                                 feature_group_count=u.shape[-1])
    return y + bias


def ssd_chunked(x, dt, a, bm, cm):
    b, L, h, p = x.shape
    n = bm.shape[-1]
    c = L // CHUNK
    xc = x.reshape(b, c, CHUNK, h, p)
    dtc = dt.reshape(b, c, CHUNK, h)
    bc = bm.reshape(b, c, CHUNK, h, n)
    cc = cm.reshape(b, c, CHUNK, h, n)
    a_cum = jnp.cumsum(dtc * a, axis=2)
    seg = a_cum[:, :, :, None, :] - a_cum[:, :, None, :, :]
    tril = jnp.tril(jnp.ones((CHUNK, CHUNK), dtype=bool))[None, None, :, :, None]
    decay = jnp.exp(jnp.where(tril, seg, -jnp.inf))
    xdt = xc * dtc[..., None]
    cb = jnp.einsum('bcihn,bcjhn->bcijh', cc, bc)
    y_diag = jnp.einsum('bcijh,bcjhp->bcihp', cb * decay, xdt)
    decay_to_end = jnp.exp(a_cum[:, :, -1:, :] - a_cum)
    states = jnp.einsum('bcjhn,bcjhp->bchpn', bc * decay_to_end[..., None], xdt)
    chunk_decay = jnp.exp(a_cum[:, :, -1, :])

    def step(carry, inp):
        s_c, d_c = inp
        return carry * d_c[:, :, None, None] + s_c, carry

    h0 = jnp.zeros((b, h, p, n), dtype=x.dtype)
    _, states_in = lax.scan(step, h0, (jnp.moveaxis(states, 1, 0), jnp.moveaxis(chunk_decay, 1, 0)))
    states_in = jnp.moveaxis(states_in, 0, 1)
    y_off = jnp.einsum('bcihn,bchpn->bcihp', cc, states_in) * jnp.exp(a_cum)[..., None]
    return (y_diag + y_off).reshape(b, L, h, p)


def bidirectional_ssd(z, xbc, dt_raw, conv_w, conv_b, dt_bias, a_log, d_skip, norm_w):
    b, s = xbc.shape[0], xbc.shape[1]
    xbc = jax.nn.silu(depthwise_centred_conv(xbc, conv_w, conv_b))
    xs, bm, cm = jnp.split(xbc, [SSD_W, SSD_W + SSD_GROUPS * D_STATE], axis=-1)
    f32 = jnp.float32
    xs = xs.reshape(b, s, SSD_HEADS, SSD_HEAD_DIM).astype(f32)
    bm = jnp.repeat(bm.reshape(b, s, SSD_GROUPS, D_STATE), HEADS_PER_GROUP, axis=2).astype(f32)
    cm = jnp.repeat(cm.reshape(b, s, SSD_GROUPS, D_STATE), HEADS_PER_GROUP, axis=2).astype(f32)
    dt = jax.nn.softplus(dt_raw.reshape(b, s, 2, SSD_HEADS).astype(f32) + dt_bias.astype(f32))
    a = -jnp.exp(a_log.astype(f32))
    y_fwd = ssd_chunked(xs, dt[:, :, 0], a[0], bm, cm)
    flip = lambda u: jnp.flip(u, axis=1)
    y_bwd = flip(ssd_chunked(flip(xs), flip(dt[:, :, 1]), a[1], flip(bm), flip(cm)))
    y = y_fwd + y_bwd + d_skip.astype(f32)[None, None, :, None] * xs
    y = y.reshape(b, s, SSD_W).astype(z.dtype)
    return rmsnorm(y * jax.nn.silu(z), norm_w)


def hierarchical_moe(h, w_rg, b_rg, w_re, b_re, w1, w3, w2):
    b, s, d = h.shape
    t = h.reshape(b * s, d)
    p_group = jax.nn.softmax((t @ w_rg + b_rg).astype(jnp.float32), axis=-1)
    g_val, g_idx = lax.top_k(p_group, 1)
    fine = (t @ w_re + b_re).astype(jnp.float32).reshape(-1, N_EXPERT_GROUPS, EXPERTS_PER_GROUP)
    fine_sel = jnp.take_along_axis(fine, g_idx[:, :, None], axis=1)[:, 0]
    e_val, e_idx = lax.top_k(jax.nn.softmax(fine_sel, axis=-1), TOP_K_IN_GROUP)
    e_val = e_val / jnp.sum(e_val, axis=-1, keepdims=True)
    within = jnp.sum(jax.nn.one_hot(e_idx, EXPERTS_PER_GROUP, dtype=jnp.float32) * e_val[..., None], axis=1)
    combine = (jax.nn.one_hot(g_idx[:, 0], N_EXPERT_GROUPS, dtype=jnp.float32)[:, :, None]
               * within[:, None, :] * g_val[:, :, None]).reshape(-1, N_EXPERTS).astype(t.dtype)
    out = jnp.zeros_like(t)
    for e in range(N_EXPERTS):
        y_e = (jax.nn.silu(t @ w1[e]) * (t @ w3[e])) @ w2[e]
        out = out + combine[:, e:e + 1] * y_e
    return out.reshape(b, s, d)


def setup_inputs(seed: int = 0) -> dict:
    key = jax.random.key(seed)
    ks = jax.random.split(key, 24)
    f32 = jnp.float32
    L = DEPTH

    def nrm(k, shape, fan_in):
        return jax.random.normal(k, shape, f32) * (fan_in ** -0.5)

    def gain(k, shape):
        return 1.0 + 0.02 * jax.random.normal(k, shape, f32)

    dt0 = jnp.exp(jax.random.uniform(ks[9], (L, 2, SSD_HEADS), f32, math.log(1e-3), math.log(1e-1)))
    dt_bias = dt0 + jnp.log(-jnp.expm1(-dt0))
    return {
        "x": jax.random.normal(ks[0], (BATCH, SEQ, D_MODEL), f32),
        "norm_mix_w": gain(ks[1], (L, D_MODEL)),
        "w_in": nrm(ks[2], (L, D_MODEL, IN_PROJ), D_MODEL),
        "b_gate": 0.02 * jax.random.normal(ks[3], (L, 2 * D_MODEL), f32),
        "q_norm_w": gain(ks[4], (L, HEAD_DIM)),
        "k_norm_w": gain(ks[5], (L, HEAD_DIM)),
        "w_attn_o": nrm(ks[6], (L, ATTN_W, D_MODEL), ATTN_W),
        "conv_w": nrm(ks[7], (L, D_CONV, CONV_CH), D_CONV),
        "conv_b": 0.02 * jax.random.normal(ks[8], (L, CONV_CH), f32),
        "dt_bias": dt_bias,
        "a_log": jnp.log(jax.random.uniform(ks[10], (L, 2, SSD_HEADS), f32, 1.0, 16.0)),
        "d_skip": 1.0 + 0.1 * jax.random.normal(ks[11], (L, SSD_HEADS), f32),
        "ssd_norm_w": gain(ks[12], (L, SSD_W)),
        "w_ssd_o": nrm(ks[13], (L, SSD_W, D_MODEL), SSD_W),
        "w_out": nrm(ks[14], (L, D_MODEL, D_MODEL), D_MODEL),
        "norm_ffn_w": gain(ks[15], (L, D_MODEL)),
        "w_router_group": nrm(ks[16], (L, D_MODEL, N_EXPERT_GROUPS), D_MODEL),
        "b_router_group": 0.01 * jax.random.normal(ks[17], (L, N_EXPERT_GROUPS), f32),
        "w_router_expert": nrm(ks[18], (L, D_MODEL, N_EXPERTS), D_MODEL),
        "b_router_expert": 0.01 * jax.random.normal(ks[19], (L, N_EXPERTS), f32),
        "w1": nrm(ks[20], (L, N_EXPERTS, D_MODEL, D_EXPERT), D_MODEL),
        "w3": nrm(ks[21], (L, N_EXPERTS, D_MODEL, D_EXPERT), D_MODEL),
        "w2": nrm(ks[22], (L, N_EXPERTS, D_EXPERT, D_MODEL), D_EXPERT),
    }


def reference(x, norm_mix_w, w_in, b_gate, q_norm_w, k_norm_w, w_attn_o, conv_w, conv_b,
              dt_bias, a_log, d_skip, ssd_norm_w, w_ssd_o, w_out, norm_ffn_w,
              w_router_group, b_router_group, w_router_expert, b_router_expert, w1, w3, w2):
    b, s, _ = x.shape
    cos, sin = axial_rope_tables(s, x.dtype)
    split_at = [int(v) for v in np.cumsum(SPLIT_SIZES)[:-1]]
    for l in range(DEPTH):
        h = rmsnorm(x, norm_mix_w[l])
        proj = h @ w_in[l]
        q, k, v, z, xbc, dt_raw, gates = jnp.split(proj, split_at, axis=-1)
        q = q.reshape(b, s, N_Q_HEADS, HEAD_DIM)
        k = k.reshape(b, s, N_KV_HEADS, HEAD_DIM)
        v = v.reshape(b, s, N_KV_HEADS, HEAD_DIM)
        attn = gqa_attention(q, k, v, q_norm_w[l], k_norm_w[l], cos, sin)
        ssd = bidirectional_ssd(z, xbc, dt_raw, conv_w[l], conv_b[l], dt_bias[l],
                                a_log[l], d_skip[l], ssd_norm_w[l])
        g_attn, g_ssd = jnp.split(jax.nn.sigmoid(gates + b_gate[l]), 2, axis=-1)
        merged = g_attn * (attn @ w_attn_o[l]) + g_ssd * (ssd @ w_ssd_o[l])
        x = x + merged @ w_out[l]
        h2 = rmsnorm(x, norm_ffn_w[l])
        x = x + hierarchical_moe(h2, w_router_group[l], b_router_group[l], w_router_expert[l],
                                 b_router_expert[l], w1[l], w3[l], w2[l])
    return x
```

```python
import os
from contextlib import ExitStack

import numpy as np
import ml_dtypes

import concourse.bass as bass
import concourse.mybir as mybir
from concourse.bass_utils import run_bass_kernel_spmd

F32 = mybir.dt.float32
BF16 = mybir.dt.bfloat16
AF = mybir.ActivationFunctionType
ALU = mybir.AluOpType
AX = mybir.AxisListType

S = 4096
D = 1024
NT = S // 128
NS = S // 512
EPS = 1e-6
INP = 4112
C_Q, C_K, C_V, C_Z, C_XBC, C_DT, C_G = 0, 512, 640, 768, 1280, 2048, 2064
NE = 16
DE = 512


class Dep:
    __slots__ = ("name", "last_w", "readers", "epoch")

    def __init__(self, name):
        self.name = name
        self.last_w = None
        self.readers = []
        self.epoch = -1


class Instr:
    __slots__ = ("eng", "fn", "deps", "is_dma", "sem", "val", "signal", "idx", "pos", "grp")


ENGS = ("sync", "scalar", "vector", "gpsimd", "tensor")


class Prog:
    def __init__(self, nc):
        self.nc = nc
        self.es = ExitStack()
        self.eng_sem = {}
        for e in ENGS:
            self.eng_sem[e] = self.es.enter_context(nc.semaphore("tick_" + e))
        self.eng_tick = {e: 0 for e in ENGS}
        self.dma_sems = {}
        self.instrs = []
        self.known = {e: {} for e in ENGS}
        self.n_total = 0
        self.epoch = 0

    def close(self):
        self.es.close()

    def _rec(self, eng, fn, reads, writes, is_dma=False, semkey=None, grp=None):
        it = Instr()
        it.grp = grp
        it.eng = eng
        it.fn = fn
        it.is_dma = is_dma
        it.signal = False
        it.sem = None
        it.val = None
        it.idx = len(self.instrs)
        for dd in list(reads) + list(writes):
            if dd.epoch != self.epoch:
                dd.epoch = self.epoch
                dd.last_w = None
                dd.readers = []
        deps = set()
        for r in reads:
            if r.last_w is not None:
                deps.add(r.last_w)
        for w in writes:
            if w.last_w is not None:
                if not (grp is not None and self.instrs[w.last_w].grp == grp):
                    deps.add(w.last_w)
            deps.update(w.readers)
        for r in reads:
            r.readers.append(it.idx)
        for w in writes:
            w.last_w = it.idx
            w.readers = []
        if is_dma:
            ent = self.dma_sems.get(semkey)
            if ent is None:
                sem = self.es.enter_context(self.nc.semaphore("dma_%d" % len(self.dma_sems)))
                ent = [sem, 0, None, -1]
                self.dma_sems[semkey] = ent
            if ent[3] == self.epoch and ent[2] is not None:
                if not (grp is not None and self.instrs[ent[2]].grp == grp):
                    deps.add(ent[2])
            ent[1] += 1
            ent[2] = it.idx
            ent[3] = self.epoch
            it.sem = ent[0]
            it.val = 16 * ent[1]
        deps.discard(it.idx)
        it.deps = deps
        self.instrs.append(it)
        return it

    def op(self, eng, fn, reads=(), writes=()):
        return self._rec(eng, fn, list(reads), list(writes))

    def dma(self, out, in_, reads=(), writes=(), semkey=None, eng="sync"):
        assert semkey is not None
        return self._rec(eng, lambda e: e.dma_start(out=out, in_=in_), list(reads), list(writes),
                         is_dma=True, semkey=semkey)

    def barrier(self):
        last = {}
        pend = set()
        for it in self.instrs:
            if it.is_dma:
                pend.add(it.idx)
            else:
                last[it.eng] = it.idx
        allidx = set(last.values()) | pend
        for e in ENGS:
            it = self._rec(e, lambda eng: eng.nop(), [], [])
            it.deps = set(allidx)

    def emit(self):
        instrs = self.instrs
        per_eng = {e: [] for e in ENGS}
        for it in instrs:
            it.pos = len(per_eng[it.eng])
            per_eng[it.eng].append(it)

        def skipped(src, it):
            if src.is_dma or it.is_dma:
                return False
            if src.eng != it.eng:
                return False
            if src.eng == "tensor":
                return True
            return it.pos - src.pos > 2

        for it in instrs:
            for d in it.deps:
                src = instrs[d]
                if src.is_dma or skipped(src, it):
                    continue
                src.signal = True
        for e in ENGS:
            t = self.eng_tick[e]
            for it in per_eng[e]:
                if not it.is_dma and it.signal:
                    t += 1
                    it.sem = self.eng_sem[e]
                    it.val = t
            self.eng_tick[e] = t
        known = self.known

        def run_engine(ename, eobj):
            kn = known[ename]
            for it in per_eng[ename]:
                need = {}
                for d in it.deps:
                    src = instrs[d]
                    if skipped(src, it):
                        continue
                    key = src.sem.name
                    if kn.get(key, 0) >= src.val:
                        continue
                    if key not in need or need[key][1] < src.val:
                        need[key] = (src.sem, src.val)
                for key, (sem, val) in need.items():
                    eobj.wait_ge(sem, val)
                    kn[key] = val
                bi = it.fn(eobj)
                if it.is_dma:
                    bi.then_inc(it.sem, 16)
                elif it.signal:
                    bi.then_inc(it.sem, 1)

        with self.nc.Block() as block:
            @block.sync
            def _(e):
                run_engine("sync", e)

            @block.scalar
            def _(e):
                run_engine("scalar", e)

            @block.vector
            def _(e):
                run_engine("vector", e)

            @block.gpsimd
            def _(e):
                run_engine("gpsimd", e)

            @block.tensor
            def _(e):
                run_engine("tensor", e)
        self.n_total += len(instrs)
        self.instrs = []
        self.epoch += 1


def build_program(dbg=None, stop_after=None):
    nc = bass.Bass("TRN2", target_bir_lowering=False)
    P = Prog(nc)
    dbg = dbg or []
    _uq = [0]

    def uq(name):
        _uq[0] += 1
        return "%s_%d" % (name, _uq[0])

    def din(name, shape, dt=F32):
        return nc.dram_tensor(name, list(shape), dt, kind="ExternalInput").ap()

    def dscr(name, shape, dt):
        return nc.dram_tensor(name, list(shape), dt, kind="Internal").ap()

    def dout(name, shape, dt):
        return nc.dram_tensor(name, list(shape), dt, kind="ExternalOutput").ap()

    x_d = din("x", [S, D])
    w_in_d = din("w_in", [D, INP])
    out_d = dout("out", [S, D], F32)
    ident_bf_d = din("ident_bf", [128, 128], BF16)
    nw_mix_d = din("nw_mix", [128, 8])
    qkw_d = din("qkw", [128, 2])
    onesblk_d = din("onesblk", [128, 128], BF16)
    rmat_d = din("rmat", [128, 128], BF16)
    cosT_d = din("cosT", [128, S], BF16)
    sinT_d = din("sinT", [128, S], BF16)
    masks_d = din("masks", [128, 4, 128])
    cw_d = din("cw", [128, 42])
    cb_d = din("cbias", [128, 6])
    dtb_d = din("dtb", [128, 16])
    alog_d = din("alog", [128, 16])
    dsk_d = din("dsk", [128, 8])
    nssd_d = din("nssd", [128, 4])
    w_ao_d = din("w_attn_o", [512, D])
    w_so_d = din("w_ssd_o", [512, D])
    w_out_d = din("w_out", [D, D])
    bg_d = din("bgate", [128, 16])
    nf_d = din("nffn", [128, 8])
    wr_d = din("wr", [D, 20])
    br_d = din("br", [128, 20])
    w1_d = din("w1", [NE * 128 * 2, 2048])
    w3_d = din("w3", [NE * 128 * 2, 2048])
    w2_d = din("w2", [NE * 128 * 2, 2048])

    hT_d = dscr("hT_scratch", [NS, 128, 8, 512], BF16)
    x1_d = dscr("x1_scratch", [S, D], F32)

    def dbgout(name, shape, dt):
        return dout("dbg_" + name, shape, dt)

    def mm(out, lhsT, rhs, start, stop, reads, writes):
        P.op("tensor", lambda e: e.matmul(out, lhsT=lhsT, rhs=rhs, start=start, stop=stop), reads, writes)

    def tr(out, in_, idn, reads, writes):
        P.op("tensor", lambda e: e.transpose(out=out, in_=in_, identity=idn), reads, writes)

    def act(out, in_, func, reads, writes, **kw):
        P.op("scalar", lambda e: e.activation(out=out, in_=in_, func=func, **kw), reads, writes)

    def tt(eng, out, in0, in1, op, reads, writes):
        P.op(eng, lambda e: e.tensor_tensor(out=out, in0=in0, in1=in1, op=op), reads, writes)

    def ts(eng, out, in0, s1, s2, op0, op1, reads, writes):
        if op1 is None:
            P.op(eng, lambda e: e.tensor_scalar(out=out, in0=in0, scalar1=s1, scalar2=None, op0=op0), reads, writes)
        else:
            P.op(eng, lambda e: e.tensor_scalar(out=out, in0=in0, scalar1=s1, scalar2=s2, op0=op0, op1=op1), reads, writes)

    def stt(out, in0, scalar, in1, op0, op1, reads, writes):
        P.op("vector", lambda e: e.scalar_tensor_tensor(out=out, in0=in0, scalar=scalar, in1=in1, op0=op0, op1=op1),
             reads, writes)

    def cp(eng, out, in_, reads, writes):
        if eng == "scalar":
            P.op(eng, lambda e: e.copy(out=out, in_=in_), reads, writes)
        else:
            P.op(eng, lambda e: e.tensor_copy(out=out, in_=in_), reads, writes)

    def recip(out, in_, reads, writes):
        P.op("vector", lambda e: e.reciprocal(out=out, in_=in_), reads, writes)

    def memset(eng, ap, val, writes):
        P.op(eng, lambda e: e.memset(ap, val), [], writes)

    def red(out, in_, op, reads, writes):
        P.op("vector", lambda e: e.tensor_reduce(out=out, in_=in_, axis=AX.X, op=op), reads, writes)

    stg_ctr = [0]

    def load_w(stg, d_stg, src_view, KC, ncols, dst_fn, d_dst, scale=None, d_scale=None, piece=256, eng="vector"):
        for c0 in range(0, ncols, piece):
            n = min(piece, ncols - c0)
            sl = stg_ctr[0] % len(stg)
            stg_ctr[0] += 1
            P.dma(stg[sl][:, 0:KC, 0:n], src_view[:, :, c0:c0 + n], writes=[d_stg[sl]], semkey=("stg", sl))
            if scale is not None:
                tt(eng, dst_fn(c0, n), stg[sl][:, 0:KC, 0:n], scale.unsqueeze(2).to_broadcast([128, KC, n]), ALU.mult,
                   [d_stg[sl], d_scale], [d_dst])
            else:
                cp(eng, dst_fn(c0, n), stg[sl][:, 0:KC, 0:n], [d_stg[sl]], [d_dst])

    w_view = w_in_d.rearrange("(kc p) n -> p kc n", p=128)

    es_top = ExitStack()
    sbT = lambda name, shape, dt: es_top.enter_context(nc.sbuf_tensor(uq(name), list(shape), dt))
    ident = sbT("ident", [128, 128], BF16)
    d_ident = Dep("ident")
    nw = sbT("s_nw", [128, 8], F32)
    d_nw = Dep("nw")
    epsb = sbT("epsb", [128, 1], F32)
    d_eps = Dep("eps")
    es_ssd = ExitStack()
    ssdT = es_ssd.enter_context(nc.sbuf_tensor(uq("ssdT"), [128, 4, S], BF16))
    d_ssdT = Dep("ssdT")

    def finish():
        es_ssd.close()
        es_top.close()
        P.close()
        return nc

    with ExitStack() as ph:
        sb = lambda name, shape, dt: ph.enter_context(nc.sbuf_tensor(uq(name), list(shape), dt))
        ps = lambda name, shape, dt: ph.enter_context(nc.psum_tensor(uq(name), list(shape), dt))
        P.dma(ident[:, :], ident_bf_d[:, :], writes=[d_ident], semkey="c0")
        P.dma(nw[:, :], nw_mix_d[:, :], writes=[d_nw], semkey="c1")
        memset("gpsimd", epsb[:, :], EPS, [d_eps])
        NXB = 6
        xt = [sb("xt%d" % i, [128, D], F32) for i in range(NXB)]
        d_xt = [Dep("xt%d" % i) for i in range(NXB)]
        junk = sb("junk", [128, D], BF16)
        d_junk = Dep("junk")
        xn = [sb("xn%d" % i, [128, D], BF16) for i in range(2)]
        d_xn = [Dep("xn%d" % i) for i in range(2)]
        ss = sb("ss", [128, NT], F32)
        rs = sb("rs", [128, NT], F32)
        d_ss = [Dep("ss%d" % i) for i in range(NT)]
        d_rs = [Dep("rs%d" % i) for i in range(NT)]
        tp = [ps("tp%d" % i, [128, 8, 128], BF16) for i in range(2)]
        d_tp = [Dep("tp%d" % i) for i in range(2)]
        hs = [sb("hs%d" % i, [128, 8, 512], BF16) for i in range(2)]
        d_hs = [Dep("hs%d" % i) for i in range(2)]
        def p1_load(t):
            xb = t % NXB
            P.dma(xt[xb][:, :], x_d[t * 128:(t + 1) * 128, :], writes=[d_xt[xb]], semkey=("xt", xb))

        def p1_stage1(t):
            xb = t % NXB
            act(junk[:, :], xt[xb][:, :], AF.Square, [d_xt[xb]], [d_junk, d_ss[t]], accum_out=ss[:, t:t + 1])
            act(rs[:, t:t + 1], ss[:, t:t + 1], AF.Sqrt, [d_ss[t], d_eps], [d_rs[t]], bias=epsb[:, 0:1], scale=1.0 / D)
            recip(rs[:, t:t + 1], rs[:, t:t + 1], [d_rs[t]], [d_rs[t]])
            nb = t % 2
            ts("vector", xn[nb][:, :], xt[xb][:, :], rs[:, t:t + 1], None, ALU.mult, None, [d_xt[xb], d_rs[t]], [d_xn[nb]])

        def p1_stage2(t):
            j, i4 = divmod(t, 4)
            nb = t % 2
            for c in range(8):
                tr(tp[nb][:, c, :], xn[nb][:, c * 128:(c + 1) * 128], ident[:, :], [d_xn[nb], d_ident], [d_tp[nb]])
            hb = j % 2
            cp("scalar", hs[hb][:, :, i4 * 128:(i4 + 1) * 128], tp[nb][:, :, :], [d_tp[nb]], [d_hs[hb]])
            if i4 == 3:
                P.dma(hT_d[j], hs[hb][:, :, :], reads=[d_hs[hb]], semkey=("hs", hb))

        LA = 4
        for t in range(LA):
            p1_load(t)
        p1_stage1(0)
        for t in range(NT):
            if t + LA < NT:
                p1_load(t + LA)
            if t + 1 < NT:
                p1_stage1(t + 1)
            p1_stage2(t)
        P.barrier()
        P.emit()
    if stop_after == 1:
        return finish()

    es4 = ExitStack()
    sb4 = lambda name, shape, dt: es4.enter_context(nc.sbuf_tensor(uq(name), list(shape), dt))
    xs_tok = sb4("xs_tok", [128, NT, 512], BF16)
    B_tok = sb4("B_tok", [128, NT, 128], BF16)
    BT = sb4("BT", [128, S], BF16)
    CTz = sb4("CTz", [128, 2, S], BF16)
    dtv = sb4("dtv", [128, NT, 16], F32)
    dAv = sb4("dAv", [128, NT, 16], F32)
    d_xs = [Dep("xs%d" % t) for t in range(NT)]
    d_Bt = [Dep("Bt%d" % t) for t in range(NT)]
    d_BT = [Dep("BT%d" % j) for j in range(NS)]
    d_CT = [Dep("CT%d" % j) for j in range(NS)]
    d_dt = [Dep("dt%d" % t) for t in range(NT)]
    d_CTinit = Dep("CTinit")

    with ExitStack() as ph:
        sb = lambda name, shape, dt: ph.enter_context(nc.sbuf_tensor(uq(name), list(shape), dt))
        ps = lambda name, shape, dt: ph.enter_context(nc.psum_tensor(uq(name), list(shape), dt))
        cw = sb("s_cw", [128, 42], F32); d_cw = Dep("cw")
        cbias = sb("s_cb", [128, 6], F32); d_cb = Dep("cb")
        dtb = sb("s_dtb", [128, 16], F32); d_dtb = Dep("dtb")
        aneg = sb("s_aneg", [128, 16], F32); d_an = Dep("aneg")
        P.dma(cw[:, :], cw_d[:, :], writes=[d_cw], semkey="c0")
        P.dma(cbias[:, :], cb_d[:, :], writes=[d_cb], semkey="c1")
        P.dma(dtb[:, :], dtb_d[:, :], writes=[d_dtb], semkey="c2")
        P.dma(aneg[:, :], alog_d[:, :], writes=[d_an], semkey="c3")
        act(aneg[:, :], aneg[:, :], AF.Exp, [d_an], [d_an])
        ts("vector", aneg[:, :], aneg[:, :], -1.0, None, ALU.mult, None, [d_an], [d_an])
        diagw = sb("diagw", [128, 42, 128], BF16); d_dg = Dep("diagw")
        for i in range(42):
            ts("vector", diagw[:, i, :], ident[:, :], cw[:, i:i + 1], None, ALU.mult, None, [d_ident, d_cw], [d_dg])
        memset("gpsimd", CTz[:, :, :], 0.0, [d_CTinit])
        Wx = sb("Wx", [128, 8, 768], BF16); Wd = sb("Wd", [128, 8, 16], BF16); d_W = Dep("W4a")
        stg = [sb("stg%d" % i, [128, 8, 256], F32) for i in range(2)]; d_stg = [Dep("stg%d" % i) for i in range(2)]
        load_w(stg, d_stg, w_view[:, :, C_XBC:C_XBC + 768], 8, 768, lambda c0, n: Wx[:, :, c0:c0 + n], d_W, nw[:, :], d_nw)
        load_w(stg, d_stg, w_view[:, :, C_DT:C_DT + 16], 8, 16, lambda c0, n: Wd[:, :, c0:c0 + n], d_W, nw[:, :], d_nw)
        xraw = sb("xraw", [128, 6, S + 8], BF16)
        d_xr = [Dep("xr%d" % j) for j in range(NS)]
        d_xpad = Dep("xpad")
        memset("gpsimd", xraw[:, :, 0:3], 0.0, [d_xpad])
        memset("gpsimd", xraw[:, :, S + 3:S + 8], 0.0, [d_xpad])
        hts = [sb("hts%d" % i, [128, 8, 512], BF16) for i in range(2)]; d_hts = [Dep("hts%d" % i) for i in range(2)]
        pp = [ps("pp%d" % i, [128, 512], F32) for i in range(2)]; d_pp = [Dep("pp%d" % i) for i in range(2)]
        pd = [ps("pd%d" % i, [128, 16], F32) for i in range(2)]; d_pd = [Dep("pd%d" % i) for i in range(2)]
        ptr = [ps("ptr%d" % i, [128, 4, 128], BF16) for i in range(2)]; d_ptr = [Dep("ptr%d" % i) for i in range(2)]
        cvo = [sb("cvo%d" % i, [128, 512], BF16) for i in range(2)]; d_cvo = [Dep("cvo%d" % i) for i in range(2)]
        dtt = [sb("dtt%d" % i, [128, 16], F32) for i in range(2)]; d_dtt = [Dep("dtt%d" % i) for i in range(2)]
        u = 0
        for j in range(NS):
            hb = j % 2
            P.dma(hts[hb][:, :, :], hT_d[j], writes=[d_hts[hb]], semkey=("hts", hb))
            for c in range(6):
                b = u % 2
                u += 1
                for kc in range(8):
                    mm(pp[b][:, :], Wx[:, kc, c * 128:(c + 1) * 128], hts[hb][:, kc, :], kc == 0, kc == 7,
                       [d_W, d_hts[hb]], [d_pp[b]])
                cp("scalar", xraw[:, c, 3 + j * 512:3 + (j + 1) * 512], pp[b][:, :], [d_pp[b], d_xpad], [d_xr[j]])
            for i4 in range(4):
                t = j * 4 + i4
                b = t % 2
                for kc in range(8):
                    mm(pd[b][:, :], hts[hb][:, kc, i4 * 128:(i4 + 1) * 128], Wd[:, kc, :], kc == 0, kc == 7,
                       [d_W, d_hts[hb]], [d_pd[b]])
                tt("vector", dtt[b][:, :], pd[b][:, :], dtb[:, :], ALU.add, [d_pd[b], d_dtb], [d_dtt[b]])
                act(dtt[b][:, :], dtt[b][:, :], AF.Exp, [d_dtt[b]], [d_dtt[b]])
                act(dtv[:, t, :], dtt[b][:, :], AF.Ln, [d_dtt[b]], [d_dt[t]], bias=1.0)
                tt("vector", dAv[:, t, :], dtv[:, t, :], aneg[:, :], ALU.mult, [d_dt[t], d_an], [d_dt[t]])
        for j in range(NS):
            rd = [d_xr[j], d_xpad, d_dg]
            if j > 0:
                rd.append(d_xr[j - 1])
            if j < NS - 1:
                rd.append(d_xr[j + 1])
            for c in range(6):
                b = u % 2
                u += 1
                for tap in range(7):
                    mm(pp[b][:, :], diagw[:, c * 7 + tap, :], xraw[:, c, j * 512 + tap:j * 512 + tap + 512], tap == 0, tap == 6,
                       rd, [d_pp[b]])
                js = slice(j * 512, (j + 1) * 512)
                if c < 5:
                    dst = cvo[b][:, :] if c < 4 else BT[:, js]
                    wr = [d_cvo[b]] if c < 4 else [d_BT[j]]
                    act(dst, pp[b][:, :], AF.Silu, [d_pp[b], d_cb], wr, bias=cbias[:, c:c + 1])
                    for i4 in range(4):
                        t = j * 4 + i4
                        if c < 4:
                            tr(ptr[b][:, i4, :], cvo[b][:, i4 * 128:(i4 + 1) * 128], ident[:, :], [d_cvo[b], d_ident], [d_ptr[b]])
                        else:
                            tr(ptr[b][:, i4, :], BT[:, t * 128:(t + 1) * 128], ident[:, :], [d_BT[j], d_ident], [d_ptr[b]])
                    if c < 4:
                        cp("vector", xs_tok[:, j * 4:(j + 1) * 4, c * 128:(c + 1) * 128], ptr[b][:, :, :], [d_ptr[b]],
                           [d_xs[j * 4 + i] for i in range(4)])
                    else:
                        cp("vector", B_tok[:, j * 4:(j + 1) * 4, :], ptr[b][:, :, :], [d_ptr[b]],
                           [d_Bt[j * 4 + i] for i in range(4)])
                else:
                    act(CTz[0:64, 0, js], pp[b][0:64, :], AF.Silu, [d_pp[b], d_cb, d_CTinit], [d_CT[j]], bias=cbias[0:64, 5:6])
                    act(CTz[64:128, 1, js], pp[b][64:128, :], AF.Silu, [d_pp[b], d_cb, d_CTinit], [d_CT[j]], bias=cbias[64:128, 5:6])
        if "ssd_a" in dbg:
            o1 = dbgout("xs_tok", [128, NT, 512], BF16); o2 = dbgout("B_tok", [128, NT, 128], BF16)
            o3 = dbgout("CTz", [128, 2, S], BF16); o4 = dbgout("dtv", [128, NT, 16], F32); o5 = dbgout("BT", [128, S], BF16)
            P.dma(o1, xs_tok[:, :, :], reads=d_xs, semkey="d0")
            P.dma(o2, B_tok[:, :, :], reads=d_Bt, semkey="d1")
            P.dma(o3, CTz[:, :, :], reads=d_CT, semkey="d2")
            P.dma(o4, dtv[:, :, :], reads=d_dt, semkey="d3")
            P.dma(o5, BT[:, :], reads=d_BT, semkey="d4")
        P.barrier()
        P.emit()
    if stop_after == 41:
        es4.close()
        return finish()

    with ExitStack() as ph:
        sb = lambda name, shape, dt: ph.enter_context(nc.sbuf_tensor(uq(name), list(shape), dt))
        ps = lambda name, shape, dt: ph.enter_context(nc.psum_tensor(uq(name), list(shape), dt))
        masks = sb("s_masks", [128, 4, 128], F32); d_mk = Dep("masks")
        P.dma(masks[:, :, :], masks_d[:, :, :], writes=[d_mk], semkey="c0")
        MU, MSL, ML, MSU = 0, 1, 2, 3
        dsk = sb("s_dsk", [128, 8], F32); d_dsk = Dep("dsk")
        P.dma(dsk[:, :], dsk_d[:, :], writes=[d_dsk], semkey="c1")
        ones_f = sb("ones_f", [128, 128], F32); d_of = Dep("ones_f")
        memset("gpsimd", ones_f[:, :], 1.0, [d_of])
        Wz = sb("Wz", [128, 8, 512], BF16); d_Wz = Dep("Wz")
        stg = [sb("stg%d" % i, [128, 8, 256], F32) for i in range(2)]; d_stg = [Dep("stg%d" % i) for i in range(2)]
        load_w(stg, d_stg, w_view[:, :, C_Z:C_Z + 512], 8, 512, lambda c0, n: Wz[:, :, c0:c0 + n], d_Wz, nw[:, :], d_nw)
        Sb_all = sb("Sb_all", [128, NT, 256], BF16)
        d_Sb = [Dep("Sb%d" % t) for t in range(NT)]
        St = [sb("St%d" % i, [128, 256], F32) for i in range(2)]
        d_St = [Dep("St%d" % i) for i in range(2)]
        Sf_bf = [sb("Sfbf%d" % i, [128, 256], BF16) for i in range(2)]; d_Sfbf = [Dep("Sfbf%d" % i) for i in range(2)]
        pcb_t = ps("pcb_t", [128, 512], F32)
        pcb = pcb_t[:, 0:256].rearrange("p (g i) -> p g i", g=2); d_pcb = Dep("pcb")
        psm_main = [pcb_t[:, 256:352]]; d_psm_main = [d_pcb]
        pst = ps("pst", [128, 512], F32); d_pst = Dep("pst")
        pseg = [ps("pseg%d" % i, [128, 4, 128], F32) for i in range(2)]; d_pseg = [Dep("pseg%d" % i) for i in range(2)]
        pyd = ps("pyd", [128, 512], F32); d_pyd = Dep("pyd")
        pyo = ps("pyo", [128, 512], F32); d_pyo = Dep("pyo")
        pz = ps("pz", [128, 512], F32); d_pz = Dep("pz")
        sm_ = [sb("sm%d" % i, [128, 96], F32) for i in range(2)]; d_sm_ = [Dep("sm%d" % i) for i in range(2)]
        cd2 = sb("cd2", [128, 2, 4], F32); d_cd2 = Dep("cd2")
        dtw = sb("dtw", [128, 16], F32); d_dtw = Dep("dtw")
        xdt = [sb("xdt%d" % i, [128, 512], BF16) for i in range(2)]; d_xdt = [Dep("xdt%d" % i) for i in range(2)]
        xw = sb("xw", [128, 512], BF16); d_xw = Dep("xw")
        cbm = [sb("cbm%d" % i, [128, 2, 128], F32) for i in range(2)]; d_cbm = [Dep("cbm%d" % i) for i in range(2)]
        Lh_ = [sb("Lh%d" % i, [128, 8, 128], F32) for i in range(2)]; d_Lh_ = [Dep("Lh%d" % i) for i in range(2)]
        Ee_ = [sb("Ee%d" % i, [128, 8, 128], F32) for i in range(2)]; d_Ee_ = [Dep("Ee%d" % i) for i in range(2)]
        MT_ = [sb("MT%d" % i, [128, 8, 128], BF16) for i in range(2)]; d_MT_ = [Dep("MT%d" % i) for i in range(2)]
        ya_ = [sb("ya%d" % i, [128, 512], F32) for i in range(2)]; d_ya_ = [Dep("ya%d" % i) for i in range(2)]
        yb = sb("yb", [128, 512], F32); d_yb = Dep("yb")
        zs_ = [sb("zs%d" % i, [128, 512], F32) for i in range(2)]; d_zs_ = [Dep("zs%d" % i) for i in range(2)]
        yn = sb("yn", [128, 512], BF16); d_yn = Dep("yn")
        junk4 = sb("junk4", [128, 512], BF16); d_j4 = Dep("junk4")
        ssq = sb("ssq", [128, 2], F32); d_ssq = Dep("ssq")
        ptr4 = ps("ptr4", [128, 4, 128], BF16); d_ptr4 = Dep("ptr4")
        hts = [sb("hts%d" % i, [128, 8, 512], BF16) for i in range(2)]; d_hts = [Dep("hts%d" % i) for i in range(2)]
        for i in range(2):
            memset("gpsimd", St[i][:, :], 0.0, [d_St[i]])

        Bdef = (cd2, d_cd2, dtw, d_dtw, xw, d_xw, pst, d_pst)
        def small_sums(t, which, sm, d_sm, psmo=None):
            psm, d_psm = ([psmo[0]], [psmo[1]]) if psmo is not None else (psm_main, d_psm_main)
            rdA = [d_dt[t], d_mk, d_of]
            if "acum" in which:
                mm(psm[0][:, 0:8], masks[:, MU, :], dAv[:, t, 0:8], True, True, rdA, [d_psm[0]])
                mm(psm[0][:, 8:16], masks[:, ML, :], dAv[:, t, 8:16], True, True, rdA, [d_psm[0]])
                mm(psm[0][:, 16:24], masks[:, MSL, :], dAv[:, t, 0:8], True, True, rdA, [d_psm[0]])
                mm(psm[0][:, 24:40], ones_f[:, :], dAv[:, t, 0:16], True, True, rdA, [d_psm[0]])
                act(sm[:, 0:40], psm[0][:, 0:40], AF.Exp, [d_psm[0]], [d_sm])
            else:
                mm(psm[0][:, 64:72], masks[:, MSU, :], dAv[:, t, 8:16], True, True, rdA, [d_psm[0]])
                mm(psm[0][:, 72:88], ones_f[:, :], dAv[:, t, 0:16], True, True, rdA, [d_psm[0]])
                act(sm[:, 64:88], psm[0][:, 64:88], AF.Exp, [d_psm[0]], [d_sm])

        def state_step1(t, d, sm, d_sm, bufs=None):
            cd2, d_cd2, dtw, d_dtw, xw, d_xw, pst, d_pst = bufs if bufs is not None else Bdef
            tot0 = 24 if d == 0 else 72
            cdv = sm[:, tot0:tot0 + 16].rearrange("p (a h) -> p a h", a=2)
            cp("gpsimd", cd2[0:64, :, :], cdv[0:64, :, 0:4], [d_sm], [d_cd2])
            cp("gpsimd", cd2[64:128, :, :], cdv[64:128, :, 4:8], [d_sm], [d_cd2])
            dcol = 16 if d == 0 else 64
            tt("gpsimd", dtw[:, 0:8], dtv[:, t, d * 8:(d + 1) * 8], sm[:, dcol:dcol + 8], ALU.mult, [d_dt[t], d_sm], [d_dtw])
            tt("gpsimd", xw[:, :].rearrange("p (h q) -> p h q", h=8), xs_tok[:, t, :].rearrange("p (h q) -> p h q", h=8),
               dtw[:, 0:8].unsqueeze(2).to_broadcast([128, 8, 64]), ALU.mult, [d_xs[t], d_dtw], [d_xw])
            for g in range(2):
                mm(pst[:, g * 256:(g + 1) * 256], B_tok[:, t, :], xw[:, g * 256:(g + 1) * 256], True, True,
                   [d_Bt[t], d_xw], [d_pst])

        def state_step2(t, d, bufs=None):
            cd2, d_cd2, dtw, d_dtw, xw, d_xw, pst, d_pst = bufs if bufs is not None else Bdef
            tt("vector", St[d][:, :].rearrange("p (h q) -> p h q", h=4), St[d][:, :].rearrange("p (h q) -> p h q", h=4),
               cd2[:, d, :].unsqueeze(2).to_broadcast([128, 4, 64]), ALU.mult, [d_St[d], d_cd2], [d_St[d]])
            tt("vector", St[d][0:64, :], St[d][0:64, :], pst[0:64, 0:256], ALU.add, [d_St[d], d_pst], [d_St[d]])
            tt("vector", St[d][64:128, :], St[d][64:128, :], pst[64:128, 256:512], ALU.add, [d_St[d], d_pst], [d_St[d]])

        RB = 4
        Bring = [Bdef]
        pst_ring = [(pst, d_pst), (pyd, d_pyd), (pyo, d_pyo), (pz, d_pz)]
        for r in range(1, RB):
            Bring.append((sb("cd2r%d" % r, [128, 2, 4], F32), Dep("cd2r%d" % r), sb("dtwr%d" % r, [128, 16], F32), Dep("dtwr%d" % r),
                          sb("xwr%d" % r, [128, 512], BF16), Dep("xwr%d" % r), pst_ring[r][0], pst_ring[r][1]))
        smB = [sm_[0], sm_[1], sb("smr2", [128, 96], F32), sb("smr3", [128, 96], F32)]
        d_smB = [d_sm_[0], d_sm_[1], Dep("smr2"), Dep("smr3")]
        psmB = [(pseg[i][:, 0, :], d_pseg[i]) for i in range(2)]
        orderB = list(range(NT - 1, -1, -1))

        def preB(k):
            t, r = orderB[k], k % RB
            small_sums(t, ["dte_b"], smB[r], d_smB[r], psmB[k % 2])
            state_step1(t, 1, smB[r], d_smB[r], Bring[r])

        for k in range(min(RB - 1, NT)):
            preB(k)
        for k, t in enumerate(orderB):
            cp("vector", Sb_all[:, t, :], St[1][:, :], [d_St[1]], [d_Sb[t]])
            state_step2(t, 1, Bring[k % RB])
            if k + RB - 1 < NT:
                preB(k + RB - 1)
        A_NT = int(os.environ.get("SSD_NT", str(NT)))

        def tail_pool(t):
            ya, d_ya, zs, d_zs = ya_[t % 2], d_ya_[t % 2], zs_[t % 2], d_zs_[t % 2]
            tt("gpsimd", ya[:, :], ya[:, :], yb[:, :], ALU.add, [d_ya, d_yb], [d_ya])
            tt("gpsimd", yb[:, :].rearrange("p (h q) -> p h q", h=8), xs_tok[:, t, :].rearrange("p (h q) -> p h q", h=8),
               dsk[:, :].unsqueeze(2).to_broadcast([128, 8, 64]), ALU.mult, [d_xs[t], d_dsk], [d_yb])
            tt("gpsimd", ya[:, :], ya[:, :], yb[:, :], ALU.add, [d_ya, d_yb], [d_ya])
            tt("gpsimd", ya[:, :], ya[:, :], zs[:, :], ALU.mult, [d_ya, d_zs], [d_ya])

        def tail_act(t):
            ya, d_ya = ya_[t % 2], d_ya_[t % 2]
            act(junk4[:, :], ya[:, :], AF.Square, [d_ya], [d_j4, d_ssq], accum_out=ssq[:, 0:1])
            act(ssq[:, 1:2], ssq[:, 0:1], AF.Sqrt, [d_ssq, d_eps], [d_ssq], bias=epsb[:, 0:1], scale=1.0 / 512)

        def tail_dve(t):
            ya, d_ya = ya_[t % 2], d_ya_[t % 2]
            recip(ssq[:, 1:2], ssq[:, 1:2], [d_ssq], [d_ssq])
            ts("vector", yn[:, :], ya[:, :], ssq[:, 1:2], None, ALU.mult, None, [d_ya, d_ssq], [d_yn])

        def tail_pe(t):
            for c in range(4):
                tr(ptr4[:, c, :], yn[:, c * 128:(c + 1) * 128], ident[:, :], [d_yn, d_ident], [d_ptr4])

        def tail_out(t):
            cp("scalar", ssdT[:, :, t * 128:(t + 1) * 128], ptr4[:, :, :], [d_ptr4], [d_ssdT])

        pend = None
        for t in range(A_NT):
            j, i4 = divmod(t, 4)
            hb = j % 2
            tok = slice(t * 128, (t + 1) * 128)
            if i4 == 0:
                P.dma(hts[hb][:, :, :], hT_d[j], writes=[d_hts[hb]], semkey=("hts", hb))
            fb = t % 2
            ya, d_ya, zs, d_zs = ya_[t % 2], d_ya_[t % 2], zs_[t % 2], d_zs_[t % 2]
            sm, d_sm = sm_[t % 2], d_sm_[t % 2]
            for kc in range(8):
                mm(pz[:, :], hts[hb][:, kc, i4 * 128:(i4 + 1) * 128], Wz[:, kc, :], kc == 0, kc == 7, [d_hts[hb], d_Wz], [d_pz])
            act(zs[:, :], pz[:, :], AF.Silu, [d_pz], [d_zs])
            cp("gpsimd", Sf_bf[fb][:, :], St[0][:, :], [d_St[0]], [d_Sfbf[fb]])
            for d in range(2):
                dsl = slice(d * 8, (d + 1) * 8)
                lmask = MSL if d == 0 else MSU
                tt("vector" if d == 0 else "gpsimd", xdt[d][:, :].rearrange("p (h q) -> p h q", h=8), xs_tok[:, t, :].rearrange("p (h q) -> p h q", h=8),
                   dtv[:, t, dsl].unsqueeze(2).to_broadcast([128, 8, 64]), ALU.mult, [d_xs[t], d_dt[t]], [d_xdt[d]])
                tt("vector" if d == 0 else "gpsimd", Lh_[d][:, :, :], masks[:, lmask:lmask + 1, :].to_broadcast([128, 8, 128]),
                   dAv[:, t, dsl].unsqueeze(2).to_broadcast([128, 8, 128]), ALU.mult, [d_mk, d_dt[t]], [d_Lh_[d]])
            small_sums(t, ["acum", "dte_f"], sm, d_sm)
            if pend is not None:
                tail_act(pend)
            for g in range(2):
                mm(pcb[:, g, :], BT[:, tok], CTz[:, g, tok], True, True, [d_BT[j], d_CT[j]], [d_pcb])

            def seg(d):
                tri = MU if d == 0 else ML
                for h in range(8):
                    mm(pseg[h // 4][:, h % 4, :], Lh_[d][:, h, :], masks[:, tri, :], True, True, [d_Lh_[d], d_mk], [d_pseg[h // 4]])
                for q in range(2):
                    act(Ee_[d][:, q * 4:(q + 1) * 4, :], pseg[q][:, :, :], AF.Exp, [d_pseg[q]], [d_Ee_[d]])

            def mtmul(d):
                for g in range(2):
                    tt("vector" if d == 0 else "gpsimd", MT_[d][:, g * 4:(g + 1) * 4, :], Ee_[d][:, g * 4:(g + 1) * 4, :],
                       cbm[d][:, g:g + 1, :].to_broadcast([128, 4, 128]), ALU.mult, [d_Ee_[d], d_cbm[d]], [d_MT_[d]])

            def ymm(d):
                for h in range(8):
                    mm(pyd[:, h * 64:(h + 1) * 64], MT_[d][:, h, :], xdt[d][:, h * 64:(h + 1) * 64], True, True,
                       [d_MT_[d], d_xdt[d]], [d_pyd])
                Sin = Sf_bf[fb][:, :] if d == 0 else Sb_all[:, t, :]
                dS = d_Sfbf[fb] if d == 0 else d_Sb[t]
                for g in range(2):
                    mm(pyo[:, g * 256:(g + 1) * 256], CTz[:, g, tok], Sin, True, True, [d_CT[j], dS], [d_pyo])

            def yacc(d):
                dsl = slice(d * 8, (d + 1) * 8)
                yy = ya if d == 0 else yb
                dy = d_ya if d == 0 else d_yb
                tt("vector", yy[:, :].rearrange("p (h q) -> p h q", h=8), pyo[:, :].rearrange("p (h q) -> p h q", h=8),
                   sm[:, dsl].unsqueeze(2).to_broadcast([128, 8, 64]), ALU.mult, [d_pyo, d_sm], [dy])
                tt("vector", yy[:, :], yy[:, :], pyd[:, :], ALU.add, [dy, d_pyd], [dy])

            seg(0)
            state_step1(t, 0, sm, d_sm)
            tt("vector", cbm[0][:, :, :], pcb[:, :, :], masks[:, MU:MU + 1, :].to_broadcast([128, 2, 128]), ALU.mult,
               [d_pcb, d_mk], [d_cbm[0]])
            tt("vector", cbm[1][:, :, :], pcb[:, :, :], masks[:, ML:ML + 1, :].to_broadcast([128, 2, 128]), ALU.mult,
               [d_pcb, d_mk], [d_cbm[1]])
            seg(1)
            mtmul(0)
            ymm(0)
            state_step2(t, 0)
            mtmul(1)
            yacc(0)
            if pend is not None:
                tail_dve(pend)
            ymm(1)
            if pend is not None:
                tail_pe(pend)
            yacc(1)
            if pend is not None:
                tail_out(pend)
            tail_pool(t)
            pend = t
        tail_act(pend)
        tail_dve(pend)
        tail_pe(pend)
        tail_out(pend)
        if "ssdT" in dbg:
            o1 = dbgout("ssdT", [128, 4, S], BF16)
            P.dma(o1, ssdT[:, :, :], reads=[d_ssdT], semkey="d0")
        P.barrier()
        P.emit()
    es4.close()
    if stop_after == 4:
        return finish()


    es35 = ExitStack()
    sb35 = lambda name, shape, dt: es35.enter_context(nc.sbuf_tensor(uq(name), list(shape), dt))
    attnT = sb35("attnT", [128, 4, S], BF16)
    d_attn = Dep("attnT")
    es23 = ExitStack()
    sb23 = lambda name, shape, dt: es23.enter_context(nc.sbuf_tensor(uq(name), list(shape), dt))
    kTd = sb23("kTd", [128, 2, S], BF16)
    Va = sb23("Va", [128, NT, 2, 128], BF16)
    Vb = sb23("Vb", [128, NT, 2, 128], BF16)
    cosT = sb23("s_cosT", [128, S], BF16); d_cos = Dep("cos")
    sinT = sb23("s_sinT", [128, S], BF16); d_sin = Dep("sin")
    qkw = sb23("s_qkw", [128, 2], F32); d_qkw = Dep("qkw")
    onesblk = sb23("s_onesblk", [128, 128], BF16); d_ob = Dep("ob")
    rmat = sb23("s_rmat", [128, 128], BF16); d_rm = Dep("rm")
    sq = sb23("sq", [128, 512], BF16); d_sq = Dep("sq")
    rq = sb23("rq", [128, 512], F32); d_rq = Dep("rq")
    qn = sb23("qn", [128, 512], BF16); d_qn = Dep("qn")
    t1 = sb23("t1", [128, 512], F32); d_t1 = Dep("t1")
    t2 = sb23("t2", [128, 512], F32); d_t2 = Dep("t2")
    sq0, d_sq0, rq0, d_rq0, qn0, d_qn0, t10, d_t10, t20, d_t20 = sq, d_sq, rq, d_rq, qn, d_qn, t1, d_t1, t2, d_t2
    d_kT = [[Dep("kT%d_%d" % (m, j)) for j in range(NS)] for m in range(2)]
    d_V = [Dep("V%d" % t) for t in range(NT)]
    d_Vinit = Dep("Vinit")

    def normrope(qp_ap, d_qp, aux_ap, d_aux, wcol, j, dst, d_dst, alt=None):
        sq, d_sq, rq, d_rq, qn, d_qn, t1, d_t1, t2, d_t2 = alt if alt is not None else (sq0, d_sq0, rq0, d_rq0, qn0, d_qn0, t10, d_t10, t20, d_t20)
        act(sq[:, :], qp_ap, AF.Square, [d_qp], [d_sq])
        mm(aux_ap, onesblk[:, :], sq[:, :], True, True, [d_ob, d_sq], [d_aux])
        act(rq[:, :], aux_ap, AF.Sqrt, [d_aux, d_eps], [d_rq], bias=epsb[:, 0:1], scale=1.0 / 64)
        recip(rq[:, :], rq[:, :], [d_rq], [d_rq])
        stt(qn[:, :], qp_ap, qkw[:, wcol:wcol + 1], rq[:, :], ALU.mult, ALU.mult, [d_qp, d_rq, d_qkw], [d_qn])
        mm(aux_ap, rmat[:, :], qn[:, :], True, True, [d_rm, d_qn], [d_aux])
        js = slice(j * 512, (j + 1) * 512)
        tt("vector", t1[:, :], qn[:, :], cosT[:, js], ALU.mult, [d_qn, d_cos], [d_t1])
        tt("vector", t2[:, :], aux_ap, sinT[:, js], ALU.mult, [d_aux, d_sin], [d_t2])
        tt("gpsimd", dst, t1[:, :], t2[:, :], ALU.add, [d_t1, d_t2], [d_dst])

    with ExitStack() as ph:
        sb = lambda name, shape, dt: ph.enter_context(nc.sbuf_tensor(uq(name), list(shape), dt))
        ps = lambda name, shape, dt: ph.enter_context(nc.psum_tensor(uq(name), list(shape), dt))
        P.dma(qkw[:, :], qkw_d[:, :], writes=[d_qkw], semkey="c0")
        P.dma(onesblk[:, :], onesblk_d[:, :], writes=[d_ob], semkey="c1")
        P.dma(rmat[:, :], rmat_d[:, :], writes=[d_rm], semkey="c2")
        P.dma(cosT[:, :], cosT_d[:, :], writes=[d_cos], semkey="c3")
        P.dma(sinT[:, :], sinT_d[:, :], writes=[d_sin], semkey="c4")
        memset("gpsimd", Va[:, :, :, 64:128], 0.0, [d_Vinit])
        memset("gpsimd", Va[:, :, :, 64:65], 1.0, [d_Vinit])
        memset("gpsimd", Vb[:, :, :, 0:64], 0.0, [d_Vinit])
        memset("gpsimd", Vb[:, :, :, 0:1], 1.0, [d_Vinit])
        Wk = sb("Wk", [128, 8, 256], BF16); Wv = sb("Wv", [128, 8, 128], BF16); d_W = Dep("W2")
        stg = [sb("stg%d" % i, [128, 8, 256], F32) for i in range(2)]; d_stg = [Dep("stg%d" % i) for i in range(2)]
        for (dlo, slo) in [(0, 0), (64, 0), (128, 64), (192, 64)]:
            load_w(stg, d_stg, w_view[:, :, C_K + slo:C_K + slo + 64], 8, 64,
                   lambda c0, n, dlo=dlo: Wk[:, :, dlo + c0:dlo + c0 + n], d_W, nw[:, :], d_nw)
        load_w(stg, d_stg, w_view[:, :, C_V:C_V + 128], 8, 128, lambda c0, n: Wv[:, :, c0:c0 + n], d_W, nw[:, :], d_nw)
        hts = [sb("hts%d" % i, [128, 8, 512], BF16) for i in range(2)]; d_hts = [Dep("hts%d" % i) for i in range(2)]
        qp = [ps("qp%d" % i, [128, 512], F32) for i in range(2)]; d_qp = [Dep("qp%d" % i) for i in range(2)]
        aux = [ps("aux%d" % i, [128, 512], F32) for i in range(2)]; d_aux = [Dep("aux%d" % i) for i in range(2)]
        vp = [ps("vp%d" % i, [128, 128], F32) for i in range(2)]; d_vp = [Dep("vp%d" % i) for i in range(2)]
        alt1 = (sb("sq1", [128, 512], BF16), Dep("sq1"), sb("rq1", [128, 512], F32), Dep("rq1"), sb("qn1", [128, 512], BF16), Dep("qn1"),
                sb("t11", [128, 512], F32), Dep("t11"), sb("t21", [128, 512], F32), Dep("t21"))
        u = 0
        for j in range(NS):
            hb = j % 2
            P.dma(hts[hb][:, :, :], hT_d[j], writes=[d_hts[hb]], semkey=("hts", hb))
            for m in range(2):
                b = u % 2
                u += 1
                for kc in range(8):
                    mm(qp[b][:, :], Wk[:, kc, m * 128:(m + 1) * 128], hts[hb][:, kc, :], kc == 0, kc == 7,
                       [d_W, d_hts[hb]], [d_qp[b]])
                normrope(qp[b][:, :], d_qp[b], aux[b][:, :], d_aux[b], 1, j, kTd[:, m, j * 512:(j + 1) * 512], d_kT[m][j],
                         alt=(None if b == 0 else alt1))
            for i4 in range(4):
                t = j * 4 + i4
                vb = t % 2
                for kc in range(8):
                    mm(vp[vb][:, :], hts[hb][:, kc, i4 * 128:(i4 + 1) * 128], Wv[:, kc, :], kc == 0, kc == 7,
                       [d_W, d_hts[hb]], [d_vp[vb]])
                vv = vp[vb][:, :].rearrange("p (a b) -> p a b", a=2)
                cp("scalar", Va[:, t, :, 0:64], vv, [d_vp[vb], d_Vinit], [d_V[t]])
                cp("vector", Vb[:, t, :, 64:128], vv, [d_vp[vb], d_Vinit], [d_V[t]])
        P.barrier()
        P.emit()

    with ExitStack() as ph:
        sb = lambda name, shape, dt: ph.enter_context(nc.sbuf_tensor(uq(name), list(shape), dt))
        ps = lambda name, shape, dt: ph.enter_context(nc.psum_tensor(uq(name), list(shape), dt))
        ones_f = sb("ones_f", [128, 128], F32); d_of = Dep("ones_f")
        memset("gpsimd", ones_f[:, :], 1.0, [d_of])
        Wq = sb("Wq", [128, 8, 512], BF16); d_Wq = Dep("Wq")
        stg = [sb("stg%d" % i, [128, 8, 256], F32) for i in range(2)]; d_stg = [Dep("stg%d" % i) for i in range(2)]
        load_w(stg, d_stg, w_view[:, :, C_Q:C_Q + 512], 8, 512, lambda c0, n: Wq[:, :, c0:c0 + n], d_Wq, nw[:, :], d_nw)
        hts = [sb("hts%d" % i, [128, 8, 512], BF16) for i in range(2)]; d_hts = [Dep("hts%d" % i) for i in range(2)]
        qm = [sb("qm%d" % i, [128, 512], BF16) for i in range(2)]; d_qm = [Dep("qm%d" % i) for i in range(2)]
        sc = [ps("sc%d" % i, [128, 512], F32) for i in range(4)]; d_sc = [Dep("sc%d" % i) for i in range(4)]
        ov = [ps("ov%d" % i, [128, 512], F32) for i in range(2)]; d_ov = [Dep("ov%d" % i) for i in range(2)]
        aux = [ps("aux%d" % i, [128, 512], F32) for i in range(2)]; d_aux = [Dep("aux%d" % i) for i in range(2)]
        pT = [sb("pT%d" % i, [128, 512], BF16) for i in range(4)]; d_pT = [Dep("pT%d" % i) for i in range(4)]
        den = [sb("den%d" % i, [128, 512], F32) for i in range(2)]; d_den = [Dep("den%d" % i) for i in range(2)]
        rb = [sb("rb%d" % i, [128, 512], F32) for i in range(2)]; d_rb = [Dep("rb%d" % i) for i in range(2)]
        A_M = int(os.environ.get("ATT_M", "4")); A_J = int(os.environ.get("ATT_J", str(NS)))
        iters = [(m, j) for m in range(A_M) for j in range(A_J)]
        N_FILL = int(os.environ.get("ATT_FILL", "0"))

        ovs = [sb("ovs%d" % i, [128, 512], F32) for i in range(2)]; d_ovs = [Dep("ovs%d" % i) for i in range(2)]

        def q_stages(n):
            m, j = iters[n]
            hb = n % 2
            js = slice(j * 512, (j + 1) * 512)
            qp_ap, d_qp, aux_ap, d_ax = aux[0][:, :], d_aux[0], aux[1][:, :], d_aux[1]
            P.dma(hts[hb][:, :, :], hT_d[j], writes=[d_hts[hb]], semkey=("hts", hb))
            yield
            for kc in range(8):
                mm(qp_ap, Wq[:, kc, m * 128:(m + 1) * 128], hts[hb][:, kc, :], kc == 0, kc == 7, [d_Wq, d_hts[hb]], [d_qp])
            yield
            act(sq[:, :], qp_ap, AF.Square, [d_qp], [d_sq])
            yield
            mm(aux_ap, onesblk[:, :], sq[:, :], True, True, [d_ob, d_sq], [d_ax])
            yield
            act(rq[:, :], aux_ap, AF.Sqrt, [d_ax, d_eps], [d_rq], bias=epsb[:, 0:1], scale=1.0 / 64)
            yield
            recip(rq[:, :], rq[:, :], [d_rq], [d_rq])
            yield
            stt(qn[:, :], qp_ap, qkw[:, 0:1], rq[:, :], ALU.mult, ALU.mult, [d_qp, d_rq, d_qkw], [d_qn])
            yield
            mm(aux_ap, rmat[:, :], qn[:, :], True, True, [d_rm, d_qn], [d_ax])
            yield
            tt("vector", t1[:, :], qn[:, :], cosT[:, js], ALU.mult, [d_qn, d_cos], [d_t1])
            yield
            tt("vector", t2[:, :], aux_ap, sinT[:, js], ALU.mult, [d_ax, d_sin], [d_t2])
            yield
            tt("gpsimd", qm[n % 2][:, :], t1[:, :], t2[:, :], ALU.add, [d_t1, d_t2], [d_qm[n % 2]])
            yield

        def norm_stages(n):
            m, j = iters[n]
            qs = slice(j * 512, (j + 1) * 512)
            for hh in range(2):
                dr = 64 if hh == 0 else 0
                lo = 0 if hh == 0 else 64
                if hh == 0:
                    mm(aux[hh][0:64, :], ones_f[dr:dr + 1, 0:64], ovs[hh][dr:dr + 1, :], True, True, [d_of, d_ovs[hh]], [d_aux[hh]])
                else:
                    mm(aux[hh][:, :], ones_f[dr:dr + 1, :], ovs[hh][dr:dr + 1, :], True, True, [d_of, d_ovs[hh]], [d_aux[hh]])
                yield
                recip(rb[hh][lo:lo + 64, :], aux[hh][lo:lo + 64, :], [d_aux[hh]], [d_rb[hh]])
                yield
                tt("vector", attnT[lo:lo + 64, m, qs], ovs[hh][lo:lo + 64, :], rb[hh][lo:lo + 64, :], ALU.mult,
                   [d_ovs[hh], d_rb[hh]], [d_attn])
                yield

        def drain(gen):
            for _ in gen:
                pass

        drain(q_stages(0))

        def scores(n, kt):
            m, j = iters[n]
            qb = n % 2
            it = n * NT + kt
            ks = slice(kt * 128, (kt + 1) * 128)
            for hh in range(2):
                kv = (2 * m + hh) // 4
                off = hh * 64
                sbk = (it % 2) * 2 + hh
                mm(sc[sbk][:, :], kTd[off:off + 64, kv, ks], qm[qb][off:off + 64, :], True, True,
                   [d_kT[kv][kt // 4], d_qm[qb]], [d_sc[sbk]])
                act(pT[sbk][:, :], sc[sbk][:, :], AF.Exp, [d_sc[sbk]], [d_pT[sbk]], scale=0.125)

        def pv(n, kt):
            m, j = iters[n]
            it = n * NT + kt
            for hh in range(2):
                kv = (2 * m + hh) // 4
                sbk = (it % 2) * 2 + hh
                if hh == 0:
                    mm(ov[0][0:65, :], Va[:, kt, kv, 0:65], pT[sbk][:, :], kt == 0, kt == NT - 1,
                       [d_V[kt], d_pT[sbk]], [d_ov[0]])
                else:
                    mm(ov[1][:, :], Vb[:, kt, kv, :], pT[sbk][:, :], kt == 0, kt == NT - 1,
                       [d_V[kt], d_pT[sbk]], [d_ov[1]])

        for n, (m, j) in enumerate(iters):
            gens = []
            if n >= 1:
                gens.append(norm_stages(n - 1))
            if n + 1 < len(iters):
                gens.append(q_stages(n + 1))
            scores(n, 0)
            for kt in range(NT):
                if kt + 1 < NT:
                    scores(n, kt + 1)
                pv(n, kt)
                while gens:
                    try:
                        next(gens[0])
                        break
                    except StopIteration:
                        gens.pop(0)
            for g_ in gens:
                drain(g_)
            cp("vector", ovs[0][0:65, :], ov[0][0:65, :], [d_ov[0]], [d_ovs[0]])
            cp("vector", ovs[1][:, :], ov[1][:, :], [d_ov[1]], [d_ovs[1]])
        drain(norm_stages(len(iters) - 1))
        if "attnT" in dbg:
            da = dbgout("attnT", [128, 4, S], BF16)
            P.dma(da, attnT[:, :, :], reads=[d_attn], semkey="d0")
        P.barrier()
        P.emit()
    es23.close()
    if stop_after == 3:
        es35.close()
        return finish()


    with ExitStack() as ph:
        sb = lambda name, shape, dt: ph.enter_context(nc.sbuf_tensor(uq(name), list(shape), dt))
        ps = lambda name, shape, dt: ph.enter_context(nc.psum_tensor(uq(name), list(shape), dt))
        nssd = sb("s_nssd", [128, 4], F32); d_nssd = Dep("nssd")
        bg = sb("s_bg", [128, 16], F32); d_bg = Dep("bg")
        P.dma(nssd[:, :], nssd_d[:, :], writes=[d_nssd], semkey="c0")
        P.dma(bg[:, :], bg_d[:, :], writes=[d_bg], semkey="c1")
        Wg = sb("Wg", [128, 8, 2048], BF16); Wao = sb("Wao", [128, 4, 1024], BF16)
        Wso = sb("Wso", [128, 4, 1024], BF16); Wout = sb("Wout", [128, 8, 1024], BF16)
        d_W = Dep("W5")
        stg = [sb("stg%d" % i, [128, 8, 128], F32) for i in range(2)]; d_stg = [Dep("stg%d" % i) for i in range(2)]
        load_w(stg, d_stg, w_view[:, :, C_G:C_G + 2048], 8, 2048, lambda c0, n: Wg[:, :, c0:c0 + n], d_W, nw[:, :], d_nw, piece=128)
        load_w(stg, d_stg, w_ao_d.rearrange("(c p) n -> p c n", p=128), 4, 1024, lambda c0, n: Wao[:, :, c0:c0 + n], d_W, piece=128)
        load_w(stg, d_stg, w_so_d.rearrange("(c p) n -> p c n", p=128), 4, 1024, lambda c0, n: Wso[:, :, c0:c0 + n], d_W,
               nssd[:, :], d_nssd, piece=128)
        load_w(stg, d_stg, w_out_d.rearrange("(c p) n -> p c n", p=128), 8, 1024, lambda c0, n: Wout[:, :, c0:c0 + n], d_W, piece=128)
        hts = [sb("hts%d" % i, [128, 8, 512], BF16) for i in range(2)]; d_hts = [Dep("hts%d" % i) for i in range(2)]
        mg = [sb("mg%d" % i, [128, 8, 512], BF16) for i in range(2)]; d_mg = [Dep("mg%d" % i) for i in range(2)]
        xr = [sb("xr%d" % i, [128, D], F32) for i in range(2)]; d_xr = [Dep("xr%d" % i) for i in range(2)]
        sgA = [sb("sgA%d" % i, [128, 512], F32) for i in range(2)]; d_sgA = [Dep("sgA%d" % i) for i in range(2)]
        sgS = [sb("sgS%d" % i, [128, 512], F32) for i in range(2)]; d_sgS = [Dep("sgS%d" % i) for i in range(2)]
        m1 = sb("m1", [128, 512], F32); d_m1 = Dep("m1")
        m2 = sb("m2", [128, 512], F32); d_m2 = Dep("m2")
        pgA = [ps("pgA%d" % i, [128, 512], F32) for i in range(2)]; d_pgA = [Dep("pgA%d" % i) for i in range(2)]
        pgS = [ps("pgS%d" % i, [128, 512], F32) for i in range(2)]; d_pgS = [Dep("pgS%d" % i) for i in range(2)]
        pA = ps("pA", [128, 512], F32); d_pA = Dep("pA")
        pS = ps("pS", [128, 512], F32); d_pS = Dep("pS")
        po = [ps("po%d" % i, [128, 512], F32) for i in range(2)]; d_po = [Dep("po%d" % i) for i in range(2)]
        u = 0
        for j in range(NS):
            hb = j % 2
            js = slice(j * 512, (j + 1) * 512)
            P.dma(hts[hb][:, :, :], hT_d[j], writes=[d_hts[hb]], semkey=("hts", hb))
            def gates(mo):
                b = mo % 2
                for kc in range(8):
                    mm(pgA[b][:, :], Wg[:, kc, mo * 128:(mo + 1) * 128], hts[hb][:, kc, :], kc == 0, kc == 7,
                       [d_W, d_hts[hb]], [d_pgA[b]])
                for kc in range(8):
                    mm(pgS[b][:, :], Wg[:, kc, 1024 + mo * 128:1024 + (mo + 1) * 128], hts[hb][:, kc, :], kc == 0, kc == 7,
                       [d_W, d_hts[hb]], [d_pgS[b]])
                act(sgA[b][:, :], pgA[b][:, :], AF.Sigmoid, [d_pgA[b], d_bg], [d_sgA[b]], bias=bg[:, mo:mo + 1])
                act(sgS[b][:, :], pgS[b][:, :], AF.Sigmoid, [d_pgS[b], d_bg], [d_sgS[b]], bias=bg[:, 8 + mo:9 + mo])

            gates(0)
            for mo in range(8):
                b = mo % 2
                if mo + 1 < 8:
                    gates(mo + 1)
                for c in range(4):
                    mm(pA[:, :], Wao[:, c, mo * 128:(mo + 1) * 128], attnT[:, c, js], c == 0, c == 3, [d_W, d_attn], [d_pA])
                for c in range(4):
                    mm(pS[:, :], Wso[:, c, mo * 128:(mo + 1) * 128], ssdT[:, c, js], c == 0, c == 3, [d_W, d_ssdT], [d_pS])
                tt("vector", m1[:, :], sgA[b][:, :], pA[:, :], ALU.mult, [d_sgA[b], d_pA], [d_m1])
                tt("vector", m2[:, :], sgS[b][:, :], pS[:, :], ALU.mult, [d_sgS[b], d_pS], [d_m2])
                tt("gpsimd", mg[hb][:, mo, :], m1[:, :], m2[:, :], ALU.add, [d_m1, d_m2], [d_mg[hb]])
            for i4 in range(4):
                t = j * 4 + i4
                xb = t % 2
                P.dma(xr[xb][:, :], x_d[t * 128:(t + 1) * 128, :], writes=[d_xr[xb]], semkey=("xr", xb))
                for nh in range(2):
                    for kc in range(8):
                        mm(po[nh][:, :], mg[hb][:, kc, i4 * 128:(i4 + 1) * 128], Wout[:, kc, nh * 512:(nh + 1) * 512],
                           kc == 0, kc == 7, [d_W, d_mg[hb]], [d_po[nh]])
                    tt("vector", xr[xb][:, nh * 512:(nh + 1) * 512], xr[xb][:, nh * 512:(nh + 1) * 512], po[nh][:, :], ALU.add,
                       [d_xr[xb], d_po[nh]], [d_xr[xb]])
                P.dma(x1_d[t * 128:(t + 1) * 128, :], xr[xb][:, :], reads=[d_xr[xb]], semkey=("x1s", xb))
                if "x1" in dbg:
                    if t == 0:
                        dbg_x1 = dbgout("x1", [S, D], F32)
                    P.dma(dbg_x1[t * 128:(t + 1) * 128, :], xr[xb][:, :], reads=[d_xr[xb]], semkey=("x1d", xb))
        P.barrier()
        P.emit()
    es35.close()
    es_ssd.close()
    if stop_after == 5:
        return finish()

    NSLOT = 11
    RW = 1032
    sorted_d = dscr("sorted_h2", [NSLOT * 512, RW], BF16)
    sout_d = dscr("sorted_out", [NSLOT * 512, D], F32)
    nfbc_d = din("nf_bc", [128, D])
    sconst_d = din("sconst", [128, 32])

    def dma_fn(eng, fn, reads, writes, semkey, grp=None):
        return P._rec(eng, fn, list(reads), list(writes), is_dma=True, semkey=semkey, grp=grp)

    es6 = ExitStack()
    sb6 = lambda name, shape, dt: es6.enter_context(nc.sbuf_tensor(uq(name), list(shape), dt))
    widx1 = sb6("widx1", [128, NSLOT, 8], mybir.dt.int32)
    d_widx = Dep("widx")
    wc_d = din("wconst", [128, 48])
    pos_i = sb6("pos_i", [128, NT], mybir.dt.int32); d_pos = Dep("pos_i")
    gs_i = sb6("gs_i", [128, 16], mybir.dt.int32); d_gs = Dep("gs_i")
    d_sorted = Dep("sorted_d")

    with ExitStack() as ph:
        sb = lambda name, shape, dt: ph.enter_context(nc.sbuf_tensor(uq(name), list(shape), dt))
        ps = lambda name, shape, dt: ph.enter_context(nc.psum_tensor(uq(name), list(shape), dt))
        nfbc = sb("s_nfbc", [128, D], F32); d_nfbc = Dep("nfbc")
        brt = sb("s_br", [128, 20], F32); d_br = Dep("br")
        sconst = sb("s_sconst", [128, 32], F32); d_sc0 = Dep("sconst")
        masks = sb("s_masks6", [128, 4, 128], F32); d_mk = Dep("masks6")
        Wr = sb("Wr", [128, 8, 20], BF16); d_Wr = Dep("Wr")
        P.dma(nfbc[:, :], nfbc_d[:, :], writes=[d_nfbc], semkey="c0")
        P.dma(brt[:, :], br_d[:, :], writes=[d_br], semkey="c1")
        P.dma(sconst[:, :], sconst_d[:, :], writes=[d_sc0], semkey="c2")
        P.dma(masks[:, :, :], masks_d[:, :, :], writes=[d_mk], semkey="c3")
        P.dma(Wr[:, :, :], wr_d.rearrange("(kc p) n -> p kc n", p=128), writes=[d_Wr], semkey="wr_cast", eng="gpsimd")
        ones_f = sb("ones_f6", [128, 128], F32); d_of = Dep("ones_f6")
        memset("gpsimd", ones_f[:, :], 1.0, [d_of])
        xn_all = sb("xn_all", [128, NT, RW], BF16); d_xn = [Dep("xn_all%d" % t) for t in range(NT)]
        oh_all = sb("oh_all", [128, NT, 4], F32); d_oh = Dep("oh_all")
        x1t = [sb("x1t%d" % i, [128, D], F32) for i in range(3)]; d_x1t = [Dep("x1t%d" % i) for i in range(3)]
        junk = sb("junk6", [128, D], BF16); d_junk = Dep("junk6")
        h2t = [sb("h2t%d" % i, [128, 8, 128], BF16) for i in range(2)]; d_h2t = [Dep("h2t%d" % i) for i in range(2)]
        rt = sb("rt", [128, 96], F32); d_rt = Dep("rt")
        rt2 = sb("rt2", [128, NT, 2], F32); d_rt2 = [Dep("rt2_%d" % i) for i in range(NT)]
        Lall = sb("Lall", [128, NT, 20], F32); d_L = Dep("Lall"); d_rz = Dep("rz")
        gmax = sb("gmax", [128, NT], F32); gsum = sb("gsum", [128, NT], F32); m1c = sb("m1c", [128, NT], F32)
        m2c = sb("m2c", [128, NT], F32); esum = sb("esum", [128, NT], F32)
        eg = sb("eg", [128, NT, 4], F32); fs = sb("fs", [128, NT, 4], F32); fs2 = sb("fs2", [128, NT, 4], F32)
        mk1 = sb("mk1", [128, NT, 4], F32); mk2 = sb("mk2", [128, NT, 4], F32); ef = sb("ef", [128, NT, 4], F32)
        tmp16 = sb("tmp16", [128, NT, 4, 4], F32)
        tpp = [ps("tpp%d" % i, [128, 8, 128], BF16) for i in range(2)]; d_tpp = [Dep("tpp%d" % i) for i in range(2)]
        plg = [ps("plg%d" % i, [128, 32], F32) for i in range(2)]; d_plg = [Dep("plg%d" % i) for i in range(2)]
        def p6_stage1(t):
            xb = t % 3
            P.dma(x1t[xb][:, :], x1_d[t * 128:(t + 1) * 128, :], writes=[d_x1t[xb]], semkey=("x1t", xb))
            R = [d_rt2[t]]
            act(junk[:, :], x1t[xb][:, :], AF.Square, [d_x1t[xb]], [d_junk] + R, accum_out=rt2[:, t, 0:1])
            act(rt2[:, t, 1:2], rt2[:, t, 0:1], AF.Sqrt, R + [d_eps], R, bias=epsb[:, 0:1], scale=1.0 / D)
            recip(rt2[:, t, 1:2], rt2[:, t, 1:2], R, R)
            stt(xn_all[:, t, 0:D], x1t[xb][:, :], rt2[:, t, 1:2], nfbc[:, :], ALU.mult, ALU.mult, [d_x1t[xb], d_nfbc] + R, [d_xn[t]])

        def p6_stage2(t):
            nb = t % 2
            for c in range(8):
                tr(tpp[nb][:, c, :], xn_all[:, t, c * 128:(c + 1) * 128], ident[:, :], [d_xn[t], d_ident], [d_tpp[nb]])
            cp("scalar", h2t[nb][:, :, :], tpp[nb][:, :, :], [d_tpp[nb]], [d_h2t[nb]])
            for kc in range(8):
                mm(plg[nb][:, 0:20], h2t[nb][:, kc, :], Wr[:, kc, :], kc == 0, kc == 7, [d_h2t[nb], d_Wr], [d_plg[nb]])
            tt("vector", Lall[:, t, :], plg[nb][:, 0:20], brt[:, :], ALU.add, [d_plg[nb], d_br], [d_L])

        p6_stage1(0)
        for t in range(NT):
            if t + 1 < NT:
                p6_stage1(t + 1)
            p6_stage2(t)
        B3 = lambda ap, n: ap.unsqueeze(2).to_broadcast([128, NT, n])
        Lg = Lall[:, :, 0:4]
        Fv = Lall[:, :, 4:20].rearrange("p t (g k) -> p t g k", g=4)
        Z = [d_rz]
        red(gmax[:, :], Lg, ALU.max, [d_L], Z)
        tt("vector", eg[:, :, :], Lg, B3(gmax[:, :], 4), ALU.subtract, [d_L] + Z, Z)
        ts("vector", oh_all[:, :, :], eg[:, :, :], 0.0, None, ALU.is_equal, None, Z, [d_oh])
        act(eg[:, :, :], eg[:, :, :], AF.Exp, Z, Z)
        red(gsum[:, :], eg[:, :, :], ALU.add, Z, Z)
        tt("vector", tmp16[:, :, :, :], Fv, oh_all[:, :, :].unsqueeze(3).to_broadcast([128, NT, 4, 4]), ALU.mult, [d_L, d_oh] + Z, Z)
        red(fs[:, :, :], tmp16[:, :, :, :].rearrange("p t g k -> p t k g"), ALU.add, Z, Z)
        red(m1c[:, :], fs[:, :, :], ALU.max, Z, Z)
        tt("vector", fs[:, :, :], fs[:, :, :], B3(m1c[:, :], 4), ALU.subtract, Z, Z)
        ts("vector", mk1[:, :, :], fs[:, :, :], 0.0, None, ALU.is_equal, None, Z, Z)
        stt(fs2[:, :, :], mk1[:, :, :], -1.0e30, fs[:, :, :], ALU.mult, ALU.add, Z, Z)
        red(m2c[:, :], fs2[:, :, :], ALU.max, Z, Z)
        tt("vector", mk2[:, :, :], fs2[:, :, :], B3(m2c[:, :], 4), ALU.is_equal, Z, Z)
        tt("vector", mk1[:, :, :], mk1[:, :, :], mk2[:, :, :], ALU.add, Z, Z)
        act(ef[:, :, :], fs[:, :, :], AF.Exp, Z, Z)
        tt("vector", ef[:, :, :], ef[:, :, :], mk1[:, :, :], ALU.mult, Z, Z)
        red(esum[:, :], ef[:, :, :], ALU.add, Z, Z)
        tt("vector", esum[:, :], esum[:, :], gsum[:, :], ALU.mult, Z, Z)
        recip(esum[:, :], esum[:, :], Z, Z)
        tt("vector", xn_all[:, :, D:RW].bitcast(F32), ef[:, :, :], B3(esum[:, :], 4), ALU.mult, Z, d_xn)
        pcw = ps("pcw", [128, 128], F32); d_pcw = Dep("pcw")
        ptot = ps("ptot", [128, 128], F32); d_ptot = Dep("ptot")
        ohf = oh_all[:, :, :].rearrange("p t g -> p (t g)")
        mm(pcw[:, :], masks[:, 0, :], ohf, True, True, [d_mk, d_oh], [d_pcw])
        mm(ptot[:, :], ones_f[:, :], ohf, True, True, [d_of, d_oh], [d_ptot])
        tot = sb("tot", [128, NT, 4], F32); pre = sb("pre", [128, NT, 4], F32); Aa = sb("Aa", [128, NT, 4], F32)
        sm6 = sb("sm6", [128, 64], F32)
        d_q = Dep("posq")
        Q = [d_q]
        cp("vector", tot[:, :, :], ptot[:, :].rearrange("p (t g) -> p t g", g=4), [d_ptot], Q)
        memset("vector", pre[:, 0, :], 0.0, Q)
        for t in range(1, NT):
            tt("vector", pre[:, t, :], pre[:, t - 1, :], tot[:, t - 1, :], ALU.add, Q, Q)
        ng = sm6[:, 0:4]; cnt = sm6[:, 4:8]; pn = sm6[:, 8:12]; st = sm6[:, 12:16]; en = sm6[:, 16:20]; stm1 = sm6[:, 20:24]
        cmp8 = sm6[:, 24:32]; posf = sb("posf", [128, NT], F32); gsf = sm6[:, 32:48]; cmp11 = sm6[:, 48:64]
        tt("vector", ng, pre[:, NT - 1, :], tot[:, NT - 1, :], ALU.add, Q, Q)
        for g in range(4):
            ts("vector", cmp8, sconst[:, 16:24], ng[:, g:g + 1], None, ALU.is_lt, None, Q + [d_sc0], Q)
            red(cnt[:, g:g + 1], cmp8, ALU.add, Q, Q)
        ts("vector", pn, cnt, 512.0, None, ALU.mult, None, Q, Q)
        memset("vector", st[:, 0:1], 0.0, Q)
        for g in range(1, 4):
            tt("vector", st[:, g:g + 1], st[:, g - 1:g], pn[:, g - 1:g], ALU.add, Q, Q)
        tt("vector", en, st, pn, ALU.add, Q, Q)
        ts("vector", stm1, st, -1.0, None, ALU.add, None, Q, Q)
        tt("vector", Aa[:, :, :], pcw[:, :].rearrange("p (t g) -> p t g", g=4), pre[:, :, :], ALU.add, Q + [d_pcw], Q)
        tt("vector", Aa[:, :, :], Aa[:, :, :], stm1.unsqueeze(1).to_broadcast([128, NT, 4]), ALU.add, Q, Q)
        tt("vector", Aa[:, :, :], Aa[:, :, :], oh_all[:, :, :], ALU.mult, Q + [d_oh], Q)
        red(posf[:, :], Aa[:, :, :], ALU.add, Q, Q)
        cp("vector", pos_i[:, :], posf[:, :], Q, [d_pos])
        memset("vector", gsf, 0.0, Q)
        for g in range(3):
            ts("vector", cmp11, sconst[:, 0:16], en[:, g:g + 1], None, ALU.is_ge, None, Q + [d_sc0], Q)
            tt("vector", gsf, gsf, cmp11, ALU.add, Q, Q)
        cp("vector", gs_i[:, :], gsf, Q, [d_gs])
        wcst = sb("s_wconst", [128, 48], F32); d_wc = Dep("wconst")
        P.dma(wcst[:, :], wc_d[:, :], writes=[d_wc], semkey="c5")
        wf1 = sb("wf1", [128, NSLOT, 8], F32)
        g1 = sm6[:, 48:64]
        ts("vector", g1, gsf, 1024.0, None, ALU.mult, None, Q, Q)
        for s in range(NSLOT):
            ts("vector", wf1[:, s, :], wcst[:, 0:8], g1[:, s:s + 1], None, ALU.add, None, Q + [d_wc], Q)
        same = sb("same6", [128, 16], F32)
        memset("vector", same[:, 0:1], 0.0, Q)
        tt("vector", same[:, 1:NSLOT], gsf[:, 1:NSLOT], gsf[:, 0:NSLOT - 1], ALU.is_equal, Q, Q)
        ts("vector", same[:, 0:NSLOT], same[:, 0:NSLOT], 8192.0, None, ALU.mult, None, Q, Q)
        tt("vector", wf1[:, :, :], wf1[:, :, :], same[:, 0:NSLOT].unsqueeze(2).to_broadcast([128, NSLOT, 8]), ALU.add, Q, Q)
        cp("vector", widx1[:, :, :], wf1[:, :, :], Q, [d_widx])
        for t in range(NT):
            dma_fn("gpsimd", lambda e, t=t: e.indirect_dma_start(
                out=sorted_d[:, :], out_offset=bass.IndirectOffsetOnAxis(ap=pos_i[:, t:t + 1], axis=0),
                in_=xn_all[:, t, :], in_offset=None, bounds_check=None, oob_is_err=False),
                [d_xn[t], d_pos], [d_sorted], ("scat", t % 4))
        if "sort" in dbg:
            o1 = dbgout("pos", [128, NT], mybir.dt.int32); o2 = dbgout("gs", [128, 16], mybir.dt.int32)
            o3 = dbgout("xn_all", [128, NT, RW], BF16)
            P.dma(o1, pos_i[:, :], reads=[d_pos], semkey="d0")
            P.dma(o2, gs_i[:, :], reads=[d_gs], semkey="d1")
            P.dma(o3, xn_all[:, :, :], reads=d_xn, semkey="d2")
        P.barrier()
        P.emit()
    if stop_after == 61:
        es6.close()
        return finish()

    with ExitStack() as ph:
        sb = lambda name, shape, dt: ph.enter_context(nc.sbuf_tensor(uq(name), list(shape), dt))
        ps = lambda name, shape, dt: ph.enter_context(nc.psum_tensor(uq(name), list(shape), dt))
        NWB = 4
        W1s = [sb("W1s%d" % i, [128, 8, DE], BF16) for i in range(NWB)]
        W3s = [sb("W3s%d" % i, [128, 8, DE], BF16) for i in range(NWB)]
        W2s = [sb("W2s%d" % i, [128, 4, D], BF16) for i in range(NWB)]
        d_W1 = [Dep("W1s%d" % i) for i in range(NWB)]; d_W3 = [Dep("W3s%d" % i) for i in range(NWB)]
        d_W2 = [Dep("W2s%d" % i) for i in range(NWB)]
        xs = [sb("xs%d" % i, [128, 4, RW], BF16) for i in range(2)]; d_xs6 = [Dep("xs6_%d" % i) for i in range(2)]
        h2s = [sb("h2s%d" % i, [128, 8, 512], BF16) for i in range(2)]; d_h2s = [Dep("h2s%d" % i) for i in range(2)]
        gT = [sb("gT%d" % i, [128, 4, 512], BF16) for i in range(2)]; d_gT = [Dep("gT%d" % i) for i in range(2)]
        s1 = [sb("s1_%d" % i, [128, 512], F32) for i in range(2)]; d_s1 = [Dep("s1_%d" % i) for i in range(2)]
        yacc = [sb("yacc%d" % i, [128, 4, D], F32) for i in range(2)]; d_ya6 = [Dep("yacc%d" % i) for i in range(2)]
        tpp = ps("tpp6", [128, 8, 128], BF16); d_tpp = Dep("tpp6")
        ph1 = [ps("ph1_%d" % i, [128, 512], F32) for i in range(2)]; d_ph1 = [Dep("ph1_%d" % i) for i in range(2)]
        ph3 = [ps("ph3_%d" % i, [128, 512], F32) for i in range(2)]; d_ph3 = [Dep("ph3_%d" % i) for i in range(2)]
        py = [ps("py%d" % i, [128, 512], F32) for i in range(2)]; d_py = [Dep("py%d" % i) for i in range(2)]
        d_sout = Dep("sout")

        grp_ctr = [0]
        bnd_cache = {}

        def wload(s, ee, wb):
            for (rows, dst, nchunk, ncol, dd, key) in ((w1_d, W1s[wb], 8, DE, d_W1[wb], "w1"),
                                                       (w3_d, W3s[wb], 8, DE, d_W3[wb], "w3"),
                                                       (w2_d, W2s[wb], 4, D, d_W2[wb], "w2")):
                grp_ctr[0] += 1
                gid = grp_ctr[0]
                hc = nchunk // 2
                for hf in range(2):
                    def fn(e, rows=rows, dst=dst, hf=hf, hc=hc, ncol=ncol):
                        if "bval" not in bnd_cache:
                            rg = e.alloc_register("wbound")
                            e.reg_mov(rg, NE * 128 * 2 - 1)
                            bnd_cache["bval"] = e.snap(rg)
                        return e.indirect_dma_start(
                            out=dst[:, hf * hc:(hf + 1) * hc, :].rearrange("p c n -> p (c n)"), out_offset=None,
                            in_=rows[:, :],
                            in_offset=bass.IndirectOffsetOnAxis(ap=widx1[:, s, ee * 2 + hf:ee * 2 + hf + 1], axis=0),
                            bounds_check=bnd_cache["bval"], oob_is_err=False)
                    dma_fn("gpsimd", fn, [d_widx], [dd], (key, wb), grp=gid)

        NSL = int(os.environ.get("MOE_NSLOT", str(NSLOT)))

        def slot_rows(s2):
            for i4 in range(4):
                r0 = (s2 * 4 + i4) * 128
                P.dma(xs[s2 % 2][:, i4, :], sorted_d[r0:r0 + 128, :], reads=[d_sorted], writes=[d_xs6[s2 % 2]], semkey=("xs6", i4))

        def slot_tr(s2, i4):
            for c in range(8):
                tr(tpp[:, c, :], xs[s2 % 2][:, i4, c * 128:(c + 1) * 128], ident[:, :], [d_xs6[s2 % 2], d_ident], [d_tpp])
            cp("scalar", h2s[s2 % 2][:, :, i4 * 128:(i4 + 1) * 128], tpp[:, :, :], [d_tpp], [d_h2s[s2 % 2]])

        work = [(s, ee) for s in range(NSL) for ee in range(4)]
        for ee0 in range(4):
            wload(0, ee0, ee0)
        uu = 0
        for n, (s, ee) in enumerate(work):
            if n >= 1:
                ps_, pe_ = work[n - 1]
                if ps_ + 1 < NSL:
                    wload(ps_ + 1, pe_, pe_)
            wb = ee
            sb_ = s % 2
            if n == 0:
                slot_rows(0)
                for i4 in range(4):
                    slot_tr(0, i4)
            if s + 1 < NSL:
                if ee == 0:
                    slot_rows(s + 1)
                else:
                    slot_tr(s + 1, ee - 1)
                    if ee == 3:
                        slot_tr(s + 1, 3)
            gb = n % 2
            for mc in range(4):
                b = uu % 2
                uu += 1
                for kc in range(8):
                    mm(ph1[b][:, :], W1s[wb][:, kc, mc * 128:(mc + 1) * 128], h2s[sb_][:, kc, :], kc == 0, kc == 7,
                       [d_W1[wb], d_h2s[sb_]], [d_ph1[b]])
                for kc in range(8):
                    mm(ph3[b][:, :], W3s[wb][:, kc, mc * 128:(mc + 1) * 128], h2s[sb_][:, kc, :], kc == 0, kc == 7,
                       [d_W3[wb], d_h2s[sb_]], [d_ph3[b]])
                act(s1[b][:, :], ph1[b][:, :], AF.Silu, [d_ph1[b]], [d_s1[b]])
                tt("vector", gT[gb][:, mc, :], s1[b][:, :], ph3[b][:, :], ALU.mult, [d_s1[b], d_ph3[b]], [d_gT[gb]])
            for i4 in range(4):
                wcol = xs[sb_][:, i4, D:RW].bitcast(F32)[:, ee:ee + 1]
                for nh in range(2):
                    for mc in range(4):
                        mm(py[nh][:, :], gT[gb][:, mc, i4 * 128:(i4 + 1) * 128], W2s[wb][:, mc, nh * 512:(nh + 1) * 512],
                           mc == 0, mc == 3, [d_gT[gb], d_W2[wb]], [d_py[nh]])
                    ysl = yacc[sb_][:, i4, nh * 512:(nh + 1) * 512]
                    if ee == 0:
                        ts("vector", ysl, py[nh][:, :], wcol, None, ALU.mult, None, [d_py[nh], d_xs6[sb_]], [d_ya6[sb_]])
                    else:
                        stt(ysl, py[nh][:, :], wcol, ysl, ALU.mult, ALU.add, [d_py[nh], d_xs6[sb_], d_ya6[sb_]], [d_ya6[sb_]])
            if ee == 3:
                for i4 in range(4):
                    r0 = (s * 4 + i4) * 128
                    P.dma(sout_d[r0:r0 + 128, :], yacc[sb_][:, i4, :], reads=[d_ya6[sb_]], writes=[d_sout], semkey=("ys6", i4))
        P.barrier()
        P.emit()

    with ExitStack() as ph:
        sb = lambda name, shape, dt: ph.enter_context(nc.sbuf_tensor(uq(name), list(shape), dt))
        NB6 = 6
        yt = [sb("yt%d" % i, [128, D], F32) for i in range(NB6)]; d_yt = [Dep("yt%d" % i) for i in range(NB6)]
        x1r = [sb("x1r%d" % i, [128, D], F32) for i in range(NB6)]; d_x1r = [Dep("x1r%d" % i) for i in range(NB6)]
        for t in range(NT):
            b = t % NB6
            dma_fn("gpsimd", lambda e, t=t, b=b: e.indirect_dma_start(
                out=yt[b][:, :], out_offset=None, in_=sout_d[:, :],
                in_offset=bass.IndirectOffsetOnAxis(ap=pos_i[:, t:t + 1], axis=0),
                bounds_check=None, oob_is_err=False), [d_pos], [d_yt[b]], ("gat", b))
            P.dma(x1r[b][:, :], x1_d[t * 128:(t + 1) * 128, :], writes=[d_x1r[b]], semkey=("x1r", b))
            tt("vector", x1r[b][:, :], x1r[b][:, :], yt[b][:, :], ALU.add, [d_x1r[b], d_yt[b]], [d_x1r[b]])
            P.dma(out_d[t * 128:(t + 1) * 128, :], x1r[b][:, :], reads=[d_x1r[b]], semkey=("outs", b), eng="scalar")
        P.barrier()
        P.emit()
    es6.close()

    return finish()


_CACHE = {}


def _consts():
    bf = ml_dtypes.bfloat16
    c = {}
    c["ident_bf"] = np.eye(128, dtype=np.float32).astype(bf)
    ob = np.zeros((128, 128), np.float32)
    ob[:64, :64] = 1.0
    ob[64:, 64:] = 1.0
    c["onesblk"] = ob.astype(bf)
    rm = np.zeros((128, 128), np.float32)
    for blk in range(4):
        base = blk * 32
        for i in range(16):
            rm[base + i + 16, base + i] = -1.0
            rm[base + i, base + i + 16] = 1.0
    c["rmat"] = rm.astype(bf)
    t = np.arange(S)
    row = (t // 64).astype(np.float32)
    col = (t % 64).astype(np.float32)
    inv = (np.float32(10000.0) ** (-(np.arange(0, 32, 2, dtype=np.float32)) / np.float32(32))).astype(np.float32)
    ar = row[:, None] * inv[None, :]
    ac = col[:, None] * inv[None, :]
    cos = np.concatenate([np.cos(ar), np.cos(ar), np.cos(ac), np.cos(ac)], -1).astype(np.float32)
    sin = np.concatenate([np.sin(ar), np.sin(ar), np.sin(ac), np.sin(ac)], -1).astype(np.float32)
    c["cosT"] = np.ascontiguousarray(np.concatenate([cos.T, cos.T], 0)).astype(bf)
    c["sinT"] = np.ascontiguousarray(np.concatenate([sin.T, sin.T], 0)).astype(bf)
    r = np.arange(128)[:, None]
    q = np.arange(128)[None, :]
    mk = np.stack([(r <= q) + 0 * q, (r > q) + 0 * q, (r >= q) + 0 * q, (r < q) + 0 * q], axis=1).astype(np.float32)
    c["masks"] = np.ascontiguousarray(mk)
    sc = np.zeros((128, 32), np.float32)
    sc[:, 0:16] = 512.0 * np.arange(16)[None, :]
    sc[:, 16:24] = 512.0 * np.arange(8)[None, :]
    c["sconst"] = sc
    wc = np.zeros((128, 48), np.float32)
    for ee in range(4):
        for hf in range(2):
            wc[:, ee * 2 + hf] = (ee * 128 + np.arange(128)) * 2 + hf
    c["wconst"] = wc
    return c


def _layout_inputs(inputs, b):
    f = lambda k: np.ascontiguousarray(np.asarray(inputs[k], dtype=np.float32)[0])
    bc = lambda v: np.ascontiguousarray(np.broadcast_to(v.reshape(1, -1), (128, v.size)))
    m = {}
    m["x"] = np.ascontiguousarray(np.asarray(inputs["x"], dtype=np.float32)[b])
    m["w_in"] = f("w_in")
    m["nw_mix"] = np.ascontiguousarray(f("norm_mix_w").reshape(8, 128).T)
    qw = f("q_norm_w")
    kw = f("k_norm_w")
    m["qkw"] = np.ascontiguousarray(np.stack([np.tile(qw, 2), np.tile(kw, 2)], axis=1))
    cw = f("conv_w")
    m["cw"] = np.ascontiguousarray(cw.T.reshape(6, 128, 7).transpose(1, 0, 2).reshape(128, 42))
    m["cbias"] = np.ascontiguousarray(f("conv_b").reshape(6, 128).T)
    m["dtb"] = bc(f("dt_bias").reshape(-1))
    m["alog"] = bc(f("a_log").reshape(-1))
    m["dsk"] = bc(f("d_skip").reshape(-1))
    m["nssd"] = np.ascontiguousarray(f("ssd_norm_w").reshape(4, 128).T)
    m["w_attn_o"] = f("w_attn_o")
    m["w_ssd_o"] = f("w_ssd_o")
    m["w_out"] = f("w_out")
    m["bgate"] = np.ascontiguousarray(f("b_gate").reshape(16, 128).T)
    m["nffn"] = np.ascontiguousarray(f("norm_ffn_w").reshape(8, 128).T)
    m["wr"] = np.ascontiguousarray(np.concatenate([f("w_router_group"), f("w_router_expert")], axis=1))
    m["br"] = bc(np.concatenate([f("b_router_group"), f("b_router_expert")]))
    m["nf_bc"] = bc(f("norm_ffn_w"))
    m["w1"] = np.ascontiguousarray(f("w1").reshape(NE, 8, 128, DE).transpose(0, 2, 1, 3)).reshape(NE * 128 * 2, 2048)
    m["w3"] = np.ascontiguousarray(f("w3").reshape(NE, 8, 128, DE).transpose(0, 2, 1, 3)).reshape(NE * 128 * 2, 2048)
    m["w2"] = np.ascontiguousarray(f("w2").reshape(NE, 4, 128, D).transpose(0, 2, 1, 3)).reshape(NE * 128 * 2, 2048)
    return m


def kernel(**inputs):
    B = np.asarray(inputs["x"]).shape[0]
    if "nc" not in _CACHE:
        _CACHE["nc"] = build_program()
    nc = _CACHE["nc"]
    cst = _consts()
    in_maps = []
    for b in range(B):
        m = _layout_inputs(inputs, b)
        m.update(cst)
        in_maps.append(m)
    res = run_bass_kernel_spmd(nc, in_maps, core_ids=list(range(B)))
    return np.stack([np.asarray(r["out"]) for r in res.results], axis=0)
```

```python
import os
from contextlib import ExitStack

import numpy as np
import ml_dtypes

import concourse.bass as bass
import concourse.mybir as mybir
from concourse.bass_utils import run_bass_kernel_spmd

F32 = mybir.dt.float32
BF16 = mybir.dt.bfloat16
AF = mybir.ActivationFunctionType
ALU = mybir.AluOpType
AX = mybir.AxisListType

S = 4096
D = 1024
NT = S // 128
NS = S // 512
EPS = 1e-6
INP = 4112
C_Q, C_K, C_V, C_Z, C_XBC, C_DT, C_G = 0, 512, 640, 768, 1280, 2048, 2064
NE = 16
DE = 512


class Dep:
    __slots__ = ("name", "last_w", "readers", "epoch")

    def __init__(self, name):
        self.name = name
        self.last_w = None
        self.readers = []
        self.epoch = -1


class Instr:
    __slots__ = ("eng", "fn", "deps", "is_dma", "sem", "val", "signal", "idx", "pos", "grp")


ENGS = ("sync", "scalar", "vector", "gpsimd", "tensor")


class Prog:
    def __init__(self, nc):
        self.nc = nc
        self.es = ExitStack()
        self.eng_sem = {}
        for e in ENGS:
            self.eng_sem[e] = self.es.enter_context(nc.semaphore("tick_" + e))
        self.eng_tick = {e: 0 for e in ENGS}
        self.dma_sems = {}
        self.instrs = []
        self.known = {e: {} for e in ENGS}
        self.n_total = 0
        self.epoch = 0

    def close(self):
        self.es.close()

    def _rec(self, eng, fn, reads, writes, is_dma=False, semkey=None, grp=None):
        it = Instr()
        it.grp = grp
        it.eng = eng
        it.fn = fn
        it.is_dma = is_dma
        it.signal = False
        it.sem = None
        it.val = None
        it.idx = len(self.instrs)
        for dd in list(reads) + list(writes):
            if dd.epoch != self.epoch:
                dd.epoch = self.epoch
                dd.last_w = None
                dd.readers = []
        deps = set()
        for r in reads:
            if r.last_w is not None:
                deps.add(r.last_w)
        for w in writes:
            if w.last_w is not None:
                if not (grp is not None and self.instrs[w.last_w].grp == grp):
                    deps.add(w.last_w)
            deps.update(w.readers)
        for r in reads:
            r.readers.append(it.idx)
        for w in writes:
            w.last_w = it.idx
            w.readers = []
        if is_dma:
            ent = self.dma_sems.get(semkey)
            if ent is None:
                sem = self.es.enter_context(self.nc.semaphore("dma_%d" % len(self.dma_sems)))
                ent = [sem, 0, None, -1]
                self.dma_sems[semkey] = ent
            if ent[3] == self.epoch and ent[2] is not None:
                if not (grp is not None and self.instrs[ent[2]].grp == grp):
                    deps.add(ent[2])
            ent[1] += 1
            ent[2] = it.idx
            ent[3] = self.epoch
            it.sem = ent[0]
            it.val = 16 * ent[1]
        deps.discard(it.idx)
        it.deps = deps
        self.instrs.append(it)
        return it

    def op(self, eng, fn, reads=(), writes=()):
        return self._rec(eng, fn, list(reads), list(writes))

    def dma(self, out, in_, reads=(), writes=(), semkey=None, eng="sync"):
        assert semkey is not None
        return self._rec(eng, lambda e: e.dma_start(out=out, in_=in_), list(reads), list(writes),
                         is_dma=True, semkey=semkey)

    def barrier(self):
        last = {}
        pend = set()
        for it in self.instrs:
            if it.is_dma:
                pend.add(it.idx)
            else:
                last[it.eng] = it.idx
        allidx = set(last.values()) | pend
        for e in ENGS:
            it = self._rec(e, lambda eng: eng.nop(), [], [])
            it.deps = set(allidx)

    def emit(self):
        instrs = self.instrs
        per_eng = {e: [] for e in ENGS}
        for it in instrs:
            it.pos = len(per_eng[it.eng])
            per_eng[it.eng].append(it)

        def skipped(src, it):
            if src.is_dma or it.is_dma:
                return False
            if src.eng != it.eng:
                return False
            if src.eng == "tensor":
                return True
            return it.pos - src.pos > 2

        for it in instrs:
            for d in it.deps:
                src = instrs[d]
                if src.is_dma or skipped(src, it):
                    continue
                src.signal = True
        for e in ENGS:
            t = self.eng_tick[e]
            for it in per_eng[e]:
                if not it.is_dma and it.signal:
                    t += 1
                    it.sem = self.eng_sem[e]
                    it.val = t
            self.eng_tick[e] = t
        known = self.known

        def run_engine(ename, eobj):
            kn = known[ename]
            for it in per_eng[ename]:
                need = {}
                for d in it.deps:
                    src = instrs[d]
                    if skipped(src, it):
                        continue
                    key = src.sem.name
                    if kn.get(key, 0) >= src.val:
                        continue
                    if key not in need or need[key][1] < src.val:
                        need[key] = (src.sem, src.val)
                for key, (sem, val) in need.items():
                    eobj.wait_ge(sem, val)
                    kn[key] = val
                bi = it.fn(eobj)
                if it.is_dma:
                    bi.then_inc(it.sem, 16)
                elif it.signal:
                    bi.then_inc(it.sem, 1)

        with self.nc.Block() as block:
            @block.sync
            def _(e):
                run_engine("sync", e)

            @block.scalar
            def _(e):
                run_engine("scalar", e)

            @block.vector
            def _(e):
                run_engine("vector", e)

            @block.gpsimd
            def _(e):
                run_engine("gpsimd", e)

            @block.tensor
            def _(e):
                run_engine("tensor", e)
        self.n_total += len(instrs)
        self.instrs = []
        self.epoch += 1


def build_program(dbg=None, stop_after=None):
    nc = bass.Bass("TRN2", target_bir_lowering=False)
    P = Prog(nc)
    dbg = dbg or []
    _uq = [0]

    def uq(name):
        _uq[0] += 1
        return "%s_%d" % (name, _uq[0])

    def din(name, shape, dt=F32):
        return nc.dram_tensor(name, list(shape), dt, kind="ExternalInput").ap()

    def dscr(name, shape, dt):
        return nc.dram_tensor(name, list(shape), dt, kind="Internal").ap()

    def dout(name, shape, dt):
        return nc.dram_tensor(name, list(shape), dt, kind="ExternalOutput").ap()

    x_d = din("x", [S, D])
    w_in_d = din("w_in", [D, INP])
    out_d = dout("out", [S, D], F32)
    ident_bf_d = din("ident_bf", [128, 128], BF16)
    nw_mix_d = din("nw_mix", [128, 8])
    qkw_d = din("qkw", [128, 2])
    onesblk_d = din("onesblk", [128, 128], BF16)
    rmat_d = din("rmat", [128, 128], BF16)
    cosT_d = din("cosT", [128, S], BF16)
    sinT_d = din("sinT", [128, S], BF16)
    masks_d = din("masks", [128, 4, 128])
    cw_d = din("cw", [128, 42])
    cb_d = din("cbias", [128, 6])
    dtb_d = din("dtb", [128, 16])
    alog_d = din("alog", [128, 16])
    dsk_d = din("dsk", [128, 8])
    nssd_d = din("nssd", [128, 4])
    w_ao_d = din("w_attn_o", [512, D])
    w_so_d = din("w_ssd_o", [512, D])
    w_out_d = din("w_out", [D, D])
    bg_d = din("bgate", [128, 16])
    nf_d = din("nffn", [128, 8])
    wr_d = din("wr", [D, 20])
    br_d = din("br", [128, 20])
    w1_d = din("w1", [NE * 128 * 2, 2048])
    w3_d = din("w3", [NE * 128 * 2, 2048])
    w2_d = din("w2", [NE * 128 * 2, 2048])

    hT_d = dscr("hT_scratch", [NS, 128, 8, 512], BF16)
    x1_d = dscr("x1_scratch", [S, D], F32)

    def dbgout(name, shape, dt):
        return dout("dbg_" + name, shape, dt)

    def mm(out, lhsT, rhs, start, stop, reads, writes):
        P.op("tensor", lambda e: e.matmul(out, lhsT=lhsT, rhs=rhs, start=start, stop=stop), reads, writes)

    def tr(out, in_, idn, reads, writes):
        P.op("tensor", lambda e: e.transpose(out=out, in_=in_, identity=idn), reads, writes)

    def act(out, in_, func, reads, writes, **kw):
        P.op("scalar", lambda e: e.activation(out=out, in_=in_, func=func, **kw), reads, writes)

    def tt(eng, out, in0, in1, op, reads, writes):
        P.op(eng, lambda e: e.tensor_tensor(out=out, in0=in0, in1=in1, op=op), reads, writes)

    def ts(eng, out, in0, s1, s2, op0, op1, reads, writes):
        if op1 is None:
            P.op(eng, lambda e: e.tensor_scalar(out=out, in0=in0, scalar1=s1, scalar2=None, op0=op0), reads, writes)
        else:
            P.op(eng, lambda e: e.tensor_scalar(out=out, in0=in0, scalar1=s1, scalar2=s2, op0=op0, op1=op1), reads, writes)

    def stt(out, in0, scalar, in1, op0, op1, reads, writes):
        P.op("vector", lambda e: e.scalar_tensor_tensor(out=out, in0=in0, scalar=scalar, in1=in1, op0=op0, op1=op1),
             reads, writes)

    def cp(eng, out, in_, reads, writes):
        if eng == "scalar":
            P.op(eng, lambda e: e.copy(out=out, in_=in_), reads, writes)
        else:
            P.op(eng, lambda e: e.tensor_copy(out=out, in_=in_), reads, writes)

    def recip(out, in_, reads, writes):
        P.op("vector", lambda e: e.reciprocal(out=out, in_=in_), reads, writes)

    def memset(eng, ap, val, writes):
        P.op(eng, lambda e: e.memset(ap, val), [], writes)

    def red(out, in_, op, reads, writes):
        P.op("vector", lambda e: e.tensor_reduce(out=out, in_=in_, axis=AX.X, op=op), reads, writes)

    stg_ctr = [0]

    def load_w(stg, d_stg, src_view, KC, ncols, dst_fn, d_dst, scale=None, d_scale=None, piece=256, eng="vector"):
        for c0 in range(0, ncols, piece):
            n = min(piece, ncols - c0)
            sl = stg_ctr[0] % len(stg)
            stg_ctr[0] += 1
            P.dma(stg[sl][:, 0:KC, 0:n], src_view[:, :, c0:c0 + n], writes=[d_stg[sl]], semkey=("stg", sl))
            if scale is not None:
                tt(eng, dst_fn(c0, n), stg[sl][:, 0:KC, 0:n], scale.unsqueeze(2).to_broadcast([128, KC, n]), ALU.mult,
                   [d_stg[sl], d_scale], [d_dst])
            else:
                cp(eng, dst_fn(c0, n), stg[sl][:, 0:KC, 0:n], [d_stg[sl]], [d_dst])

    w_view = w_in_d.rearrange("(kc p) n -> p kc n", p=128)

    es_top = ExitStack()
    sbT = lambda name, shape, dt: es_top.enter_context(nc.sbuf_tensor(uq(name), list(shape), dt))
    ident = sbT("ident", [128, 128], BF16)
    d_ident = Dep("ident")
    nw = sbT("s_nw", [128, 8], F32)
    d_nw = Dep("nw")
    epsb = sbT("epsb", [128, 1], F32)
    d_eps = Dep("eps")
    es_ssd = ExitStack()
    ssdT = es_ssd.enter_context(nc.sbuf_tensor(uq("ssdT"), [128, 4, S], BF16))
    d_ssdT = Dep("ssdT")

    def finish():
        es_ssd.close()
        es_top.close()
        P.close()
        return nc

    with ExitStack() as ph:
        sb = lambda name, shape, dt: ph.enter_context(nc.sbuf_tensor(uq(name), list(shape), dt))
        ps = lambda name, shape, dt: ph.enter_context(nc.psum_tensor(uq(name), list(shape), dt))
        P.dma(ident[:, :], ident_bf_d[:, :], writes=[d_ident], semkey="c0")
        P.dma(nw[:, :], nw_mix_d[:, :], writes=[d_nw], semkey="c1")
        memset("gpsimd", epsb[:, :], EPS, [d_eps])
        NXB = 6
        xt = [sb("xt%d" % i, [128, D], F32) for i in range(NXB)]
        d_xt = [Dep("xt%d" % i) for i in range(NXB)]
        junk = sb("junk", [128, D], BF16)
        d_junk = Dep("junk")
        xn = [sb("xn%d" % i, [128, D], BF16) for i in range(2)]
        d_xn = [Dep("xn%d" % i) for i in range(2)]
        ss = sb("ss", [128, NT], F32)
        rs = sb("rs", [128, NT], F32)
        d_ss = [Dep("ss%d" % i) for i in range(NT)]
        d_rs = [Dep("rs%d" % i) for i in range(NT)]
        tp = [ps("tp%d" % i, [128, 8, 128], BF16) for i in range(2)]
        d_tp = [Dep("tp%d" % i) for i in range(2)]
        hs = [sb("hs%d" % i, [128, 8, 512], BF16) for i in range(2)]
        d_hs = [Dep("hs%d" % i) for i in range(2)]
        def p1_load(t):
            xb = t % NXB
            P.dma(xt[xb][:, :], x_d[t * 128:(t + 1) * 128, :], writes=[d_xt[xb]], semkey=("xt", xb))

        def p1_stage1(t):
            xb = t % NXB
            act(junk[:, :], xt[xb][:, :], AF.Square, [d_xt[xb]], [d_junk, d_ss[t]], accum_out=ss[:, t:t + 1])
            act(rs[:, t:t + 1], ss[:, t:t + 1], AF.Sqrt, [d_ss[t], d_eps], [d_rs[t]], bias=epsb[:, 0:1], scale=1.0 / D)
            recip(rs[:, t:t + 1], rs[:, t:t + 1], [d_rs[t]], [d_rs[t]])
            nb = t % 2
            ts("vector", xn[nb][:, :], xt[xb][:, :], rs[:, t:t + 1], None, ALU.mult, None, [d_xt[xb], d_rs[t]], [d_xn[nb]])

        def p1_stage2(t):
            j, i4 = divmod(t, 4)
            nb = t % 2
            for c in range(8):
                tr(tp[nb][:, c, :], xn[nb][:, c * 128:(c + 1) * 128], ident[:, :], [d_xn[nb], d_ident], [d_tp[nb]])
            hb = j % 2
            cp("scalar", hs[hb][:, :, i4 * 128:(i4 + 1) * 128], tp[nb][:, :, :], [d_tp[nb]], [d_hs[hb]])
            if i4 == 3:
                P.dma(hT_d[j], hs[hb][:, :, :], reads=[d_hs[hb]], semkey=("hs", hb))

        LA = 4
        for t in range(LA):
            p1_load(t)
        p1_stage1(0)
        for t in range(NT):
            if t + LA < NT:
                p1_load(t + LA)
            if t + 1 < NT:
                p1_stage1(t + 1)
            p1_stage2(t)
        P.barrier()
        P.emit()
    if stop_after == 1:
        return finish()

    es4 = ExitStack()
    sb4 = lambda name, shape, dt: es4.enter_context(nc.sbuf_tensor(uq(name), list(shape), dt))
    xs_tok = sb4("xs_tok", [128, NT, 512], BF16)
    B_tok = sb4("B_tok", [128, NT, 128], BF16)
    BT = sb4("BT", [128, S], BF16)
    CTz = sb4("CTz", [128, 2, S], BF16)
    dtv = sb4("dtv", [128, NT, 16], F32)
    dAv = sb4("dAv", [128, NT, 16], F32)
    d_xs = [Dep("xs%d" % t) for t in range(NT)]
    d_Bt = [Dep("Bt%d" % t) for t in range(NT)]
    d_BT = [Dep("BT%d" % j) for j in range(NS)]
    d_CT = [Dep("CT%d" % j) for j in range(NS)]
    d_dt = [Dep("dt%d" % t) for t in range(NT)]
    d_CTinit = Dep("CTinit")

    with ExitStack() as ph:
        sb = lambda name, shape, dt: ph.enter_context(nc.sbuf_tensor(uq(name), list(shape), dt))
        ps = lambda name, shape, dt: ph.enter_context(nc.psum_tensor(uq(name), list(shape), dt))
        cw = sb("s_cw", [128, 42], F32); d_cw = Dep("cw")
        cbias = sb("s_cb", [128, 6], F32); d_cb = Dep("cb")
        dtb = sb("s_dtb", [128, 16], F32); d_dtb = Dep("dtb")
        aneg = sb("s_aneg", [128, 16], F32); d_an = Dep("aneg")
        P.dma(cw[:, :], cw_d[:, :], writes=[d_cw], semkey="c0")
        P.dma(cbias[:, :], cb_d[:, :], writes=[d_cb], semkey="c1")
        P.dma(dtb[:, :], dtb_d[:, :], writes=[d_dtb], semkey="c2")
        P.dma(aneg[:, :], alog_d[:, :], writes=[d_an], semkey="c3")
        act(aneg[:, :], aneg[:, :], AF.Exp, [d_an], [d_an])
        ts("vector", aneg[:, :], aneg[:, :], -1.0, None, ALU.mult, None, [d_an], [d_an])
        diagw = sb("diagw", [128, 42, 128], BF16); d_dg = Dep("diagw")
        for i in range(42):
            ts("vector", diagw[:, i, :], ident[:, :], cw[:, i:i + 1], None, ALU.mult, None, [d_ident, d_cw], [d_dg])
        memset("gpsimd", CTz[:, :, :], 0.0, [d_CTinit])
        Wx = sb("Wx", [128, 8, 768], BF16); Wd = sb("Wd", [128, 8, 16], BF16); d_W = Dep("W4a")
        stg = [sb("stg%d" % i, [128, 8, 256], F32) for i in range(2)]; d_stg = [Dep("stg%d" % i) for i in range(2)]
        load_w(stg, d_stg, w_view[:, :, C_XBC:C_XBC + 768], 8, 768, lambda c0, n: Wx[:, :, c0:c0 + n], d_W, nw[:, :], d_nw)
        load_w(stg, d_stg, w_view[:, :, C_DT:C_DT + 16], 8, 16, lambda c0, n: Wd[:, :, c0:c0 + n], d_W, nw[:, :], d_nw)
        xraw = sb("xraw", [128, 6, S + 8], BF16)
        d_xr = [Dep("xr%d" % j) for j in range(NS)]
        d_xpad = Dep("xpad")
        memset("gpsimd", xraw[:, :, 0:3], 0.0, [d_xpad])
        memset("gpsimd", xraw[:, :, S + 3:S + 8], 0.0, [d_xpad])
        hts = [sb("hts%d" % i, [128, 8, 512], BF16) for i in range(2)]; d_hts = [Dep("hts%d" % i) for i in range(2)]
        pp = [ps("pp%d" % i, [128, 512], F32) for i in range(2)]; d_pp = [Dep("pp%d" % i) for i in range(2)]
        pd = [ps("pd%d" % i, [128, 16], F32) for i in range(2)]; d_pd = [Dep("pd%d" % i) for i in range(2)]
        ptr = [ps("ptr%d" % i, [128, 4, 128], BF16) for i in range(2)]; d_ptr = [Dep("ptr%d" % i) for i in range(2)]
        cvo = [sb("cvo%d" % i, [128, 512], BF16) for i in range(2)]; d_cvo = [Dep("cvo%d" % i) for i in range(2)]
        dtt = [sb("dtt%d" % i, [128, 16], F32) for i in range(2)]; d_dtt = [Dep("dtt%d" % i) for i in range(2)]
        u = 0
        for j in range(NS):
            hb = j % 2
            P.dma(hts[hb][:, :, :], hT_d[j], writes=[d_hts[hb]], semkey=("hts", hb))
            for c in range(6):
                b = u % 2
                u += 1
                for kc in range(8):
                    mm(pp[b][:, :], Wx[:, kc, c * 128:(c + 1) * 128], hts[hb][:, kc, :], kc == 0, kc == 7,
                       [d_W, d_hts[hb]], [d_pp[b]])
                cp("scalar", xraw[:, c, 3 + j * 512:3 + (j + 1) * 512], pp[b][:, :], [d_pp[b], d_xpad], [d_xr[j]])
            for i4 in range(4):
                t = j * 4 + i4
                b = t % 2
                for kc in range(8):
                    mm(pd[b][:, :], hts[hb][:, kc, i4 * 128:(i4 + 1) * 128], Wd[:, kc, :], kc == 0, kc == 7,
                       [d_W, d_hts[hb]], [d_pd[b]])
                tt("vector", dtt[b][:, :], pd[b][:, :], dtb[:, :], ALU.add, [d_pd[b], d_dtb], [d_dtt[b]])
                act(dtt[b][:, :], dtt[b][:, :], AF.Exp, [d_dtt[b]], [d_dtt[b]])
                act(dtv[:, t, :], dtt[b][:, :], AF.Ln, [d_dtt[b]], [d_dt[t]], bias=1.0)
                tt("vector", dAv[:, t, :], dtv[:, t, :], aneg[:, :], ALU.mult, [d_dt[t], d_an], [d_dt[t]])
        for j in range(NS):
            rd = [d_xr[j], d_xpad, d_dg]
            if j > 0:
                rd.append(d_xr[j - 1])
            if j < NS - 1:
                rd.append(d_xr[j + 1])
            for c in range(6):
                b = u % 2
                u += 1
                for tap in range(7):
                    mm(pp[b][:, :], diagw[:, c * 7 + tap, :], xraw[:, c, j * 512 + tap:j * 512 + tap + 512], tap == 0, tap == 6,
                       rd, [d_pp[b]])
                js = slice(j * 512, (j + 1) * 512)
                if c < 5:
                    dst = cvo[b][:, :] if c < 4 else BT[:, js]
                    wr = [d_cvo[b]] if c < 4 else [d_BT[j]]
                    act(dst, pp[b][:, :], AF.Silu, [d_pp[b], d_cb], wr, bias=cbias[:, c:c + 1])
                    for i4 in range(4):
                        t = j * 4 + i4
                        if c < 4:
                            tr(ptr[b][:, i4, :], cvo[b][:, i4 * 128:(i4 + 1) * 128], ident[:, :], [d_cvo[b], d_ident], [d_ptr[b]])
                        else:
                            tr(ptr[b][:, i4, :], BT[:, t * 128:(t + 1) * 128], ident[:, :], [d_BT[j], d_ident], [d_ptr[b]])
                    if c < 4:
                        cp("vector", xs_tok[:, j * 4:(j + 1) * 4, c * 128:(c + 1) * 128], ptr[b][:, :, :], [d_ptr[b]],
                           [d_xs[j * 4 + i] for i in range(4)])
                    else:
                        cp("vector", B_tok[:, j * 4:(j + 1) * 4, :], ptr[b][:, :, :], [d_ptr[b]],
                           [d_Bt[j * 4 + i] for i in range(4)])
                else:
                    act(CTz[0:64, 0, js], pp[b][0:64, :], AF.Silu, [d_pp[b], d_cb, d_CTinit], [d_CT[j]], bias=cbias[0:64, 5:6])
                    act(CTz[64:128, 1, js], pp[b][64:128, :], AF.Silu, [d_pp[b], d_cb, d_CTinit], [d_CT[j]], bias=cbias[64:128, 5:6])
        if "ssd_a" in dbg:
            o1 = dbgout("xs_tok", [128, NT, 512], BF16); o2 = dbgout("B_tok", [128, NT, 128], BF16)
            o3 = dbgout("CTz", [128, 2, S], BF16); o4 = dbgout("dtv", [128, NT, 16], F32); o5 = dbgout("BT", [128, S], BF16)
            P.dma(o1, xs_tok[:, :, :], reads=d_xs, semkey="d0")
            P.dma(o2, B_tok[:, :, :], reads=d_Bt, semkey="d1")
            P.dma(o3, CTz[:, :, :], reads=d_CT, semkey="d2")
            P.dma(o4, dtv[:, :, :], reads=d_dt, semkey="d3")
            P.dma(o5, BT[:, :], reads=d_BT, semkey="d4")
        P.barrier()
        P.emit()
    if stop_after == 41:
        es4.close()
        return finish()

    with ExitStack() as ph:
        sb = lambda name, shape, dt: ph.enter_context(nc.sbuf_tensor(uq(name), list(shape), dt))
        ps = lambda name, shape, dt: ph.enter_context(nc.psum_tensor(uq(name), list(shape), dt))
        masks = sb("s_masks", [128, 4, 128], F32); d_mk = Dep("masks")
        P.dma(masks[:, :, :], masks_d[:, :, :], writes=[d_mk], semkey="c0")
        MU, MSL, ML, MSU = 0, 1, 2, 3
        dsk = sb("s_dsk", [128, 8], F32); d_dsk = Dep("dsk")
        P.dma(dsk[:, :], dsk_d[:, :], writes=[d_dsk], semkey="c1")
        ones_f = sb("ones_f", [128, 128], F32); d_of = Dep("ones_f")
        memset("gpsimd", ones_f[:, :], 1.0, [d_of])
        Wz = sb("Wz", [128, 8, 512], BF16); d_Wz = Dep("Wz")
        stg = [sb("stg%d" % i, [128, 8, 256], F32) for i in range(2)]; d_stg = [Dep("stg%d" % i) for i in range(2)]
        load_w(stg, d_stg, w_view[:, :, C_Z:C_Z + 512], 8, 512, lambda c0, n: Wz[:, :, c0:c0 + n], d_Wz, nw[:, :], d_nw)
        Sb_all = sb("Sb_all", [128, NT, 256], BF16)
        d_Sb = [Dep("Sb%d" % t) for t in range(NT)]
        St = [sb("St%d" % i, [128, 256], F32) for i in range(2)]
        d_St = [Dep("St%d" % i) for i in range(2)]
        Sf_bf = [sb("Sfbf%d" % i, [128, 256], BF16) for i in range(2)]; d_Sfbf = [Dep("Sfbf%d" % i) for i in range(2)]
        pcb_t = ps("pcb_t", [128, 512], F32)
        pcb = pcb_t[:, 0:256].rearrange("p (g i) -> p g i", g=2); d_pcb = Dep("pcb")
        psm_main = [pcb_t[:, 256:352]]; d_psm_main = [d_pcb]
        pst = ps("pst", [128, 512], F32); d_pst = Dep("pst")
        pseg = [ps("pseg%d" % i, [128, 4, 128], F32) for i in range(2)]; d_pseg = [Dep("pseg%d" % i) for i in range(2)]
        pyd = ps("pyd", [128, 512], F32); d_pyd = Dep("pyd")
        pyo = ps("pyo", [128, 512], F32); d_pyo = Dep("pyo")
        pz = ps("pz", [128, 512], F32); d_pz = Dep("pz")
        sm_ = [sb("sm%d" % i, [128, 96], F32) for i in range(2)]; d_sm_ = [Dep("sm%d" % i) for i in range(2)]
        cd2 = sb("cd2", [128, 2, 4], F32); d_cd2 = Dep("cd2")
        dtw = sb("dtw", [128, 16], F32); d_dtw = Dep("dtw")
        xdt = [sb("xdt%d" % i, [128, 512], BF16) for i in range(2)]; d_xdt = [Dep("xdt%d" % i) for i in range(2)]
        xw = sb("xw", [128, 512], BF16); d_xw = Dep("xw")
        cbm = [sb("cbm%d" % i, [128, 2, 128], F32) for i in range(2)]; d_cbm = [Dep("cbm%d" % i) for i in range(2)]
        Lh_ = [sb("Lh%d" % i, [128, 8, 128], F32) for i in range(2)]; d_Lh_ = [Dep("Lh%d" % i) for i in range(2)]
        Ee_ = [sb("Ee%d" % i, [128, 8, 128], F32) for i in range(2)]; d_Ee_ = [Dep("Ee%d" % i) for i in range(2)]
        MT_ = [sb("MT%d" % i, [128, 8, 128], BF16) for i in range(2)]; d_MT_ = [Dep("MT%d" % i) for i in range(2)]
        ya_ = [sb("ya%d" % i, [128, 512], F32) for i in range(2)]; d_ya_ = [Dep("ya%d" % i) for i in range(2)]
        yb = sb("yb", [128, 512], F32); d_yb = Dep("yb")
        zs_ = [sb("zs%d" % i, [128, 512], F32) for i in range(2)]; d_zs_ = [Dep("zs%d" % i) for i in range(2)]
        yn = sb("yn", [128, 512], BF16); d_yn = Dep("yn")
        junk4 = sb("junk4", [128, 512], BF16); d_j4 = Dep("junk4")
        ssq = sb("ssq", [128, 2], F32); d_ssq = Dep("ssq")
        ptr4 = ps("ptr4", [128, 4, 128], BF16); d_ptr4 = Dep("ptr4")
        hts = [sb("hts%d" % i, [128, 8, 512], BF16) for i in range(2)]; d_hts = [Dep("hts%d" % i) for i in range(2)]
        for i in range(2):
            memset("gpsimd", St[i][:, :], 0.0, [d_St[i]])

        Bdef = (cd2, d_cd2, dtw, d_dtw, xw, d_xw, pst, d_pst)
        def small_sums(t, which, sm, d_sm, psmo=None):
            psm, d_psm = ([psmo[0]], [psmo[1]]) if psmo is not None else (psm_main, d_psm_main)
            rdA = [d_dt[t], d_mk, d_of]
            if "acum" in which:
                mm(psm[0][:, 0:8], masks[:, MU, :], dAv[:, t, 0:8], True, True, rdA, [d_psm[0]])
                mm(psm[0][:, 8:16], masks[:, ML, :], dAv[:, t, 8:16], True, True, rdA, [d_psm[0]])
                mm(psm[0][:, 16:24], masks[:, MSL, :], dAv[:, t, 0:8], True, True, rdA, [d_psm[0]])
                mm(psm[0][:, 24:40], ones_f[:, :], dAv[:, t, 0:16], True, True, rdA, [d_psm[0]])
                act(sm[:, 0:40], psm[0][:, 0:40], AF.Exp, [d_psm[0]], [d_sm])
            else:
                mm(psm[0][:, 64:72], masks[:, MSU, :], dAv[:, t, 8:16], True, True, rdA, [d_psm[0]])
                mm(psm[0][:, 72:88], ones_f[:, :], dAv[:, t, 0:16], True, True, rdA, [d_psm[0]])
                act(sm[:, 64:88], psm[0][:, 64:88], AF.Exp, [d_psm[0]], [d_sm])

        def state_step1(t, d, sm, d_sm, bufs=None):
            cd2, d_cd2, dtw, d_dtw, xw, d_xw, pst, d_pst = bufs if bufs is not None else Bdef
            tot0 = 24 if d == 0 else 72
            cdv = sm[:, tot0:tot0 + 16].rearrange("p (a h) -> p a h", a=2)
            cp("gpsimd", cd2[0:64, :, :], cdv[0:64, :, 0:4], [d_sm], [d_cd2])
            cp("gpsimd", cd2[64:128, :, :], cdv[64:128, :, 4:8], [d_sm], [d_cd2])
            dcol = 16 if d == 0 else 64
            tt("vector", dtw[:, 0:8], dtv[:, t, d * 8:(d + 1) * 8], sm[:, dcol:dcol + 8], ALU.mult, [d_dt[t], d_sm], [d_dtw])
            tt("vector", xw[:, :].rearrange("p (h q) -> p h q", h=8), xs_tok[:, t, :].rearrange("p (h q) -> p h q", h=8),
               dtw[:, 0:8].unsqueeze(2).to_broadcast([128, 8, 64]), ALU.mult, [d_xs[t], d_dtw], [d_xw])
            for g in range(2):
                mm(pst[:, g * 256:(g + 1) * 256], B_tok[:, t, :], xw[:, g * 256:(g + 1) * 256], True, True,
                   [d_Bt[t], d_xw], [d_pst])

        def state_step2(t, d, bufs=None):
            cd2, d_cd2, dtw, d_dtw, xw, d_xw, pst, d_pst = bufs if bufs is not None else Bdef
            tt("vector", St[d][:, :].rearrange("p (h q) -> p h q", h=4), St[d][:, :].rearrange("p (h q) -> p h q", h=4),
               cd2[:, d, :].unsqueeze(2).to_broadcast([128, 4, 64]), ALU.mult, [d_St[d], d_cd2], [d_St[d]])
            tt("vector", St[d][0:64, :], St[d][0:64, :], pst[0:64, 0:256], ALU.add, [d_St[d], d_pst], [d_St[d]])
            tt("vector", St[d][64:128, :], St[d][64:128, :], pst[64:128, 256:512], ALU.add, [d_St[d], d_pst], [d_St[d]])

        RB = 4
        Bring = [Bdef]
        pst_ring = [(pst, d_pst), (pyd, d_pyd), (pyo, d_pyo), (pz, d_pz)]
        for r in range(1, RB):
            Bring.append((sb("cd2r%d" % r, [128, 2, 4], F32), Dep("cd2r%d" % r), sb("dtwr%d" % r, [128, 16], F32), Dep("dtwr%d" % r),
                          sb("xwr%d" % r, [128, 512], BF16), Dep("xwr%d" % r), pst_ring[r][0], pst_ring[r][1]))
        smB = [sm_[0], sm_[1], sb("smr2", [128, 96], F32), sb("smr3", [128, 96], F32)]
        d_smB = [d_sm_[0], d_sm_[1], Dep("smr2"), Dep("smr3")]
        psmB = [(pseg[i][:, 0, :], d_pseg[i]) for i in range(2)]
        orderB = list(range(NT - 1, -1, -1))

        def preB(k):
            t, r = orderB[k], k % RB
            small_sums(t, ["dte_b"], smB[r], d_smB[r], psmB[k % 2])
            state_step1(t, 1, smB[r], d_smB[r], Bring[r])

        for k in range(min(RB - 1, NT)):
            preB(k)
        for k, t in enumerate(orderB):
            cp("vector", Sb_all[:, t, :], St[1][:, :], [d_St[1]], [d_Sb[t]])
            state_step2(t, 1, Bring[k % RB])
            if k + RB - 1 < NT:
                preB(k + RB - 1)
        A_NT = int(os.environ.get("SSD_NT", str(NT)))

        def tail_pool(t):
            ya, d_ya, zs, d_zs = ya_[t % 2], d_ya_[t % 2], zs_[t % 2], d_zs_[t % 2]
            tt("gpsimd", ya[:, :], ya[:, :], yb[:, :], ALU.add, [d_ya, d_yb], [d_ya])
            tt("gpsimd", yb[:, :].rearrange("p (h q) -> p h q", h=8), xs_tok[:, t, :].rearrange("p (h q) -> p h q", h=8),
               dsk[:, :].unsqueeze(2).to_broadcast([128, 8, 64]), ALU.mult, [d_xs[t], d_dsk], [d_yb])
            tt("gpsimd", ya[:, :], ya[:, :], yb[:, :], ALU.add, [d_ya, d_yb], [d_ya])
            tt("gpsimd", ya[:, :], ya[:, :], zs[:, :], ALU.mult, [d_ya, d_zs], [d_ya])

        def tail_act(t):
            ya, d_ya = ya_[t % 2], d_ya_[t % 2]
            act(junk4[:, :], ya[:, :], AF.Square, [d_ya], [d_j4, d_ssq], accum_out=ssq[:, 0:1])
            act(ssq[:, 1:2], ssq[:, 0:1], AF.Sqrt, [d_ssq, d_eps], [d_ssq], bias=epsb[:, 0:1], scale=1.0 / 512)

        def tail_dve(t):
            ya, d_ya = ya_[t % 2], d_ya_[t % 2]
            recip(ssq[:, 1:2], ssq[:, 1:2], [d_ssq], [d_ssq])
            ts("vector", yn[:, :], ya[:, :], ssq[:, 1:2], None, ALU.mult, None, [d_ya, d_ssq], [d_yn])

        def tail_pe(t):
            for c in range(4):
                tr(ptr4[:, c, :], yn[:, c * 128:(c + 1) * 128], ident[:, :], [d_yn, d_ident], [d_ptr4])

        def tail_out(t):
            cp("scalar", ssdT[:, :, t * 128:(t + 1) * 128], ptr4[:, :, :], [d_ptr4], [d_ssdT])

        pend = None
        for t in range(A_NT):
            j, i4 = divmod(t, 4)
            hb = j % 2
            tok = slice(t * 128, (t + 1) * 128)
            if i4 == 0:
                P.dma(hts[hb][:, :, :], hT_d[j], writes=[d_hts[hb]], semkey=("hts", hb))
            fb = t % 2
            ya, d_ya, zs, d_zs = ya_[t % 2], d_ya_[t % 2], zs_[t % 2], d_zs_[t % 2]
            sm, d_sm = sm_[t % 2], d_sm_[t % 2]
            for kc in range(8):
                mm(pz[:, :], hts[hb][:, kc, i4 * 128:(i4 + 1) * 128], Wz[:, kc, :], kc == 0, kc == 7, [d_hts[hb], d_Wz], [d_pz])
            act(zs[:, :], pz[:, :], AF.Silu, [d_pz], [d_zs])
            cp("gpsimd", Sf_bf[fb][:, :], St[0][:, :], [d_St[0]], [d_Sfbf[fb]])
            for d in range(2):
                dsl = slice(d * 8, (d + 1) * 8)
                lmask = MSL if d == 0 else MSU
                tt("vector" if d == 0 else "gpsimd", xdt[d][:, :].rearrange("p (h q) -> p h q", h=8), xs_tok[:, t, :].rearrange("p (h q) -> p h q", h=8),
                   dtv[:, t, dsl].unsqueeze(2).to_broadcast([128, 8, 64]), ALU.mult, [d_xs[t], d_dt[t]], [d_xdt[d]])
                tt("vector" if d == 0 else "gpsimd", Lh_[d][:, :, :], masks[:, lmask:lmask + 1, :].to_broadcast([128, 8, 128]),
                   dAv[:, t, dsl].unsqueeze(2).to_broadcast([128, 8, 128]), ALU.mult, [d_mk, d_dt[t]], [d_Lh_[d]])
            small_sums(t, ["acum", "dte_f"], sm, d_sm)
            for g in range(2):
                mm(pcb[:, g, :], BT[:, tok], CTz[:, g, tok], True, True, [d_BT[j], d_CT[j]], [d_pcb])

            def seg(d):
                tri = MU if d == 0 else ML
                for h in range(8):
                    mm(pseg[h // 4][:, h % 4, :], Lh_[d][:, h, :], masks[:, tri, :], True, True, [d_Lh_[d], d_mk], [d_pseg[h // 4]])
                for q in range(2):
                    act(Ee_[d][:, q * 4:(q + 1) * 4, :], pseg[q][:, :, :], AF.Exp, [d_pseg[q]], [d_Ee_[d]])

            def mtmul(d):
                for g in range(2):
                    tt("vector", MT_[d][:, g * 4:(g + 1) * 4, :], Ee_[d][:, g * 4:(g + 1) * 4, :],
                       cbm[d][:, g:g + 1, :].to_broadcast([128, 4, 128]), ALU.mult, [d_Ee_[d], d_cbm[d]], [d_MT_[d]])

            def ymm(d):
                for h in range(8):
                    mm(pyd[:, h * 64:(h + 1) * 64], MT_[d][:, h, :], xdt[d][:, h * 64:(h + 1) * 64], True, True,
                       [d_MT_[d], d_xdt[d]], [d_pyd])
                Sin = Sf_bf[fb][:, :] if d == 0 else Sb_all[:, t, :]
                dS = d_Sfbf[fb] if d == 0 else d_Sb[t]
                for g in range(2):
                    mm(pyo[:, g * 256:(g + 1) * 256], CTz[:, g, tok], Sin, True, True, [d_CT[j], dS], [d_pyo])

            def yacc(d):
                dsl = slice(d * 8, (d + 1) * 8)
                yy = ya if d == 0 else yb
                dy = d_ya if d == 0 else d_yb
                tt("vector", yy[:, :].rearrange("p (h q) -> p h q", h=8), pyo[:, :].rearrange("p (h q) -> p h q", h=8),
                   sm[:, dsl].unsqueeze(2).to_broadcast([128, 8, 64]), ALU.mult, [d_pyo, d_sm], [dy])
                tt("vector", yy[:, :], yy[:, :], pyd[:, :], ALU.add, [dy, d_pyd], [dy])

            seg(0)
            state_step1(t, 0, sm, d_sm)
            tt("vector", cbm[0][:, :, :], pcb[:, :, :], masks[:, MU:MU + 1, :].to_broadcast([128, 2, 128]), ALU.mult,
               [d_pcb, d_mk], [d_cbm[0]])
            tt("vector", cbm[1][:, :, :], pcb[:, :, :], masks[:, ML:ML + 1, :].to_broadcast([128, 2, 128]), ALU.mult,
               [d_pcb, d_mk], [d_cbm[1]])
            seg(1)
            if pend is not None:
                tail_act(pend)
            mtmul(0)
            ymm(0)
            state_step2(t, 0)
            mtmul(1)
            yacc(0)
            if pend is not None:
                tail_dve(pend)
            ymm(1)
            if pend is not None:
                tail_pe(pend)
            yacc(1)
            if pend is not None:
                tail_out(pend)
            tail_pool(t)
            pend = t
        tail_act(pend)
        tail_dve(pend)
        tail_pe(pend)
        tail_out(pend)
        if "ssdT" in dbg:
            o1 = dbgout("ssdT", [128, 4, S], BF16)
            P.dma(o1, ssdT[:, :, :], reads=[d_ssdT], semkey="d0")
        P.barrier()
        P.emit()
    es4.close()
    if stop_after == 4:
        return finish()


    es35 = ExitStack()
    sb35 = lambda name, shape, dt: es35.enter_context(nc.sbuf_tensor(uq(name), list(shape), dt))
    attnT = sb35("attnT", [128, 4, S], BF16)
    d_attn = Dep("attnT")
    es23 = ExitStack()
    sb23 = lambda name, shape, dt: es23.enter_context(nc.sbuf_tensor(uq(name), list(shape), dt))
    kTd = sb23("kTd", [128, 2, S], BF16)
    Va = sb23("Va", [128, NT, 2, 128], BF16)
    Vb = sb23("Vb", [128, NT, 2, 128], BF16)
    cosT = sb23("s_cosT", [128, S], BF16); d_cos = Dep("cos")
    sinT = sb23("s_sinT", [128, S], BF16); d_sin = Dep("sin")
    qkw = sb23("s_qkw", [128, 2], F32); d_qkw = Dep("qkw")
    onesblk = sb23("s_onesblk", [128, 128], BF16); d_ob = Dep("ob")
    rmat = sb23("s_rmat", [128, 128], BF16); d_rm = Dep("rm")
    sq = sb23("sq", [128, 512], BF16); d_sq = Dep("sq")
    rq = sb23("rq", [128, 512], F32); d_rq = Dep("rq")
    qn = sb23("qn", [128, 512], BF16); d_qn = Dep("qn")
    t1 = sb23("t1", [128, 512], F32); d_t1 = Dep("t1")
    t2 = sb23("t2", [128, 512], F32); d_t2 = Dep("t2")
    sq0, d_sq0, rq0, d_rq0, qn0, d_qn0, t10, d_t10, t20, d_t20 = sq, d_sq, rq, d_rq, qn, d_qn, t1, d_t1, t2, d_t2
    d_kT = [[Dep("kT%d_%d" % (m, j)) for j in range(NS)] for m in range(2)]
    d_V = [Dep("V%d" % t) for t in range(NT)]
    d_Vinit = Dep("Vinit")

    def normrope(qp_ap, d_qp, aux_ap, d_aux, wcol, j, dst, d_dst, alt=None):
        sq, d_sq, rq, d_rq, qn, d_qn, t1, d_t1, t2, d_t2 = alt if alt is not None else (sq0, d_sq0, rq0, d_rq0, qn0, d_qn0, t10, d_t10, t20, d_t20)
        act(sq[:, :], qp_ap, AF.Square, [d_qp], [d_sq])
        mm(aux_ap, onesblk[:, :], sq[:, :], True, True, [d_ob, d_sq], [d_aux])
        act(rq[:, :], aux_ap, AF.Sqrt, [d_aux, d_eps], [d_rq], bias=epsb[:, 0:1], scale=1.0 / 64)
        recip(rq[:, :], rq[:, :], [d_rq], [d_rq])
        stt(qn[:, :], qp_ap, qkw[:, wcol:wcol + 1], rq[:, :], ALU.mult, ALU.mult, [d_qp, d_rq, d_qkw], [d_qn])
        mm(aux_ap, rmat[:, :], qn[:, :], True, True, [d_rm, d_qn], [d_aux])
        js = slice(j * 512, (j + 1) * 512)
        tt("vector", t1[:, :], qn[:, :], cosT[:, js], ALU.mult, [d_qn, d_cos], [d_t1])
        tt("vector", t2[:, :], aux_ap, sinT[:, js], ALU.mult, [d_aux, d_sin], [d_t2])
        tt("gpsimd", dst, t1[:, :], t2[:, :], ALU.add, [d_t1, d_t2], [d_dst])

    with ExitStack() as ph:
        sb = lambda name, shape, dt: ph.enter_context(nc.sbuf_tensor(uq(name), list(shape), dt))
        ps = lambda name, shape, dt: ph.enter_context(nc.psum_tensor(uq(name), list(shape), dt))
        P.dma(qkw[:, :], qkw_d[:, :], writes=[d_qkw], semkey="c0")
        P.dma(onesblk[:, :], onesblk_d[:, :], writes=[d_ob], semkey="c1")
        P.dma(rmat[:, :], rmat_d[:, :], writes=[d_rm], semkey="c2")
        P.dma(cosT[:, :], cosT_d[:, :], writes=[d_cos], semkey="c3")
        P.dma(sinT[:, :], sinT_d[:, :], writes=[d_sin], semkey="c4")
        memset("gpsimd", Va[:, :, :, 64:128], 0.0, [d_Vinit])
        memset("gpsimd", Va[:, :, :, 64:65], 1.0, [d_Vinit])
        memset("gpsimd", Vb[:, :, :, 0:64], 0.0, [d_Vinit])
        memset("gpsimd", Vb[:, :, :, 0:1], 1.0, [d_Vinit])
        Wk = sb("Wk", [128, 8, 256], BF16); Wv = sb("Wv", [128, 8, 128], BF16); d_W = Dep("W2")
        stg = [sb("stg%d" % i, [128, 8, 256], F32) for i in range(2)]; d_stg = [Dep("stg%d" % i) for i in range(2)]
        for (dlo, slo) in [(0, 0), (64, 0), (128, 64), (192, 64)]:
            load_w(stg, d_stg, w_view[:, :, C_K + slo:C_K + slo + 64], 8, 64,
                   lambda c0, n, dlo=dlo: Wk[:, :, dlo + c0:dlo + c0 + n], d_W, nw[:, :], d_nw)
        load_w(stg, d_stg, w_view[:, :, C_V:C_V + 128], 8, 128, lambda c0, n: Wv[:, :, c0:c0 + n], d_W, nw[:, :], d_nw)
        hts = [sb("hts%d" % i, [128, 8, 512], BF16) for i in range(2)]; d_hts = [Dep("hts%d" % i) for i in range(2)]
        qp = [ps("qp%d" % i, [128, 512], F32) for i in range(2)]; d_qp = [Dep("qp%d" % i) for i in range(2)]
        aux = [ps("aux%d" % i, [128, 512], F32) for i in range(2)]; d_aux = [Dep("aux%d" % i) for i in range(2)]
        vp = [ps("vp%d" % i, [128, 128], F32) for i in range(2)]; d_vp = [Dep("vp%d" % i) for i in range(2)]
        alt1 = (sb("sq1", [128, 512], BF16), Dep("sq1"), sb("rq1", [128, 512], F32), Dep("rq1"), sb("qn1", [128, 512], BF16), Dep("qn1"),
                sb("t11", [128, 512], F32), Dep("t11"), sb("t21", [128, 512], F32), Dep("t21"))
        u = 0
        for j in range(NS):
            hb = j % 2
            P.dma(hts[hb][:, :, :], hT_d[j], writes=[d_hts[hb]], semkey=("hts", hb))
            for m in range(2):
                b = u % 2
                u += 1
                for kc in range(8):
                    mm(qp[b][:, :], Wk[:, kc, m * 128:(m + 1) * 128], hts[hb][:, kc, :], kc == 0, kc == 7,
                       [d_W, d_hts[hb]], [d_qp[b]])
                normrope(qp[b][:, :], d_qp[b], aux[b][:, :], d_aux[b], 1, j, kTd[:, m, j * 512:(j + 1) * 512], d_kT[m][j],
                         alt=(None if b == 0 else alt1))
            for i4 in range(4):
                t = j * 4 + i4
                vb = t % 2
                for kc in range(8):
                    mm(vp[vb][:, :], hts[hb][:, kc, i4 * 128:(i4 + 1) * 128], Wv[:, kc, :], kc == 0, kc == 7,
                       [d_W, d_hts[hb]], [d_vp[vb]])
                vv = vp[vb][:, :].rearrange("p (a b) -> p a b", a=2)
                cp("scalar", Va[:, t, :, 0:64], vv, [d_vp[vb], d_Vinit], [d_V[t]])
                cp("vector", Vb[:, t, :, 64:128], vv, [d_vp[vb], d_Vinit], [d_V[t]])
        P.barrier()
        P.emit()

    with ExitStack() as ph:
        sb = lambda name, shape, dt: ph.enter_context(nc.sbuf_tensor(uq(name), list(shape), dt))
        ps = lambda name, shape, dt: ph.enter_context(nc.psum_tensor(uq(name), list(shape), dt))
        ones_f = sb("ones_f", [128, 128], F32); d_of = Dep("ones_f")
        memset("gpsimd", ones_f[:, :], 1.0, [d_of])
        Wq = sb("Wq", [128, 8, 512], BF16); d_Wq = Dep("Wq")
        stg = [sb("stg%d" % i, [128, 8, 256], F32) for i in range(2)]; d_stg = [Dep("stg%d" % i) for i in range(2)]
        load_w(stg, d_stg, w_view[:, :, C_Q:C_Q + 512], 8, 512, lambda c0, n: Wq[:, :, c0:c0 + n], d_Wq, nw[:, :], d_nw)
        hts = [sb("hts%d" % i, [128, 8, 512], BF16) for i in range(2)]; d_hts = [Dep("hts%d" % i) for i in range(2)]
        qm = [sb("qm%d" % i, [128, 512], BF16) for i in range(2)]; d_qm = [Dep("qm%d" % i) for i in range(2)]
        sc = [ps("sc%d" % i, [128, 512], F32) for i in range(4)]; d_sc = [Dep("sc%d" % i) for i in range(4)]
        ov = [ps("ov%d" % i, [128, 512], F32) for i in range(2)]; d_ov = [Dep("ov%d" % i) for i in range(2)]
        aux = [ps("aux%d" % i, [128, 512], F32) for i in range(2)]; d_aux = [Dep("aux%d" % i) for i in range(2)]
        pT = [sb("pT%d" % i, [128, 512], BF16) for i in range(4)]; d_pT = [Dep("pT%d" % i) for i in range(4)]
        den = [sb("den%d" % i, [128, 512], F32) for i in range(2)]; d_den = [Dep("den%d" % i) for i in range(2)]
        rb = [sb("rb%d" % i, [128, 512], F32) for i in range(2)]; d_rb = [Dep("rb%d" % i) for i in range(2)]
        A_M = int(os.environ.get("ATT_M", "4")); A_J = int(os.environ.get("ATT_J", str(NS)))
        iters = [(m, j) for m in range(A_M) for j in range(A_J)]
        N_FILL = int(os.environ.get("ATT_FILL", "0"))

        ovs = [sb("ovs%d" % i, [128, 512], F32) for i in range(2)]; d_ovs = [Dep("ovs%d" % i) for i in range(2)]

        def q_stages(n):
            m, j = iters[n]
            hb = n % 2
            js = slice(j * 512, (j + 1) * 512)
            qp_ap, d_qp, aux_ap, d_ax = aux[0][:, :], d_aux[0], aux[1][:, :], d_aux[1]
            P.dma(hts[hb][:, :, :], hT_d[j], writes=[d_hts[hb]], semkey=("hts", hb))
            yield
            for kc in range(8):
                mm(qp_ap, Wq[:, kc, m * 128:(m + 1) * 128], hts[hb][:, kc, :], kc == 0, kc == 7, [d_Wq, d_hts[hb]], [d_qp])
            yield
            act(sq[:, :], qp_ap, AF.Square, [d_qp], [d_sq])
            yield
            mm(aux_ap, onesblk[:, :], sq[:, :], True, True, [d_ob, d_sq], [d_ax])
            yield
            act(rq[:, :], aux_ap, AF.Sqrt, [d_ax, d_eps], [d_rq], bias=epsb[:, 0:1], scale=1.0 / 64)
            yield
            recip(rq[:, :], rq[:, :], [d_rq], [d_rq])
            yield
            stt(qn[:, :], qp_ap, qkw[:, 0:1], rq[:, :], ALU.mult, ALU.mult, [d_qp, d_rq, d_qkw], [d_qn])
            yield
            mm(aux_ap, rmat[:, :], qn[:, :], True, True, [d_rm, d_qn], [d_ax])
            yield
            tt("vector", t1[:, :], qn[:, :], cosT[:, js], ALU.mult, [d_qn, d_cos], [d_t1])
            yield
            tt("vector", t2[:, :], aux_ap, sinT[:, js], ALU.mult, [d_ax, d_sin], [d_t2])
            yield
            tt("gpsimd", qm[n % 2][:, :], t1[:, :], t2[:, :], ALU.add, [d_t1, d_t2], [d_qm[n % 2]])
            yield

        def norm_stages(n):
            m, j = iters[n]
            qs = slice(j * 512, (j + 1) * 512)
            for hh in range(2):
                dr = 64 if hh == 0 else 0
                lo = 0 if hh == 0 else 64
                if hh == 0:
                    mm(aux[hh][0:64, :], ones_f[dr:dr + 1, 0:64], ovs[hh][dr:dr + 1, :], True, True, [d_of, d_ovs[hh]], [d_aux[hh]])
                else:
                    mm(aux[hh][:, :], ones_f[dr:dr + 1, :], ovs[hh][dr:dr + 1, :], True, True, [d_of, d_ovs[hh]], [d_aux[hh]])
                yield
                recip(rb[hh][lo:lo + 64, :], aux[hh][lo:lo + 64, :], [d_aux[hh]], [d_rb[hh]])
                yield
                tt("vector", attnT[lo:lo + 64, m, qs], ovs[hh][lo:lo + 64, :], rb[hh][lo:lo + 64, :], ALU.mult,
                   [d_ovs[hh], d_rb[hh]], [d_attn])
                yield

        def drain(gen):
            for _ in gen:
                pass

        drain(q_stages(0))

        def scores(n, kt):
            m, j = iters[n]
            qb = n % 2
            it = n * NT + kt
            ks = slice(kt * 128, (kt + 1) * 128)
            for hh in range(2):
                kv = (2 * m + hh) // 4
                off = hh * 64
                sbk = (it % 2) * 2 + hh
                mm(sc[sbk][:, :], kTd[off:off + 64, kv, ks], qm[qb][off:off + 64, :], True, True,
                   [d_kT[kv][kt // 4], d_qm[qb]], [d_sc[sbk]])
                act(pT[sbk][:, :], sc[sbk][:, :], AF.Exp, [d_sc[sbk]], [d_pT[sbk]], scale=0.125)

        def pv(n, kt):
            m, j = iters[n]
            it = n * NT + kt
            for hh in range(2):
                kv = (2 * m + hh) // 4
                sbk = (it % 2) * 2 + hh
                if hh == 0:
                    mm(ov[0][0:65, :], Va[:, kt, kv, 0:65], pT[sbk][:, :], kt == 0, kt == NT - 1,
                       [d_V[kt], d_pT[sbk]], [d_ov[0]])
                else:
                    mm(ov[1][:, :], Vb[:, kt, kv, :], pT[sbk][:, :], kt == 0, kt == NT - 1,
                       [d_V[kt], d_pT[sbk]], [d_ov[1]])

        for n, (m, j) in enumerate(iters):
            gens = []
            if n >= 1:
                gens.append(norm_stages(n - 1))
            if n + 1 < len(iters):
                gens.append(q_stages(n + 1))
            scores(n, 0)
            for kt in range(NT):
                if kt + 1 < NT:
                    scores(n, kt + 1)
                pv(n, kt)
                while gens:
                    try:
                        next(gens[0])
                        break
                    except StopIteration:
                        gens.pop(0)
            for g_ in gens:
                drain(g_)
            cp("vector", ovs[0][0:65, :], ov[0][0:65, :], [d_ov[0]], [d_ovs[0]])
            cp("vector", ovs[1][:, :], ov[1][:, :], [d_ov[1]], [d_ovs[1]])
        drain(norm_stages(len(iters) - 1))
        if "attnT" in dbg:
            da = dbgout("attnT", [128, 4, S], BF16)
            P.dma(da, attnT[:, :, :], reads=[d_attn], semkey="d0")
        P.barrier()
        P.emit()
    es23.close()
    if stop_after == 3:
        es35.close()
        return finish()


    with ExitStack() as ph:
        sb = lambda name, shape, dt: ph.enter_context(nc.sbuf_tensor(uq(name), list(shape), dt))
        ps = lambda name, shape, dt: ph.enter_context(nc.psum_tensor(uq(name), list(shape), dt))
        nssd = sb("s_nssd", [128, 4], F32); d_nssd = Dep("nssd")
        bg = sb("s_bg", [128, 16], F32); d_bg = Dep("bg")
        P.dma(nssd[:, :], nssd_d[:, :], writes=[d_nssd], semkey="c0")
        P.dma(bg[:, :], bg_d[:, :], writes=[d_bg], semkey="c1")
        Wg = sb("Wg", [128, 8, 2048], BF16); Wao = sb("Wao", [128, 4, 1024], BF16)
        Wso = sb("Wso", [128, 4, 1024], BF16); Wout = sb("Wout", [128, 8, 1024], BF16)
        d_W = Dep("W5")
        stg = [sb("stg%d" % i, [128, 8, 128], F32) for i in range(2)]; d_stg = [Dep("stg%d" % i) for i in range(2)]
        load_w(stg, d_stg, w_view[:, :, C_G:C_G + 2048], 8, 2048, lambda c0, n: Wg[:, :, c0:c0 + n], d_W, nw[:, :], d_nw, piece=128)
        load_w(stg, d_stg, w_ao_d.rearrange("(c p) n -> p c n", p=128), 4, 1024, lambda c0, n: Wao[:, :, c0:c0 + n], d_W, piece=128)
        load_w(stg, d_stg, w_so_d.rearrange("(c p) n -> p c n", p=128), 4, 1024, lambda c0, n: Wso[:, :, c0:c0 + n], d_W,
               nssd[:, :], d_nssd, piece=128)
        load_w(stg, d_stg, w_out_d.rearrange("(c p) n -> p c n", p=128), 8, 1024, lambda c0, n: Wout[:, :, c0:c0 + n], d_W, piece=128)
        hts = [sb("hts%d" % i, [128, 8, 512], BF16) for i in range(2)]; d_hts = [Dep("hts%d" % i) for i in range(2)]
        mg = [sb("mg%d" % i, [128, 8, 512], BF16) for i in range(2)]; d_mg = [Dep("mg%d" % i) for i in range(2)]
        xr = [sb("xr%d" % i, [128, D], F32) for i in range(2)]; d_xr = [Dep("xr%d" % i) for i in range(2)]
        sgA = [sb("sgA%d" % i, [128, 512], F32) for i in range(2)]; d_sgA = [Dep("sgA%d" % i) for i in range(2)]
        sgS = [sb("sgS%d" % i, [128, 512], F32) for i in range(2)]; d_sgS = [Dep("sgS%d" % i) for i in range(2)]
        m1 = sb("m1", [128, 512], F32); d_m1 = Dep("m1")
        m2 = sb("m2", [128, 512], F32); d_m2 = Dep("m2")
        pgA = [ps("pgA%d" % i, [128, 512], F32) for i in range(2)]; d_pgA = [Dep("pgA%d" % i) for i in range(2)]
        pgS = [ps("pgS%d" % i, [128, 512], F32) for i in range(2)]; d_pgS = [Dep("pgS%d" % i) for i in range(2)]
        pA = ps("pA", [128, 512], F32); d_pA = Dep("pA")
        pS = ps("pS", [128, 512], F32); d_pS = Dep("pS")
        po = [ps("po%d" % i, [128, 512], F32) for i in range(2)]; d_po = [Dep("po%d" % i) for i in range(2)]
        u = 0
        for j in range(NS):
            hb = j % 2
            js = slice(j * 512, (j + 1) * 512)
            P.dma(hts[hb][:, :, :], hT_d[j], writes=[d_hts[hb]], semkey=("hts", hb))
            def gates(mo):
                b = mo % 2
                for kc in range(8):
                    mm(pgA[b][:, :], Wg[:, kc, mo * 128:(mo + 1) * 128], hts[hb][:, kc, :], kc == 0, kc == 7,
                       [d_W, d_hts[hb]], [d_pgA[b]])
                for kc in range(8):
                    mm(pgS[b][:, :], Wg[:, kc, 1024 + mo * 128:1024 + (mo + 1) * 128], hts[hb][:, kc, :], kc == 0, kc == 7,
                       [d_W, d_hts[hb]], [d_pgS[b]])
                act(sgA[b][:, :], pgA[b][:, :], AF.Sigmoid, [d_pgA[b], d_bg], [d_sgA[b]], bias=bg[:, mo:mo + 1])
                act(sgS[b][:, :], pgS[b][:, :], AF.Sigmoid, [d_pgS[b], d_bg], [d_sgS[b]], bias=bg[:, 8 + mo:9 + mo])

            gates(0)
            for mo in range(8):
                b = mo % 2
                if mo + 1 < 8:
                    gates(mo + 1)
                for c in range(4):
                    mm(pA[:, :], Wao[:, c, mo * 128:(mo + 1) * 128], attnT[:, c, js], c == 0, c == 3, [d_W, d_attn], [d_pA])
                for c in range(4):
                    mm(pS[:, :], Wso[:, c, mo * 128:(mo + 1) * 128], ssdT[:, c, js], c == 0, c == 3, [d_W, d_ssdT], [d_pS])
                tt("vector", m1[:, :], sgA[b][:, :], pA[:, :], ALU.mult, [d_sgA[b], d_pA], [d_m1])
                tt("vector", m2[:, :], sgS[b][:, :], pS[:, :], ALU.mult, [d_sgS[b], d_pS], [d_m2])
                tt("gpsimd", mg[hb][:, mo, :], m1[:, :], m2[:, :], ALU.add, [d_m1, d_m2], [d_mg[hb]])
            for i4 in range(4):
                t = j * 4 + i4
                xb = t % 2
                P.dma(xr[xb][:, :], x_d[t * 128:(t + 1) * 128, :], writes=[d_xr[xb]], semkey=("xr", xb))
                for nh in range(2):
                    for kc in range(8):
                        mm(po[nh][:, :], mg[hb][:, kc, i4 * 128:(i4 + 1) * 128], Wout[:, kc, nh * 512:(nh + 1) * 512],
                           kc == 0, kc == 7, [d_W, d_mg[hb]], [d_po[nh]])
                    tt("vector", xr[xb][:, nh * 512:(nh + 1) * 512], xr[xb][:, nh * 512:(nh + 1) * 512], po[nh][:, :], ALU.add,
                       [d_xr[xb], d_po[nh]], [d_xr[xb]])
                P.dma(x1_d[t * 128:(t + 1) * 128, :], xr[xb][:, :], reads=[d_xr[xb]], semkey=("x1s", xb))
                if "x1" in dbg:
                    if t == 0:
                        dbg_x1 = dbgout("x1", [S, D], F32)
                    P.dma(dbg_x1[t * 128:(t + 1) * 128, :], xr[xb][:, :], reads=[d_xr[xb]], semkey=("x1d", xb))
        P.barrier()
        P.emit()
    es35.close()
    es_ssd.close()
    if stop_after == 5:
        return finish()

    NSLOT = 11
    RW = 1032
    sorted_d = dscr("sorted_h2", [NSLOT * 512, RW], BF16)
    sout_d = dscr("sorted_out", [NSLOT * 512, D], F32)
    nfbc_d = din("nf_bc", [128, D])
    sconst_d = din("sconst", [128, 32])

    def dma_fn(eng, fn, reads, writes, semkey, grp=None):
        return P._rec(eng, fn, list(reads), list(writes), is_dma=True, semkey=semkey, grp=grp)

    es6 = ExitStack()
    sb6 = lambda name, shape, dt: es6.enter_context(nc.sbuf_tensor(uq(name), list(shape), dt))
    widx1 = sb6("widx1", [128, NSLOT, 8], mybir.dt.int32)
    d_widx = Dep("widx")
    wc_d = din("wconst", [128, 48])
    pos_i = sb6("pos_i", [128, NT], mybir.dt.int32); d_pos = Dep("pos_i")
    gs_i = sb6("gs_i", [128, 16], mybir.dt.int32); d_gs = Dep("gs_i")
    d_sorted = Dep("sorted_d")

    with ExitStack() as ph:
        sb = lambda name, shape, dt: ph.enter_context(nc.sbuf_tensor(uq(name), list(shape), dt))
        ps = lambda name, shape, dt: ph.enter_context(nc.psum_tensor(uq(name), list(shape), dt))
        nfbc = sb("s_nfbc", [128, D], F32); d_nfbc = Dep("nfbc")
        brt = sb("s_br", [128, 20], F32); d_br = Dep("br")
        sconst = sb("s_sconst", [128, 32], F32); d_sc0 = Dep("sconst")
        masks = sb("s_masks6", [128, 4, 128], F32); d_mk = Dep("masks6")
        Wr = sb("Wr", [128, 8, 20], BF16); d_Wr = Dep("Wr")
        P.dma(nfbc[:, :], nfbc_d[:, :], writes=[d_nfbc], semkey="c0")
        P.dma(brt[:, :], br_d[:, :], writes=[d_br], semkey="c1")
        P.dma(sconst[:, :], sconst_d[:, :], writes=[d_sc0], semkey="c2")
        P.dma(masks[:, :, :], masks_d[:, :, :], writes=[d_mk], semkey="c3")
        P.dma(Wr[:, :, :], wr_d.rearrange("(kc p) n -> p kc n", p=128), writes=[d_Wr], semkey="wr_cast", eng="gpsimd")
        ones_f = sb("ones_f6", [128, 128], F32); d_of = Dep("ones_f6")
        memset("gpsimd", ones_f[:, :], 1.0, [d_of])
        xn_all = sb("xn_all", [128, NT, RW], BF16); d_xn = [Dep("xn_all%d" % t) for t in range(NT)]
        oh_all = sb("oh_all", [128, NT, 4], F32); d_oh = Dep("oh_all")
        x1t = [sb("x1t%d" % i, [128, D], F32) for i in range(3)]; d_x1t = [Dep("x1t%d" % i) for i in range(3)]
        junk = sb("junk6", [128, D], BF16); d_junk = Dep("junk6")
        h2t = [sb("h2t%d" % i, [128, 8, 128], BF16) for i in range(2)]; d_h2t = [Dep("h2t%d" % i) for i in range(2)]
        rt = sb("rt", [128, 96], F32); d_rt = Dep("rt")
        rt2 = sb("rt2", [128, NT, 2], F32); d_rt2 = [Dep("rt2_%d" % i) for i in range(NT)]
        Lall = sb("Lall", [128, NT, 20], F32); d_L = Dep("Lall"); d_rz = Dep("rz")
        gmax = sb("gmax", [128, NT], F32); gsum = sb("gsum", [128, NT], F32); m1c = sb("m1c", [128, NT], F32)
        m2c = sb("m2c", [128, NT], F32); esum = sb("esum", [128, NT], F32)
        eg = sb("eg", [128, NT, 4], F32); fs = sb("fs", [128, NT, 4], F32); fs2 = sb("fs2", [128, NT, 4], F32)
        mk1 = sb("mk1", [128, NT, 4], F32); mk2 = sb("mk2", [128, NT, 4], F32); ef = sb("ef", [128, NT, 4], F32)
        tmp16 = sb("tmp16", [128, NT, 4, 4], F32)
        tpp = [ps("tpp%d" % i, [128, 8, 128], BF16) for i in range(2)]; d_tpp = [Dep("tpp%d" % i) for i in range(2)]
        plg = [ps("plg%d" % i, [128, 32], F32) for i in range(2)]; d_plg = [Dep("plg%d" % i) for i in range(2)]
        def p6_stage1(t):
            xb = t % 3
            P.dma(x1t[xb][:, :], x1_d[t * 128:(t + 1) * 128, :], writes=[d_x1t[xb]], semkey=("x1t", xb))
            R = [d_rt2[t]]
            act(junk[:, :], x1t[xb][:, :], AF.Square, [d_x1t[xb]], [d_junk] + R, accum_out=rt2[:, t, 0:1])
            act(rt2[:, t, 1:2], rt2[:, t, 0:1], AF.Sqrt, R + [d_eps], R, bias=epsb[:, 0:1], scale=1.0 / D)
            recip(rt2[:, t, 1:2], rt2[:, t, 1:2], R, R)
            stt(xn_all[:, t, 0:D], x1t[xb][:, :], rt2[:, t, 1:2], nfbc[:, :], ALU.mult, ALU.mult, [d_x1t[xb], d_nfbc] + R, [d_xn[t]])

        def p6_stage2(t):
            nb = t % 2
            for c in range(8):
                tr(tpp[nb][:, c, :], xn_all[:, t, c * 128:(c + 1) * 128], ident[:, :], [d_xn[t], d_ident], [d_tpp[nb]])
            cp("scalar", h2t[nb][:, :, :], tpp[nb][:, :, :], [d_tpp[nb]], [d_h2t[nb]])
            for kc in range(8):
                mm(plg[nb][:, 0:20], h2t[nb][:, kc, :], Wr[:, kc, :], kc == 0, kc == 7, [d_h2t[nb], d_Wr], [d_plg[nb]])
            tt("vector", Lall[:, t, :], plg[nb][:, 0:20], brt[:, :], ALU.add, [d_plg[nb], d_br], [d_L])

        p6_stage1(0)
        for t in range(NT):
            if t + 1 < NT:
                p6_stage1(t + 1)
            p6_stage2(t)
        B3 = lambda ap, n: ap.unsqueeze(2).to_broadcast([128, NT, n])
        Lg = Lall[:, :, 0:4]
        Fv = Lall[:, :, 4:20].rearrange("p t (g k) -> p t g k", g=4)
        Z = [d_rz]
        red(gmax[:, :], Lg, ALU.max, [d_L], Z)
        tt("vector", eg[:, :, :], Lg, B3(gmax[:, :], 4), ALU.subtract, [d_L] + Z, Z)
        ts("vector", oh_all[:, :, :], eg[:, :, :], 0.0, None, ALU.is_equal, None, Z, [d_oh])
        act(eg[:, :, :], eg[:, :, :], AF.Exp, Z, Z)
        red(gsum[:, :], eg[:, :, :], ALU.add, Z, Z)
        tt("vector", tmp16[:, :, :, :], Fv, oh_all[:, :, :].unsqueeze(3).to_broadcast([128, NT, 4, 4]), ALU.mult, [d_L, d_oh] + Z, Z)
        red(fs[:, :, :], tmp16[:, :, :, :].rearrange("p t g k -> p t k g"), ALU.add, Z, Z)
        red(m1c[:, :], fs[:, :, :], ALU.max, Z, Z)
        tt("vector", fs[:, :, :], fs[:, :, :], B3(m1c[:, :], 4), ALU.subtract, Z, Z)
        ts("vector", mk1[:, :, :], fs[:, :, :], 0.0, None, ALU.is_equal, None, Z, Z)
        stt(fs2[:, :, :], mk1[:, :, :], -1.0e30, fs[:, :, :], ALU.mult, ALU.add, Z, Z)
        red(m2c[:, :], fs2[:, :, :], ALU.max, Z, Z)
        tt("vector", mk2[:, :, :], fs2[:, :, :], B3(m2c[:, :], 4), ALU.is_equal, Z, Z)
        tt("vector", mk1[:, :, :], mk1[:, :, :], mk2[:, :, :], ALU.add, Z, Z)
        act(ef[:, :, :], fs[:, :, :], AF.Exp, Z, Z)
        tt("vector", ef[:, :, :], ef[:, :, :], mk1[:, :, :], ALU.mult, Z, Z)
        red(esum[:, :], ef[:, :, :], ALU.add, Z, Z)
        tt("vector", esum[:, :], esum[:, :], gsum[:, :], ALU.mult, Z, Z)
        recip(esum[:, :], esum[:, :], Z, Z)
        tt("vector", xn_all[:, :, D:RW].bitcast(F32), ef[:, :, :], B3(esum[:, :], 4), ALU.mult, Z, d_xn)
        pcw = ps("pcw", [128, 128], F32); d_pcw = Dep("pcw")
        ptot = ps("ptot", [128, 128], F32); d_ptot = Dep("ptot")
        ohf = oh_all[:, :, :].rearrange("p t g -> p (t g)")
        mm(pcw[:, :], masks[:, 0, :], ohf, True, True, [d_mk, d_oh], [d_pcw])
        mm(ptot[:, :], ones_f[:, :], ohf, True, True, [d_of, d_oh], [d_ptot])
        tot = sb("tot", [128, NT, 4], F32); pre = sb("pre", [128, NT, 4], F32); Aa = sb("Aa", [128, NT, 4], F32)
        sm6 = sb("sm6", [128, 64], F32)
        d_q = Dep("posq")
        Q = [d_q]
        cp("vector", tot[:, :, :], ptot[:, :].rearrange("p (t g) -> p t g", g=4), [d_ptot], Q)
        memset("vector", pre[:, 0, :], 0.0, Q)
        for t in range(1, NT):
            tt("vector", pre[:, t, :], pre[:, t - 1, :], tot[:, t - 1, :], ALU.add, Q, Q)
        ng = sm6[:, 0:4]; cnt = sm6[:, 4:8]; pn = sm6[:, 8:12]; st = sm6[:, 12:16]; en = sm6[:, 16:20]; stm1 = sm6[:, 20:24]
        cmp8 = sm6[:, 24:32]; posf = sb("posf", [128, NT], F32); gsf = sm6[:, 32:48]; cmp11 = sm6[:, 48:64]
        tt("vector", ng, pre[:, NT - 1, :], tot[:, NT - 1, :], ALU.add, Q, Q)
        for g in range(4):
            ts("vector", cmp8, sconst[:, 16:24], ng[:, g:g + 1], None, ALU.is_lt, None, Q + [d_sc0], Q)
            red(cnt[:, g:g + 1], cmp8, ALU.add, Q, Q)
        ts("vector", pn, cnt, 512.0, None, ALU.mult, None, Q, Q)
        memset("vector", st[:, 0:1], 0.0, Q)
        for g in range(1, 4):
            tt("vector", st[:, g:g + 1], st[:, g - 1:g], pn[:, g - 1:g], ALU.add, Q, Q)
        tt("vector", en, st, pn, ALU.add, Q, Q)
        ts("vector", stm1, st, -1.0, None, ALU.add, None, Q, Q)
        tt("vector", Aa[:, :, :], pcw[:, :].rearrange("p (t g) -> p t g", g=4), pre[:, :, :], ALU.add, Q + [d_pcw], Q)
        tt("vector", Aa[:, :, :], Aa[:, :, :], stm1.unsqueeze(1).to_broadcast([128, NT, 4]), ALU.add, Q, Q)
        tt("vector", Aa[:, :, :], Aa[:, :, :], oh_all[:, :, :], ALU.mult, Q + [d_oh], Q)
        red(posf[:, :], Aa[:, :, :], ALU.add, Q, Q)
        cp("vector", pos_i[:, :], posf[:, :], Q, [d_pos])
        memset("vector", gsf, 0.0, Q)
        for g in range(3):
            ts("vector", cmp11, sconst[:, 0:16], en[:, g:g + 1], None, ALU.is_ge, None, Q + [d_sc0], Q)
            tt("vector", gsf, gsf, cmp11, ALU.add, Q, Q)
        cp("vector", gs_i[:, :], gsf, Q, [d_gs])
        wcst = sb("s_wconst", [128, 48], F32); d_wc = Dep("wconst")
        P.dma(wcst[:, :], wc_d[:, :], writes=[d_wc], semkey="c5")
        wf1 = sb("wf1", [128, NSLOT, 8], F32)
        g1 = sm6[:, 48:64]
        ts("vector", g1, gsf, 1024.0, None, ALU.mult, None, Q, Q)
        for s in range(NSLOT):
            ts("vector", wf1[:, s, :], wcst[:, 0:8], g1[:, s:s + 1], None, ALU.add, None, Q + [d_wc], Q)
        same = sb("same6", [128, 16], F32)
        memset("vector", same[:, 0:1], 0.0, Q)
        tt("vector", same[:, 1:NSLOT], gsf[:, 1:NSLOT], gsf[:, 0:NSLOT - 1], ALU.is_equal, Q, Q)
        ts("vector", same[:, 0:NSLOT], same[:, 0:NSLOT], 8192.0, None, ALU.mult, None, Q, Q)
        tt("vector", wf1[:, :, :], wf1[:, :, :], same[:, 0:NSLOT].unsqueeze(2).to_broadcast([128, NSLOT, 8]), ALU.add, Q, Q)
        cp("vector", widx1[:, :, :], wf1[:, :, :], Q, [d_widx])
        for t in range(NT):
            dma_fn("gpsimd", lambda e, t=t: e.indirect_dma_start(
                out=sorted_d[:, :], out_offset=bass.IndirectOffsetOnAxis(ap=pos_i[:, t:t + 1], axis=0),
                in_=xn_all[:, t, :], in_offset=None, bounds_check=None, oob_is_err=False),
                [d_xn[t], d_pos], [d_sorted], ("scat", t % 4))
        if "sort" in dbg:
            o1 = dbgout("pos", [128, NT], mybir.dt.int32); o2 = dbgout("gs", [128, 16], mybir.dt.int32)
            o3 = dbgout("xn_all", [128, NT, RW], BF16)
            P.dma(o1, pos_i[:, :], reads=[d_pos], semkey="d0")
            P.dma(o2, gs_i[:, :], reads=[d_gs], semkey="d1")
            P.dma(o3, xn_all[:, :, :], reads=d_xn, semkey="d2")
        P.barrier()
        P.emit()
    if stop_after == 61:
        es6.close()
        return finish()

    with ExitStack() as ph:
        sb = lambda name, shape, dt: ph.enter_context(nc.sbuf_tensor(uq(name), list(shape), dt))
        ps = lambda name, shape, dt: ph.enter_context(nc.psum_tensor(uq(name), list(shape), dt))
        NWB = 4
        W1s = [sb("W1s%d" % i, [128, 8, DE], BF16) for i in range(NWB)]
        W3s = [sb("W3s%d" % i, [128, 8, DE], BF16) for i in range(NWB)]
        W2s = [sb("W2s%d" % i, [128, 4, D], BF16) for i in range(NWB)]
        d_W1 = [Dep("W1s%d" % i) for i in range(NWB)]; d_W3 = [Dep("W3s%d" % i) for i in range(NWB)]
        d_W2 = [Dep("W2s%d" % i) for i in range(NWB)]
        xs = [sb("xs%d" % i, [128, 4, RW], BF16) for i in range(2)]; d_xs6 = [Dep("xs6_%d" % i) for i in range(2)]
        h2s = [sb("h2s%d" % i, [128, 8, 512], BF16) for i in range(2)]; d_h2s = [Dep("h2s%d" % i) for i in range(2)]
        gT = [sb("gT%d" % i, [128, 4, 512], BF16) for i in range(2)]; d_gT = [Dep("gT%d" % i) for i in range(2)]
        s1 = [sb("s1_%d" % i, [128, 512], F32) for i in range(2)]; d_s1 = [Dep("s1_%d" % i) for i in range(2)]
        yacc = [sb("yacc%d" % i, [128, 4, D], F32) for i in range(2)]; d_ya6 = [Dep("yacc%d" % i) for i in range(2)]
        tpp = ps("tpp6", [128, 8, 128], BF16); d_tpp = Dep("tpp6")
        ph1 = [ps("ph1_%d" % i, [128, 512], F32) for i in range(2)]; d_ph1 = [Dep("ph1_%d" % i) for i in range(2)]
        ph3 = [ps("ph3_%d" % i, [128, 512], F32) for i in range(2)]; d_ph3 = [Dep("ph3_%d" % i) for i in range(2)]
        py = [ps("py%d" % i, [128, 512], F32) for i in range(2)]; d_py = [Dep("py%d" % i) for i in range(2)]
        d_sout = Dep("sout")

        grp_ctr = [0]
        bnd_cache = {}

        def wload(s, ee, wb):
            for (rows, dst, nchunk, ncol, dd, key) in ((w1_d, W1s[wb], 8, DE, d_W1[wb], "w1"),
                                                       (w3_d, W3s[wb], 8, DE, d_W3[wb], "w3"),
                                                       (w2_d, W2s[wb], 4, D, d_W2[wb], "w2")):
                grp_ctr[0] += 1
                gid = grp_ctr[0]
                hc = nchunk // 2
                for hf in range(2):
                    def fn(e, rows=rows, dst=dst, hf=hf, hc=hc, ncol=ncol):
                        if "bval" not in bnd_cache:
                            rg = e.alloc_register("wbound")
                            e.reg_mov(rg, NE * 128 * 2 - 1)
                            bnd_cache["bval"] = e.snap(rg)
                        return e.indirect_dma_start(
                            out=dst[:, hf * hc:(hf + 1) * hc, :].rearrange("p c n -> p (c n)"), out_offset=None,
                            in_=rows[:, :],
                            in_offset=bass.IndirectOffsetOnAxis(ap=widx1[:, s, ee * 2 + hf:ee * 2 + hf + 1], axis=0),
                            bounds_check=bnd_cache["bval"], oob_is_err=False)
                    dma_fn("gpsimd", fn, [d_widx], [dd], (key, wb), grp=gid)

        NSL = int(os.environ.get("MOE_NSLOT", str(NSLOT)))

        def slot_rows(s2):
            for i4 in range(4):
                r0 = (s2 * 4 + i4) * 128
                P.dma(xs[s2 % 2][:, i4, :], sorted_d[r0:r0 + 128, :], reads=[d_sorted], writes=[d_xs6[s2 % 2]], semkey=("xs6", i4))

        def slot_tr(s2, i4):
            for c in range(8):
                tr(tpp[:, c, :], xs[s2 % 2][:, i4, c * 128:(c + 1) * 128], ident[:, :], [d_xs6[s2 % 2], d_ident], [d_tpp])
            cp("scalar", h2s[s2 % 2][:, :, i4 * 128:(i4 + 1) * 128], tpp[:, :, :], [d_tpp], [d_h2s[s2 % 2]])

        work = [(s, ee) for s in range(NSL) for ee in range(4)]
        for ee0 in range(4):
            wload(0, ee0, ee0)
        uu = 0
        for n, (s, ee) in enumerate(work):
            if n >= 1:
                ps_, pe_ = work[n - 1]
                if ps_ + 1 < NSL:
                    wload(ps_ + 1, pe_, pe_)
            wb = ee
            sb_ = s % 2
            if n == 0:
                slot_rows(0)
                for i4 in range(4):
                    slot_tr(0, i4)
            if s + 1 < NSL:
                if ee == 0:
                    slot_rows(s + 1)
                else:
                    slot_tr(s + 1, ee - 1)
                    if ee == 3:
                        slot_tr(s + 1, 3)
            gb = n % 2
            for mc in range(4):
                b = uu % 2
                uu += 1
                for kc in range(8):
                    mm(ph1[b][:, :], W1s[wb][:, kc, mc * 128:(mc + 1) * 128], h2s[sb_][:, kc, :], kc == 0, kc == 7,
                       [d_W1[wb], d_h2s[sb_]], [d_ph1[b]])
                for kc in range(8):
                    mm(ph3[b][:, :], W3s[wb][:, kc, mc * 128:(mc + 1) * 128], h2s[sb_][:, kc, :], kc == 0, kc == 7,
                       [d_W3[wb], d_h2s[sb_]], [d_ph3[b]])
                act(s1[b][:, :], ph1[b][:, :], AF.Silu, [d_ph1[b]], [d_s1[b]])
                tt("vector", gT[gb][:, mc, :], s1[b][:, :], ph3[b][:, :], ALU.mult, [d_s1[b], d_ph3[b]], [d_gT[gb]])
            for i4 in range(4):
                wcol = xs[sb_][:, i4, D:RW].bitcast(F32)[:, ee:ee + 1]
                for nh in range(2):
                    for mc in range(4):
                        mm(py[nh][:, :], gT[gb][:, mc, i4 * 128:(i4 + 1) * 128], W2s[wb][:, mc, nh * 512:(nh + 1) * 512],
                           mc == 0, mc == 3, [d_gT[gb], d_W2[wb]], [d_py[nh]])
                    ysl = yacc[sb_][:, i4, nh * 512:(nh + 1) * 512]
                    if ee == 0:
                        ts("vector", ysl, py[nh][:, :], wcol, None, ALU.mult, None, [d_py[nh], d_xs6[sb_]], [d_ya6[sb_]])
                    else:
                        stt(ysl, py[nh][:, :], wcol, ysl, ALU.mult, ALU.add, [d_py[nh], d_xs6[sb_], d_ya6[sb_]], [d_ya6[sb_]])
            if ee == 3:
                for i4 in range(4):
                    r0 = (s * 4 + i4) * 128
                    P.dma(sout_d[r0:r0 + 128, :], yacc[sb_][:, i4, :], reads=[d_ya6[sb_]], writes=[d_sout], semkey=("ys6", i4))
        P.barrier()
        P.emit()

    with ExitStack() as ph:
        sb = lambda name, shape, dt: ph.enter_context(nc.sbuf_tensor(uq(name), list(shape), dt))
        NB6 = 6
        yt = [sb("yt%d" % i, [128, D], F32) for i in range(NB6)]; d_yt = [Dep("yt%d" % i) for i in range(NB6)]
        x1r = [sb("x1r%d" % i, [128, D], F32) for i in range(NB6)]; d_x1r = [Dep("x1r%d" % i) for i in range(NB6)]
        for t in range(NT):
            b = t % NB6
            dma_fn("gpsimd", lambda e, t=t, b=b: e.indirect_dma_start(
                out=yt[b][:, :], out_offset=None, in_=sout_d[:, :],
                in_offset=bass.IndirectOffsetOnAxis(ap=pos_i[:, t:t + 1], axis=0),
                bounds_check=None, oob_is_err=False), [d_pos], [d_yt[b]], ("gat", b))
            P.dma(x1r[b][:, :], x1_d[t * 128:(t + 1) * 128, :], writes=[d_x1r[b]], semkey=("x1r", b))
            tt("vector", x1r[b][:, :], x1r[b][:, :], yt[b][:, :], ALU.add, [d_x1r[b], d_yt[b]], [d_x1r[b]])
            P.dma(out_d[t * 128:(t + 1) * 128, :], x1r[b][:, :], reads=[d_x1r[b]], semkey=("outs", b), eng="scalar")
        P.barrier()
        P.emit()
    es6.close()

    return finish()


_CACHE = {}


def _consts():
    bf = ml_dtypes.bfloat16
    c = {}
    c["ident_bf"] = np.eye(128, dtype=np.float32).astype(bf)
    ob = np.zeros((128, 128), np.float32)
    ob[:64, :64] = 1.0
    ob[64:, 64:] = 1.0
    c["onesblk"] = ob.astype(bf)
    rm = np.zeros((128, 128), np.float32)
    for blk in range(4):
        base = blk * 32
        for i in range(16):
            rm[base + i + 16, base + i] = -1.0
            rm[base + i, base + i + 16] = 1.0
    c["rmat"] = rm.astype(bf)
    t = np.arange(S)
    row = (t // 64).astype(np.float32)
    col = (t % 64).astype(np.float32)
    inv = (np.float32(10000.0) ** (-(np.arange(0, 32, 2, dtype=np.float32)) / np.float32(32))).astype(np.float32)
    ar = row[:, None] * inv[None, :]
    ac = col[:, None] * inv[None, :]
    cos = np.concatenate([np.cos(ar), np.cos(ar), np.cos(ac), np.cos(ac)], -1).astype(np.float32)
    sin = np.concatenate([np.sin(ar), np.sin(ar), np.sin(ac), np.sin(ac)], -1).astype(np.float32)
    c["cosT"] = np.ascontiguousarray(np.concatenate([cos.T, cos.T], 0)).astype(bf)
    c["sinT"] = np.ascontiguousarray(np.concatenate([sin.T, sin.T], 0)).astype(bf)
    r = np.arange(128)[:, None]
    q = np.arange(128)[None, :]
    mk = np.stack([(r <= q) + 0 * q, (r > q) + 0 * q, (r >= q) + 0 * q, (r < q) + 0 * q], axis=1).astype(np.float32)
    c["masks"] = np.ascontiguousarray(mk)
    sc = np.zeros((128, 32), np.float32)
    sc[:, 0:16] = 512.0 * np.arange(16)[None, :]
    sc[:, 16:24] = 512.0 * np.arange(8)[None, :]
    c["sconst"] = sc
    wc = np.zeros((128, 48), np.float32)
    for ee in range(4):
        for hf in range(2):
            wc[:, ee * 2 + hf] = (ee * 128 + np.arange(128)) * 2 + hf
    c["wconst"] = wc
    return c


def _layout_inputs(inputs, b):
    f = lambda k: np.ascontiguousarray(np.asarray(inputs[k], dtype=np.float32)[0])
    bc = lambda v: np.ascontiguousarray(np.broadcast_to(v.reshape(1, -1), (128, v.size)))
    m = {}
    m["x"] = np.ascontiguousarray(np.asarray(inputs["x"], dtype=np.float32)[b])
    m["w_in"] = f("w_in")
    m["nw_mix"] = np.ascontiguousarray(f("norm_mix_w").reshape(8, 128).T)
    qw = f("q_norm_w")
    kw = f("k_norm_w")
    m["qkw"] = np.ascontiguousarray(np.stack([np.tile(qw, 2), np.tile(kw, 2)], axis=1))
    cw = f("conv_w")
    m["cw"] = np.ascontiguousarray(cw.T.reshape(6, 128, 7).transpose(1, 0, 2).reshape(128, 42))
    m["cbias"] = np.ascontiguousarray(f("conv_b").reshape(6, 128).T)
    m["dtb"] = bc(f("dt_bias").reshape(-1))
    m["alog"] = bc(f("a_log").reshape(-1))
    m["dsk"] = bc(f("d_skip").reshape(-1))
    m["nssd"] = np.ascontiguousarray(f("ssd_norm_w").reshape(4, 128).T)
    m["w_attn_o"] = f("w_attn_o")
    m["w_ssd_o"] = f("w_ssd_o")
    m["w_out"] = f("w_out")
    m["bgate"] = np.ascontiguousarray(f("b_gate").reshape(16, 128).T)
    m["nffn"] = np.ascontiguousarray(f("norm_ffn_w").reshape(8, 128).T)
    m["wr"] = np.ascontiguousarray(np.concatenate([f("w_router_group"), f("w_router_expert")], axis=1))
    m["br"] = bc(np.concatenate([f("b_router_group"), f("b_router_expert")]))
    m["nf_bc"] = bc(f("norm_ffn_w"))
    m["w1"] = np.ascontiguousarray(f("w1").reshape(NE, 8, 128, DE).transpose(0, 2, 1, 3)).reshape(NE * 128 * 2, 2048)
    m["w3"] = np.ascontiguousarray(f("w3").reshape(NE, 8, 128, DE).transpose(0, 2, 1, 3)).reshape(NE * 128 * 2, 2048)
    m["w2"] = np.ascontiguousarray(f("w2").reshape(NE, 4, 128, D).transpose(0, 2, 1, 3)).reshape(NE * 128 * 2, 2048)
    return m


def kernel(**inputs):
    B = np.asarray(inputs["x"]).shape[0]
    if "nc" not in _CACHE:
        _CACHE["nc"] = build_program()
    nc = _CACHE["nc"]
    cst = _consts()
    in_maps = []
    for b in range(B):
        m = _layout_inputs(inputs, b)
        m.update(cst)
        in_maps.append(m)
    res = run_bass_kernel_spmd(nc, in_maps, core_ids=list(range(B)))
    return np.stack([np.asarray(r["out"]) for r in res.results], axis=0)
```

```python
import os
from contextlib import ExitStack

import numpy as np
import ml_dtypes

import concourse.bass as bass
import concourse.mybir as mybir
from concourse.bass_utils import run_bass_kernel_spmd

F32 = mybir.dt.float32
BF16 = mybir.dt.bfloat16
AF = mybir.ActivationFunctionType
ALU = mybir.AluOpType
AX = mybir.AxisListType

S = 4096
D = 1024
NT = S // 128
NS = S // 512
EPS = 1e-6
INP = 4112
C_Q, C_K, C_V, C_Z, C_XBC, C_DT, C_G = 0, 512, 640, 768, 1280, 2048, 2064
NE = 16
DE = 512


class Dep:
    __slots__ = ("name", "last_w", "readers", "epoch")

    def __init__(self, name):
        self.name = name
        self.last_w = None
        self.readers = []
        self.epoch = -1


class Instr:
    __slots__ = ("eng", "fn", "deps", "is_dma", "sem", "val", "signal", "idx", "pos", "grp")


ENGS = ("sync", "scalar", "vector", "gpsimd", "tensor")


class Prog:
    def __init__(self, nc):
        self.nc = nc
        self.es = ExitStack()
        self.eng_sem = {}
        for e in ENGS:
            self.eng_sem[e] = self.es.enter_context(nc.semaphore("tick_" + e))
        self.eng_tick = {e: 0 for e in ENGS}
        self.dma_sems = {}
        self.instrs = []
        self.known = {e: {} for e in ENGS}
        self.n_total = 0
        self.epoch = 0

    def close(self):
        self.es.close()

    def _rec(self, eng, fn, reads, writes, is_dma=False, semkey=None, grp=None):
        it = Instr()
        it.grp = grp
        it.eng = eng
        it.fn = fn
        it.is_dma = is_dma
        it.signal = False
        it.sem = None
        it.val = None
        it.idx = len(self.instrs)
        for dd in list(reads) + list(writes):
            if dd.epoch != self.epoch:
                dd.epoch = self.epoch
                dd.last_w = None
                dd.readers = []
        deps = set()
        for r in reads:
            if r.last_w is not None:
                deps.add(r.last_w)
        for w in writes:
            if w.last_w is not None:
                if not (grp is not None and self.instrs[w.last_w].grp == grp):
                    deps.add(w.last_w)
            deps.update(w.readers)
        for r in reads:
            r.readers.append(it.idx)
        for w in writes:
            w.last_w = it.idx
            w.readers = []
        if is_dma:
            ent = self.dma_sems.get(semkey)
            if ent is None:
                sem = self.es.enter_context(self.nc.semaphore("dma_%d" % len(self.dma_sems)))
                ent = [sem, 0, None, -1]
                self.dma_sems[semkey] = ent
            if ent[3] == self.epoch and ent[2] is not None:
                if not (grp is not None and self.instrs[ent[2]].grp == grp):
                    deps.add(ent[2])
            ent[1] += 1
            ent[2] = it.idx
            ent[3] = self.epoch
            it.sem = ent[0]
            it.val = 16 * ent[1]
        deps.discard(it.idx)
        it.deps = deps
        self.instrs.append(it)
        return it

    def op(self, eng, fn, reads=(), writes=()):
        return self._rec(eng, fn, list(reads), list(writes))

    def dma(self, out, in_, reads=(), writes=(), semkey=None, eng="sync"):
        assert semkey is not None
        return self._rec(eng, lambda e: e.dma_start(out=out, in_=in_), list(reads), list(writes),
                         is_dma=True, semkey=semkey)

    def barrier(self):
        last = {}
        pend = set()
        for it in self.instrs:
            if it.is_dma:
                pend.add(it.idx)
            else:
                last[it.eng] = it.idx
        allidx = set(last.values()) | pend
        for e in ENGS:
            it = self._rec(e, lambda eng: eng.nop(), [], [])
            it.deps = set(allidx)

    def emit(self):
        instrs = self.instrs
        per_eng = {e: [] for e in ENGS}
        for it in instrs:
            it.pos = len(per_eng[it.eng])
            per_eng[it.eng].append(it)

        def skipped(src, it):
            if src.is_dma or it.is_dma:
                return False
            if src.eng != it.eng:
                return False
            if src.eng == "tensor":
                return True
            return it.pos - src.pos > 2

        for it in instrs:
            for d in it.deps:
                src = instrs[d]
                if src.is_dma or skipped(src, it):
                    continue
                src.signal = True
        for e in ENGS:
            t = self.eng_tick[e]
            for it in per_eng[e]:
                if not it.is_dma and it.signal:
                    t += 1
                    it.sem = self.eng_sem[e]
                    it.val = t
            self.eng_tick[e] = t
        known = self.known

        def run_engine(ename, eobj):
            kn = known[ename]
            for it in per_eng[ename]:
                need = {}
                for d in it.deps:
                    src = instrs[d]
                    if skipped(src, it):
                        continue
                    key = src.sem.name
                    if kn.get(key, 0) >= src.val:
                        continue
                    if key not in need or need[key][1] < src.val:
                        need[key] = (src.sem, src.val)
                for key, (sem, val) in need.items():
                    eobj.wait_ge(sem, val)
                    kn[key] = val
                bi = it.fn(eobj)
                if it.is_dma:
                    bi.then_inc(it.sem, 16)
                elif it.signal:
                    bi.then_inc(it.sem, 1)

        with self.nc.Block() as block:
            @block.sync
            def _(e):
                run_engine("sync", e)

            @block.scalar
            def _(e):
                run_engine("scalar", e)

            @block.vector
            def _(e):
                run_engine("vector", e)

            @block.gpsimd
            def _(e):
                run_engine("gpsimd", e)

            @block.tensor
            def _(e):
                run_engine("tensor", e)
        self.n_total += len(instrs)
        self.instrs = []
        self.epoch += 1


def build_program(dbg=None, stop_after=None):
    nc = bass.Bass("TRN2", target_bir_lowering=False)
    P = Prog(nc)
    dbg = dbg or []
    _uq = [0]

    def uq(name):
        _uq[0] += 1
        return "%s_%d" % (name, _uq[0])

    def din(name, shape, dt=F32):
        return nc.dram_tensor(name, list(shape), dt, kind="ExternalInput").ap()

    def dscr(name, shape, dt):
        return nc.dram_tensor(name, list(shape), dt, kind="Internal").ap()

    def dout(name, shape, dt):
        return nc.dram_tensor(name, list(shape), dt, kind="ExternalOutput").ap()

    x_d = din("x", [S, D])
    w_in_d = din("w_in", [D, INP])
    out_d = dout("out", [S, D], F32)
    ident_bf_d = din("ident_bf", [128, 128], BF16)
    nw_mix_d = din("nw_mix", [128, 8])
    qkw_d = din("qkw", [128, 2])
    onesblk_d = din("onesblk", [128, 128], BF16)
    rmat_d = din("rmat", [128, 128], BF16)
    cosT_d = din("cosT", [128, S], BF16)
    sinT_d = din("sinT", [128, S], BF16)
    masks_d = din("masks", [128, 4, 128])
    cw_d = din("cw", [128, 42])
    cb_d = din("cbias", [128, 6])
    dtb_d = din("dtb", [128, 16])
    alog_d = din("alog", [128, 16])
    dsk_d = din("dsk", [128, 8])
    nssd_d = din("nssd", [128, 4])
    w_ao_d = din("w_attn_o", [512, D])
    w_so_d = din("w_ssd_o", [512, D])
    w_out_d = din("w_out", [D, D])
    bg_d = din("bgate", [128, 16])
    nf_d = din("nffn", [128, 8])
    wr_d = din("wr", [D, 20])
    br_d = din("br", [128, 20])
    w1_d = din("w1", [NE * 128 * 2, 2048])
    w3_d = din("w3", [NE * 128 * 2, 2048])
    w2_d = din("w2", [NE * 128 * 2, 2048])

    hT_d = dscr("hT_scratch", [NS, 128, 8, 512], BF16)
    x1_d = dscr("x1_scratch", [S, D], F32)

    def dbgout(name, shape, dt):
        return dout("dbg_" + name, shape, dt)

    def mm(out, lhsT, rhs, start, stop, reads, writes):
        P.op("tensor", lambda e: e.matmul(out, lhsT=lhsT, rhs=rhs, start=start, stop=stop), reads, writes)

    def tr(out, in_, idn, reads, writes):
        P.op("tensor", lambda e: e.transpose(out=out, in_=in_, identity=idn), reads, writes)

    def act(out, in_, func, reads, writes, **kw):
        P.op("scalar", lambda e: e.activation(out=out, in_=in_, func=func, **kw), reads, writes)

    def tt(eng, out, in0, in1, op, reads, writes):
        P.op(eng, lambda e: e.tensor_tensor(out=out, in0=in0, in1=in1, op=op), reads, writes)

    def ts(eng, out, in0, s1, s2, op0, op1, reads, writes):
        if op1 is None:
            P.op(eng, lambda e: e.tensor_scalar(out=out, in0=in0, scalar1=s1, scalar2=None, op0=op0), reads, writes)
        else:
            P.op(eng, lambda e: e.tensor_scalar(out=out, in0=in0, scalar1=s1, scalar2=s2, op0=op0, op1=op1), reads, writes)

    def stt(out, in0, scalar, in1, op0, op1, reads, writes):
        P.op("vector", lambda e: e.scalar_tensor_tensor(out=out, in0=in0, scalar=scalar, in1=in1, op0=op0, op1=op1),
             reads, writes)

    def cp(eng, out, in_, reads, writes):
        if eng == "scalar":
            P.op(eng, lambda e: e.copy(out=out, in_=in_), reads, writes)
        else:
            P.op(eng, lambda e: e.tensor_copy(out=out, in_=in_), reads, writes)

    def recip(out, in_, reads, writes):
        P.op("vector", lambda e: e.reciprocal(out=out, in_=in_), reads, writes)

    def memset(eng, ap, val, writes):
        P.op(eng, lambda e: e.memset(ap, val), [], writes)

    def red(out, in_, op, reads, writes):
        P.op("vector", lambda e: e.tensor_reduce(out=out, in_=in_, axis=AX.X, op=op), reads, writes)

    stg_ctr = [0]

    def load_w(stg, d_stg, src_view, KC, ncols, dst_fn, d_dst, scale=None, d_scale=None, piece=256, eng="vector"):
        for c0 in range(0, ncols, piece):
            n = min(piece, ncols - c0)
            sl = stg_ctr[0] % len(stg)
            stg_ctr[0] += 1
            P.dma(stg[sl][:, 0:KC, 0:n], src_view[:, :, c0:c0 + n], writes=[d_stg[sl]], semkey=("stg", sl))
            if scale is not None:
                tt(eng, dst_fn(c0, n), stg[sl][:, 0:KC, 0:n], scale.unsqueeze(2).to_broadcast([128, KC, n]), ALU.mult,
                   [d_stg[sl], d_scale], [d_dst])
            else:
                cp(eng, dst_fn(c0, n), stg[sl][:, 0:KC, 0:n], [d_stg[sl]], [d_dst])

    w_view = w_in_d.rearrange("(kc p) n -> p kc n", p=128)

    es_top = ExitStack()
    sbT = lambda name, shape, dt: es_top.enter_context(nc.sbuf_tensor(uq(name), list(shape), dt))
    ident = sbT("ident", [128, 128], BF16)
    d_ident = Dep("ident")
    nw = sbT("s_nw", [128, 8], F32)
    d_nw = Dep("nw")
    epsb = sbT("epsb", [128, 1], F32)
    d_eps = Dep("eps")
    es_ssd = ExitStack()
    ssdT = es_ssd.enter_context(nc.sbuf_tensor(uq("ssdT"), [128, 4, S], BF16))
    d_ssdT = Dep("ssdT")

    def finish():
        es_ssd.close()
        es_top.close()
        P.close()
        return nc

    with ExitStack() as ph:
        sb = lambda name, shape, dt: ph.enter_context(nc.sbuf_tensor(uq(name), list(shape), dt))
        ps = lambda name, shape, dt: ph.enter_context(nc.psum_tensor(uq(name), list(shape), dt))
        P.dma(ident[:, :], ident_bf_d[:, :], writes=[d_ident], semkey="c0")
        P.dma(nw[:, :], nw_mix_d[:, :], writes=[d_nw], semkey="c1")
        memset("gpsimd", epsb[:, :], EPS, [d_eps])
        NXB = 6
        xt = [sb("xt%d" % i, [128, D], F32) for i in range(NXB)]
        d_xt = [Dep("xt%d" % i) for i in range(NXB)]
        junk = sb("junk", [128, D], BF16)
        d_junk = Dep("junk")
        xn = [sb("xn%d" % i, [128, D], BF16) for i in range(2)]
        d_xn = [Dep("xn%d" % i) for i in range(2)]
        ss = sb("ss", [128, NT], F32)
        rs = sb("rs", [128, NT], F32)
        d_ss = [Dep("ss%d" % i) for i in range(NT)]
        d_rs = [Dep("rs%d" % i) for i in range(NT)]
        tp = [ps("tp%d" % i, [128, 8, 128], BF16) for i in range(2)]
        d_tp = [Dep("tp%d" % i) for i in range(2)]
        hs = [sb("hs%d" % i, [128, 8, 512], BF16) for i in range(2)]
        d_hs = [Dep("hs%d" % i) for i in range(2)]
        def p1_load(t):
            xb = t % NXB
            P.dma(xt[xb][:, :], x_d[t * 128:(t + 1) * 128, :], writes=[d_xt[xb]], semkey=("xt", xb))

        def p1_stage1(t):
            xb = t % NXB
            act(junk[:, :], xt[xb][:, :], AF.Square, [d_xt[xb]], [d_junk, d_ss[t]], accum_out=ss[:, t:t + 1])
            act(rs[:, t:t + 1], ss[:, t:t + 1], AF.Sqrt, [d_ss[t], d_eps], [d_rs[t]], bias=epsb[:, 0:1], scale=1.0 / D)
            recip(rs[:, t:t + 1], rs[:, t:t + 1], [d_rs[t]], [d_rs[t]])
            nb = t % 2
            ts("vector", xn[nb][:, :], xt[xb][:, :], rs[:, t:t + 1], None, ALU.mult, None, [d_xt[xb], d_rs[t]], [d_xn[nb]])

        def p1_stage2(t):
            j, i4 = divmod(t, 4)
            nb = t % 2
            for c in range(8):
                tr(tp[nb][:, c, :], xn[nb][:, c * 128:(c + 1) * 128], ident[:, :], [d_xn[nb], d_ident], [d_tp[nb]])
            hb = j % 2
            cp("scalar", hs[hb][:, :, i4 * 128:(i4 + 1) * 128], tp[nb][:, :, :], [d_tp[nb]], [d_hs[hb]])
            if i4 == 3:
                P.dma(hT_d[j], hs[hb][:, :, :], reads=[d_hs[hb]], semkey=("hs", hb))

        LA = 4
        for t in range(LA):
            p1_load(t)
        p1_stage1(0)
        for t in range(NT):
            if t + LA < NT:
                p1_load(t + LA)
            if t + 1 < NT:
                p1_stage1(t + 1)
            p1_stage2(t)
        P.barrier()
        P.emit()
    if stop_after == 1:
        return finish()

    es4 = ExitStack()
    sb4 = lambda name, shape, dt: es4.enter_context(nc.sbuf_tensor(uq(name), list(shape), dt))
    xs_tok = sb4("xs_tok", [128, NT, 512], BF16)
    B_tok = sb4("B_tok", [128, NT, 128], BF16)
    BT = sb4("BT", [128, S], BF16)
    CTz = sb4("CTz", [128, 2, S], BF16)
    dtv = sb4("dtv", [128, NT, 16], F32)
    dAv = sb4("dAv", [128, NT, 16], F32)
    d_xs = [Dep("xs%d" % t) for t in range(NT)]
    d_Bt = [Dep("Bt%d" % t) for t in range(NT)]
    d_BT = [Dep("BT%d" % j) for j in range(NS)]
    d_CT = [Dep("CT%d" % j) for j in range(NS)]
    d_dt = [Dep("dt%d" % t) for t in range(NT)]
    d_CTinit = Dep("CTinit")

    with ExitStack() as ph:
        sb = lambda name, shape, dt: ph.enter_context(nc.sbuf_tensor(uq(name), list(shape), dt))
        ps = lambda name, shape, dt: ph.enter_context(nc.psum_tensor(uq(name), list(shape), dt))
        cw = sb("s_cw", [128, 42], F32); d_cw = Dep("cw")
        cbias = sb("s_cb", [128, 6], F32); d_cb = Dep("cb")
        dtb = sb("s_dtb", [128, 16], F32); d_dtb = Dep("dtb")
        aneg = sb("s_aneg", [128, 16], F32); d_an = Dep("aneg")
        P.dma(cw[:, :], cw_d[:, :], writes=[d_cw], semkey="c0")
        P.dma(cbias[:, :], cb_d[:, :], writes=[d_cb], semkey="c1")
        P.dma(dtb[:, :], dtb_d[:, :], writes=[d_dtb], semkey="c2")
        P.dma(aneg[:, :], alog_d[:, :], writes=[d_an], semkey="c3")
        act(aneg[:, :], aneg[:, :], AF.Exp, [d_an], [d_an])
        ts("vector", aneg[:, :], aneg[:, :], -1.0, None, ALU.mult, None, [d_an], [d_an])
        diagw = sb("diagw", [128, 42, 128], BF16); d_dg = Dep("diagw")
        for i in range(42):
            ts("vector", diagw[:, i, :], ident[:, :], cw[:, i:i + 1], None, ALU.mult, None, [d_ident, d_cw], [d_dg])
        memset("gpsimd", CTz[:, :, :], 0.0, [d_CTinit])
        Wx = sb("Wx", [128, 8, 768], BF16); Wd = sb("Wd", [128, 8, 16], BF16); d_W = Dep("W4a")
        stg = [sb("stg%d" % i, [128, 8, 256], F32) for i in range(2)]; d_stg = [Dep("stg%d" % i) for i in range(2)]
        load_w(stg, d_stg, w_view[:, :, C_XBC:C_XBC + 768], 8, 768, lambda c0, n: Wx[:, :, c0:c0 + n], d_W, nw[:, :], d_nw)
        load_w(stg, d_stg, w_view[:, :, C_DT:C_DT + 16], 8, 16, lambda c0, n: Wd[:, :, c0:c0 + n], d_W, nw[:, :], d_nw)
        xraw = sb("xraw", [128, 6, S + 8], BF16)
        d_xr = [Dep("xr%d" % j) for j in range(NS)]
        d_xpad = Dep("xpad")
        memset("gpsimd", xraw[:, :, 0:3], 0.0, [d_xpad])
        memset("gpsimd", xraw[:, :, S + 3:S + 8], 0.0, [d_xpad])
        hts = [sb("hts%d" % i, [128, 8, 512], BF16) for i in range(2)]; d_hts = [Dep("hts%d" % i) for i in range(2)]
        pp = [ps("pp%d" % i, [128, 512], F32) for i in range(2)]; d_pp = [Dep("pp%d" % i) for i in range(2)]
        pd = [ps("pd%d" % i, [128, 16], F32) for i in range(2)]; d_pd = [Dep("pd%d" % i) for i in range(2)]
        ptr = [ps("ptr%d" % i, [128, 4, 128], BF16) for i in range(2)]; d_ptr = [Dep("ptr%d" % i) for i in range(2)]
        cvo = [sb("cvo%d" % i, [128, 512], BF16) for i in range(2)]; d_cvo = [Dep("cvo%d" % i) for i in range(2)]
        dtt = [sb("dtt%d" % i, [128, 16], F32) for i in range(2)]; d_dtt = [Dep("dtt%d" % i) for i in range(2)]
        u = 0
        for j in range(NS):
            hb = j % 2
            P.dma(hts[hb][:, :, :], hT_d[j], writes=[d_hts[hb]], semkey=("hts", hb))
            for c in range(6):
                b = u % 2
                u += 1
                for kc in range(8):
                    mm(pp[b][:, :], Wx[:, kc, c * 128:(c + 1) * 128], hts[hb][:, kc, :], kc == 0, kc == 7,
                       [d_W, d_hts[hb]], [d_pp[b]])
                cp("scalar", xraw[:, c, 3 + j * 512:3 + (j + 1) * 512], pp[b][:, :], [d_pp[b], d_xpad], [d_xr[j]])
            for i4 in range(4):
                t = j * 4 + i4
                b = t % 2
                for kc in range(8):
                    mm(pd[b][:, :], hts[hb][:, kc, i4 * 128:(i4 + 1) * 128], Wd[:, kc, :], kc == 0, kc == 7,
                       [d_W, d_hts[hb]], [d_pd[b]])
                tt("vector", dtt[b][:, :], pd[b][:, :], dtb[:, :], ALU.add, [d_pd[b], d_dtb], [d_dtt[b]])
                act(dtt[b][:, :], dtt[b][:, :], AF.Exp, [d_dtt[b]], [d_dtt[b]])
                act(dtv[:, t, :], dtt[b][:, :], AF.Ln, [d_dtt[b]], [d_dt[t]], bias=1.0)
                tt("vector", dAv[:, t, :], dtv[:, t, :], aneg[:, :], ALU.mult, [d_dt[t], d_an], [d_dt[t]])
        for j in range(NS):
            rd = [d_xr[j], d_xpad, d_dg]
            if j > 0:
                rd.append(d_xr[j - 1])
            if j < NS - 1:
                rd.append(d_xr[j + 1])
            for c in range(6):
                b = u % 2
                u += 1
                for tap in range(7):
                    mm(pp[b][:, :], diagw[:, c * 7 + tap, :], xraw[:, c, j * 512 + tap:j * 512 + tap + 512], tap == 0, tap == 6,
                       rd, [d_pp[b]])
                js = slice(j * 512, (j + 1) * 512)
                if c < 5:
                    dst = cvo[b][:, :] if c < 4 else BT[:, js]
                    wr = [d_cvo[b]] if c < 4 else [d_BT[j]]
                    act(dst, pp[b][:, :], AF.Silu, [d_pp[b], d_cb], wr, bias=cbias[:, c:c + 1])
                    for i4 in range(4):
                        t = j * 4 + i4
                        if c < 4:
                            tr(ptr[b][:, i4, :], cvo[b][:, i4 * 128:(i4 + 1) * 128], ident[:, :], [d_cvo[b], d_ident], [d_ptr[b]])
                        else:
                            tr(ptr[b][:, i4, :], BT[:, t * 128:(t + 1) * 128], ident[:, :], [d_BT[j], d_ident], [d_ptr[b]])
                    if c < 4:
                        cp("vector", xs_tok[:, j * 4:(j + 1) * 4, c * 128:(c + 1) * 128], ptr[b][:, :, :], [d_ptr[b]],
                           [d_xs[j * 4 + i] for i in range(4)])
                    else:
                        cp("vector", B_tok[:, j * 4:(j + 1) * 4, :], ptr[b][:, :, :], [d_ptr[b]],
                           [d_Bt[j * 4 + i] for i in range(4)])
                else:
                    act(CTz[0:64, 0, js], pp[b][0:64, :], AF.Silu, [d_pp[b], d_cb, d_CTinit], [d_CT[j]], bias=cbias[0:64, 5:6])
                    act(CTz[64:128, 1, js], pp[b][64:128, :], AF.Silu, [d_pp[b], d_cb, d_CTinit], [d_CT[j]], bias=cbias[64:128, 5:6])
        if "ssd_a" in dbg:
            o1 = dbgout("xs_tok", [128, NT, 512], BF16); o2 = dbgout("B_tok", [128, NT, 128], BF16)
            o3 = dbgout("CTz", [128, 2, S], BF16); o4 = dbgout("dtv", [128, NT, 16], F32); o5 = dbgout("BT", [128, S], BF16)
            P.dma(o1, xs_tok[:, :, :], reads=d_xs, semkey="d0")
            P.dma(o2, B_tok[:, :, :], reads=d_Bt, semkey="d1")
            P.dma(o3, CTz[:, :, :], reads=d_CT, semkey="d2")
            P.dma(o4, dtv[:, :, :], reads=d_dt, semkey="d3")
            P.dma(o5, BT[:, :], reads=d_BT, semkey="d4")
        P.barrier()
        P.emit()
    if stop_after == 41:
        es4.close()
        return finish()

    with ExitStack() as ph:
        sb = lambda name, shape, dt: ph.enter_context(nc.sbuf_tensor(uq(name), list(shape), dt))
        ps = lambda name, shape, dt: ph.enter_context(nc.psum_tensor(uq(name), list(shape), dt))
        masks = sb("s_masks", [128, 4, 128], F32); d_mk = Dep("masks")
        P.dma(masks[:, :, :], masks_d[:, :, :], writes=[d_mk], semkey="c0")
        MU, MSL, ML, MSU = 0, 1, 2, 3
        dsk = sb("s_dsk", [128, 8], F32); d_dsk = Dep("dsk")
        P.dma(dsk[:, :], dsk_d[:, :], writes=[d_dsk], semkey="c1")
        ones_f = sb("ones_f", [128, 128], F32); d_of = Dep("ones_f")
        memset("gpsimd", ones_f[:, :], 1.0, [d_of])
        Wz = sb("Wz", [128, 8, 512], BF16); d_Wz = Dep("Wz")
        stg = [sb("stg%d" % i, [128, 8, 256], F32) for i in range(2)]; d_stg = [Dep("stg%d" % i) for i in range(2)]
        load_w(stg, d_stg, w_view[:, :, C_Z:C_Z + 512], 8, 512, lambda c0, n: Wz[:, :, c0:c0 + n], d_Wz, nw[:, :], d_nw)
        Sb_all = sb("Sb_all", [128, NT, 256], BF16)
        d_Sb = [Dep("Sb%d" % t) for t in range(NT)]
        St = [sb("St%d" % i, [128, 256], F32) for i in range(2)]
        d_St = [Dep("St%d" % i) for i in range(2)]
        Sf_bf = [sb("Sfbf%d" % i, [128, 256], BF16) for i in range(2)]; d_Sfbf = [Dep("Sfbf%d" % i) for i in range(2)]
        pcb_t = ps("pcb_t", [128, 512], F32)
        pcb = pcb_t[:, 0:256].rearrange("p (g i) -> p g i", g=2); d_pcb = Dep("pcb")
        psm_main = [pcb_t[:, 256:352]]; d_psm_main = [d_pcb]
        pst = ps("pst", [128, 512], F32); d_pst = Dep("pst")
        pseg = [ps("pseg%d" % i, [128, 4, 128], F32) for i in range(2)]; d_pseg = [Dep("pseg%d" % i) for i in range(2)]
        pyd = ps("pyd", [128, 512], F32); d_pyd = Dep("pyd")
        pyo = ps("pyo", [128, 512], F32); d_pyo = Dep("pyo")
        pz = ps("pz", [128, 512], F32); d_pz = Dep("pz")
        sm_ = [sb("sm%d" % i, [128, 96], F32) for i in range(2)]; d_sm_ = [Dep("sm%d" % i) for i in range(2)]
        cd2 = sb("cd2", [128, 2, 4], F32); d_cd2 = Dep("cd2")
        dtw = sb("dtw", [128, 16], F32); d_dtw = Dep("dtw")
        xdt = [sb("xdt%d" % i, [128, 512], BF16) for i in range(2)]; d_xdt = [Dep("xdt%d" % i) for i in range(2)]
        xw = sb("xw", [128, 512], BF16); d_xw = Dep("xw")
        cbm = [sb("cbm%d" % i, [128, 2, 128], F32) for i in range(2)]; d_cbm = [Dep("cbm%d" % i) for i in range(2)]
        Lh_ = [sb("Lh%d" % i, [128, 8, 128], F32) for i in range(2)]; d_Lh_ = [Dep("Lh%d" % i) for i in range(2)]
        Ee_ = [sb("Ee%d" % i, [128, 8, 128], F32) for i in range(2)]; d_Ee_ = [Dep("Ee%d" % i) for i in range(2)]
        MT_ = [sb("MT%d" % i, [128, 8, 128], BF16) for i in range(2)]; d_MT_ = [Dep("MT%d" % i) for i in range(2)]
        ya_ = [sb("ya%d" % i, [128, 512], F32) for i in range(2)]; d_ya_ = [Dep("ya%d" % i) for i in range(2)]
        yb = sb("yb", [128, 512], F32); d_yb = Dep("yb")
        zs_ = [sb("zs%d" % i, [128, 512], F32) for i in range(2)]; d_zs_ = [Dep("zs%d" % i) for i in range(2)]
        yn = sb("yn", [128, 512], BF16); d_yn = Dep("yn")
        junk4 = sb("junk4", [128, 512], BF16); d_j4 = Dep("junk4")
        ssq = sb("ssq", [128, 2], F32); d_ssq = Dep("ssq")
        ptr4 = ps("ptr4", [128, 4, 128], BF16); d_ptr4 = Dep("ptr4")
        hts = [sb("hts%d" % i, [128, 8, 512], BF16) for i in range(2)]; d_hts = [Dep("hts%d" % i) for i in range(2)]
        for i in range(2):
            memset("gpsimd", St[i][:, :], 0.0, [d_St[i]])

        Bdef = (cd2, d_cd2, dtw, d_dtw, xw, d_xw, pst, d_pst)
        def small_sums(t, which, sm, d_sm, psmo=None):
            psm, d_psm = ([psmo[0]], [psmo[1]]) if psmo is not None else (psm_main, d_psm_main)
            rdA = [d_dt[t], d_mk, d_of]
            if "acum" in which:
                mm(psm[0][:, 0:8], masks[:, MU, :], dAv[:, t, 0:8], True, True, rdA, [d_psm[0]])
                mm(psm[0][:, 8:16], masks[:, ML, :], dAv[:, t, 8:16], True, True, rdA, [d_psm[0]])
                mm(psm[0][:, 16:24], masks[:, MSL, :], dAv[:, t, 0:8], True, True, rdA, [d_psm[0]])
                mm(psm[0][:, 24:40], ones_f[:, :], dAv[:, t, 0:16], True, True, rdA, [d_psm[0]])
                act(sm[:, 0:40], psm[0][:, 0:40], AF.Exp, [d_psm[0]], [d_sm])
            else:
                mm(psm[0][:, 64:72], masks[:, MSU, :], dAv[:, t, 8:16], True, True, rdA, [d_psm[0]])
                mm(psm[0][:, 72:88], ones_f[:, :], dAv[:, t, 0:16], True, True, rdA, [d_psm[0]])
                act(sm[:, 64:88], psm[0][:, 64:88], AF.Exp, [d_psm[0]], [d_sm])

        def state_step1(t, d, sm, d_sm, bufs=None):
            cd2, d_cd2, dtw, d_dtw, xw, d_xw, pst, d_pst = bufs if bufs is not None else Bdef
            tot0 = 24 if d == 0 else 72
            cdv = sm[:, tot0:tot0 + 16].rearrange("p (a h) -> p a h", a=2)
            cp("gpsimd", cd2[0:64, :, :], cdv[0:64, :, 0:4], [d_sm], [d_cd2])
            cp("gpsimd", cd2[64:128, :, :], cdv[64:128, :, 4:8], [d_sm], [d_cd2])
            dcol = 16 if d == 0 else 64
            tt("vector", dtw[:, 0:8], dtv[:, t, d * 8:(d + 1) * 8], sm[:, dcol:dcol + 8], ALU.mult, [d_dt[t], d_sm], [d_dtw])
            tt("vector", xw[:, :].rearrange("p (h q) -> p h q", h=8), xs_tok[:, t, :].rearrange("p (h q) -> p h q", h=8),
               dtw[:, 0:8].unsqueeze(2).to_broadcast([128, 8, 64]), ALU.mult, [d_xs[t], d_dtw], [d_xw])
            for g in range(2):
                mm(pst[:, g * 256:(g + 1) * 256], B_tok[:, t, :], xw[:, g * 256:(g + 1) * 256], True, True,
                   [d_Bt[t], d_xw], [d_pst])

        def state_step2(t, d, bufs=None):
            cd2, d_cd2, dtw, d_dtw, xw, d_xw, pst, d_pst = bufs if bufs is not None else Bdef
            tt("vector", St[d][:, :].rearrange("p (h q) -> p h q", h=4), St[d][:, :].rearrange("p (h q) -> p h q", h=4),
               cd2[:, d, :].unsqueeze(2).to_broadcast([128, 4, 64]), ALU.mult, [d_St[d], d_cd2], [d_St[d]])
            tt("vector", St[d][0:64, :], St[d][0:64, :], pst[0:64, 0:256], ALU.add, [d_St[d], d_pst], [d_St[d]])
            tt("vector", St[d][64:128, :], St[d][64:128, :], pst[64:128, 256:512], ALU.add, [d_St[d], d_pst], [d_St[d]])

        RB = 4
        Bring = [Bdef]
        pst_ring = [(pst, d_pst), (pyd, d_pyd), (pyo, d_pyo), (pz, d_pz)]
        for r in range(1, RB):
            Bring.append((sb("cd2r%d" % r, [128, 2, 4], F32), Dep("cd2r%d" % r), sb("dtwr%d" % r, [128, 16], F32), Dep("dtwr%d" % r),
                          sb("xwr%d" % r, [128, 512], BF16), Dep("xwr%d" % r), pst_ring[r][0], pst_ring[r][1]))
        smB = [sm_[0], sm_[1], sb("smr2", [128, 96], F32), sb("smr3", [128, 96], F32)]
        d_smB = [d_sm_[0], d_sm_[1], Dep("smr2"), Dep("smr3")]
        psmB = [(pseg[i][:, 0, :], d_pseg[i]) for i in range(2)]
        orderB = list(range(NT - 1, -1, -1))

        def preB(k):
            t, r = orderB[k], k % RB
            small_sums(t, ["dte_b"], smB[r], d_smB[r], psmB[k % 2])
            state_step1(t, 1, smB[r], d_smB[r], Bring[r])

        for k in range(min(RB - 1, NT)):
            preB(k)
        for k, t in enumerate(orderB):
            cp("vector", Sb_all[:, t, :], St[1][:, :], [d_St[1]], [d_Sb[t]])
            state_step2(t, 1, Bring[k % RB])
            if k + RB - 1 < NT:
                preB(k + RB - 1)
        A_NT = int(os.environ.get("SSD_NT", str(NT)))

        def tail_pool(t):
            ya, d_ya, zs, d_zs = ya_[t % 2], d_ya_[t % 2], zs_[t % 2], d_zs_[t % 2]
            tt("gpsimd", ya[:, :], ya[:, :], yb[:, :], ALU.add, [d_ya, d_yb], [d_ya])
            tt("gpsimd", yb[:, :].rearrange("p (h q) -> p h q", h=8), xs_tok[:, t, :].rearrange("p (h q) -> p h q", h=8),
               dsk[:, :].unsqueeze(2).to_broadcast([128, 8, 64]), ALU.mult, [d_xs[t], d_dsk], [d_yb])
            tt("gpsimd", ya[:, :], ya[:, :], yb[:, :], ALU.add, [d_ya, d_yb], [d_ya])
            tt("gpsimd", ya[:, :], ya[:, :], zs[:, :], ALU.mult, [d_ya, d_zs], [d_ya])

        def tail_act(t):
            ya, d_ya = ya_[t % 2], d_ya_[t % 2]
            act(junk4[:, :], ya[:, :], AF.Square, [d_ya], [d_j4, d_ssq], accum_out=ssq[:, 0:1])
            act(ssq[:, 1:2], ssq[:, 0:1], AF.Sqrt, [d_ssq, d_eps], [d_ssq], bias=epsb[:, 0:1], scale=1.0 / 512)

        def tail_dve(t):
            ya, d_ya = ya_[t % 2], d_ya_[t % 2]
            recip(ssq[:, 1:2], ssq[:, 1:2], [d_ssq], [d_ssq])
            ts("vector", yn[:, :], ya[:, :], ssq[:, 1:2], None, ALU.mult, None, [d_ya, d_ssq], [d_yn])

        def tail_pe(t):
            for c in range(4):
                tr(ptr4[:, c, :], yn[:, c * 128:(c + 1) * 128], ident[:, :], [d_yn, d_ident], [d_ptr4])

        def tail_out(t):
            cp("scalar", ssdT[:, :, t * 128:(t + 1) * 128], ptr4[:, :, :], [d_ptr4], [d_ssdT])

        pend = None
        for t in range(A_NT):
            j, i4 = divmod(t, 4)
            hb = j % 2
            tok = slice(t * 128, (t + 1) * 128)
            if i4 == 0:
                P.dma(hts[hb][:, :, :], hT_d[j], writes=[d_hts[hb]], semkey=("hts", hb))
            fb = t % 2
            ya, d_ya, zs, d_zs = ya_[t % 2], d_ya_[t % 2], zs_[t % 2], d_zs_[t % 2]
            sm, d_sm = sm_[t % 2], d_sm_[t % 2]
            for kc in range(8):
                mm(pz[:, :], hts[hb][:, kc, i4 * 128:(i4 + 1) * 128], Wz[:, kc, :], kc == 0, kc == 7, [d_hts[hb], d_Wz], [d_pz])
            act(zs[:, :], pz[:, :], AF.Silu, [d_pz], [d_zs])
            cp("gpsimd", Sf_bf[fb][:, :], St[0][:, :], [d_St[0]], [d_Sfbf[fb]])
            for d in range(2):
                dsl = slice(d * 8, (d + 1) * 8)
                lmask = MSL if d == 0 else MSU
                tt("vector" if d == 0 else "gpsimd", xdt[d][:, :].rearrange("p (h q) -> p h q", h=8), xs_tok[:, t, :].rearrange("p (h q) -> p h q", h=8),
                   dtv[:, t, dsl].unsqueeze(2).to_broadcast([128, 8, 64]), ALU.mult, [d_xs[t], d_dt[t]], [d_xdt[d]])
                tt("vector" if d == 0 else "gpsimd", Lh_[d][:, :, :], masks[:, lmask:lmask + 1, :].to_broadcast([128, 8, 128]),
                   dAv[:, t, dsl].unsqueeze(2).to_broadcast([128, 8, 128]), ALU.mult, [d_mk, d_dt[t]], [d_Lh_[d]])
            if pend is not None:
                tail_pool(pend)
            small_sums(t, ["acum", "dte_f"], sm, d_sm)
            for g in range(2):
                mm(pcb[:, g, :], BT[:, tok], CTz[:, g, tok], True, True, [d_BT[j], d_CT[j]], [d_pcb])

            def seg(d):
                tri = MU if d == 0 else ML
                for h in range(8):
                    mm(pseg[h // 4][:, h % 4, :], Lh_[d][:, h, :], masks[:, tri, :], True, True, [d_Lh_[d], d_mk], [d_pseg[h // 4]])
                for q in range(2):
                    act(Ee_[d][:, q * 4:(q + 1) * 4, :], pseg[q][:, :, :], AF.Exp, [d_pseg[q]], [d_Ee_[d]])

            def mtmul(d):
                for g in range(2):
                    tt("vector", MT_[d][:, g * 4:(g + 1) * 4, :], Ee_[d][:, g * 4:(g + 1) * 4, :],
                       cbm[d][:, g:g + 1, :].to_broadcast([128, 4, 128]), ALU.mult, [d_Ee_[d], d_cbm[d]], [d_MT_[d]])

            def ymm(d):
                for h in range(8):
                    mm(pyd[:, h * 64:(h + 1) * 64], MT_[d][:, h, :], xdt[d][:, h * 64:(h + 1) * 64], True, True,
                       [d_MT_[d], d_xdt[d]], [d_pyd])
                Sin = Sf_bf[fb][:, :] if d == 0 else Sb_all[:, t, :]
                dS = d_Sfbf[fb] if d == 0 else d_Sb[t]
                for g in range(2):
                    mm(pyo[:, g * 256:(g + 1) * 256], CTz[:, g, tok], Sin, True, True, [d_CT[j], dS], [d_pyo])

            def yacc(d):
                dsl = slice(d * 8, (d + 1) * 8)
                yy = ya if d == 0 else yb
                dy = d_ya if d == 0 else d_yb
                tt("vector", yy[:, :].rearrange("p (h q) -> p h q", h=8), pyo[:, :].rearrange("p (h q) -> p h q", h=8),
                   sm[:, dsl].unsqueeze(2).to_broadcast([128, 8, 64]), ALU.mult, [d_pyo, d_sm], [dy])
                tt("vector", yy[:, :], yy[:, :], pyd[:, :], ALU.add, [dy, d_pyd], [dy])

            seg(0)
            state_step1(t, 0, sm, d_sm)
            tt("vector", cbm[0][:, :, :], pcb[:, :, :], masks[:, MU:MU + 1, :].to_broadcast([128, 2, 128]), ALU.mult,
               [d_pcb, d_mk], [d_cbm[0]])
            tt("vector", cbm[1][:, :, :], pcb[:, :, :], masks[:, ML:ML + 1, :].to_broadcast([128, 2, 128]), ALU.mult,
               [d_pcb, d_mk], [d_cbm[1]])
            seg(1)
            if pend is not None:
                tail_act(pend)
            mtmul(0)
            ymm(0)
            state_step2(t, 0)
            mtmul(1)
            yacc(0)
            if pend is not None:
                tail_dve(pend)
            ymm(1)
            if pend is not None:
                tail_pe(pend)
            yacc(1)
            if pend is not None:
                tail_out(pend)
            pend = t
        tail_pool(pend)
        tail_act(pend)
        tail_dve(pend)
        tail_pe(pend)
        tail_out(pend)
        if "ssdT" in dbg:
            o1 = dbgout("ssdT", [128, 4, S], BF16)
            P.dma(o1, ssdT[:, :, :], reads=[d_ssdT], semkey="d0")
        P.barrier()
        P.emit()
    es4.close()
    if stop_after == 4:
        return finish()


    es35 = ExitStack()
    sb35 = lambda name, shape, dt: es35.enter_context(nc.sbuf_tensor(uq(name), list(shape), dt))
    attnT = sb35("attnT", [128, 4, S], BF16)
    d_attn = Dep("attnT")
    es23 = ExitStack()
    sb23 = lambda name, shape, dt: es23.enter_context(nc.sbuf_tensor(uq(name), list(shape), dt))
    kTd = sb23("kTd", [128, 2, S], BF16)
    Va = sb23("Va", [128, NT, 2, 128], BF16)
    Vb = sb23("Vb", [128, NT, 2, 128], BF16)
    cosT = sb23("s_cosT", [128, S], BF16); d_cos = Dep("cos")
    sinT = sb23("s_sinT", [128, S], BF16); d_sin = Dep("sin")
    qkw = sb23("s_qkw", [128, 2], F32); d_qkw = Dep("qkw")
    onesblk = sb23("s_onesblk", [128, 128], BF16); d_ob = Dep("ob")
    rmat = sb23("s_rmat", [128, 128], BF16); d_rm = Dep("rm")
    sq = sb23("sq", [128, 512], BF16); d_sq = Dep("sq")
    rq = sb23("rq", [128, 512], F32); d_rq = Dep("rq")
    qn = sb23("qn", [128, 512], BF16); d_qn = Dep("qn")
    t1 = sb23("t1", [128, 512], F32); d_t1 = Dep("t1")
    t2 = sb23("t2", [128, 512], F32); d_t2 = Dep("t2")
    sq0, d_sq0, rq0, d_rq0, qn0, d_qn0, t10, d_t10, t20, d_t20 = sq, d_sq, rq, d_rq, qn, d_qn, t1, d_t1, t2, d_t2
    d_kT = [[Dep("kT%d_%d" % (m, j)) for j in range(NS)] for m in range(2)]
    d_V = [Dep("V%d" % t) for t in range(NT)]
    d_Vinit = Dep("Vinit")

    def normrope(qp_ap, d_qp, aux_ap, d_aux, wcol, j, dst, d_dst, alt=None):
        sq, d_sq, rq, d_rq, qn, d_qn, t1, d_t1, t2, d_t2 = alt if alt is not None else (sq0, d_sq0, rq0, d_rq0, qn0, d_qn0, t10, d_t10, t20, d_t20)
        act(sq[:, :], qp_ap, AF.Square, [d_qp], [d_sq])
        mm(aux_ap, onesblk[:, :], sq[:, :], True, True, [d_ob, d_sq], [d_aux])
        act(rq[:, :], aux_ap, AF.Sqrt, [d_aux, d_eps], [d_rq], bias=epsb[:, 0:1], scale=1.0 / 64)
        recip(rq[:, :], rq[:, :], [d_rq], [d_rq])
        stt(qn[:, :], qp_ap, qkw[:, wcol:wcol + 1], rq[:, :], ALU.mult, ALU.mult, [d_qp, d_rq, d_qkw], [d_qn])
        mm(aux_ap, rmat[:, :], qn[:, :], True, True, [d_rm, d_qn], [d_aux])
        js = slice(j * 512, (j + 1) * 512)
        tt("vector", t1[:, :], qn[:, :], cosT[:, js], ALU.mult, [d_qn, d_cos], [d_t1])
        tt("vector", t2[:, :], aux_ap, sinT[:, js], ALU.mult, [d_aux, d_sin], [d_t2])
        tt("gpsimd", dst, t1[:, :], t2[:, :], ALU.add, [d_t1, d_t2], [d_dst])

    with ExitStack() as ph:
        sb = lambda name, shape, dt: ph.enter_context(nc.sbuf_tensor(uq(name), list(shape), dt))
        ps = lambda name, shape, dt: ph.enter_context(nc.psum_tensor(uq(name), list(shape), dt))
        P.dma(qkw[:, :], qkw_d[:, :], writes=[d_qkw], semkey="c0")
        P.dma(onesblk[:, :], onesblk_d[:, :], writes=[d_ob], semkey="c1")
        P.dma(rmat[:, :], rmat_d[:, :], writes=[d_rm], semkey="c2")
        P.dma(cosT[:, :], cosT_d[:, :], writes=[d_cos], semkey="c3")
        P.dma(sinT[:, :], sinT_d[:, :], writes=[d_sin], semkey="c4")
        memset("gpsimd", Va[:, :, :, 64:128], 0.0, [d_Vinit])
        memset("gpsimd", Va[:, :, :, 64:65], 1.0, [d_Vinit])
        memset("gpsimd", Vb[:, :, :, 0:64], 0.0, [d_Vinit])
        memset("gpsimd", Vb[:, :, :, 0:1], 1.0, [d_Vinit])
        Wk = sb("Wk", [128, 8, 256], BF16); Wv = sb("Wv", [128, 8, 128], BF16); d_W = Dep("W2")
        stg = [sb("stg%d" % i, [128, 8, 256], F32) for i in range(2)]; d_stg = [Dep("stg%d" % i) for i in range(2)]
        for (dlo, slo) in [(0, 0), (64, 0), (128, 64), (192, 64)]:
            load_w(stg, d_stg, w_view[:, :, C_K + slo:C_K + slo + 64], 8, 64,
                   lambda c0, n, dlo=dlo: Wk[:, :, dlo + c0:dlo + c0 + n], d_W, nw[:, :], d_nw)
        load_w(stg, d_stg, w_view[:, :, C_V:C_V + 128], 8, 128, lambda c0, n: Wv[:, :, c0:c0 + n], d_W, nw[:, :], d_nw)
        hts = [sb("hts%d" % i, [128, 8, 512], BF16) for i in range(2)]; d_hts = [Dep("hts%d" % i) for i in range(2)]
        qp = [ps("qp%d" % i, [128, 512], F32) for i in range(2)]; d_qp = [Dep("qp%d" % i) for i in range(2)]
        aux = [ps("aux%d" % i, [128, 512], F32) for i in range(2)]; d_aux = [Dep("aux%d" % i) for i in range(2)]
        vp = [ps("vp%d" % i, [128, 128], F32) for i in range(2)]; d_vp = [Dep("vp%d" % i) for i in range(2)]
        alt1 = (sb("sq1", [128, 512], BF16), Dep("sq1"), sb("rq1", [128, 512], F32), Dep("rq1"), sb("qn1", [128, 512], BF16), Dep("qn1"),
                sb("t11", [128, 512], F32), Dep("t11"), sb("t21", [128, 512], F32), Dep("t21"))
        u = 0
        for j in range(NS):
            hb = j % 2
            P.dma(hts[hb][:, :, :], hT_d[j], writes=[d_hts[hb]], semkey=("hts", hb))
            for m in range(2):
                b = u % 2
                u += 1
                for kc in range(8):
                    mm(qp[b][:, :], Wk[:, kc, m * 128:(m + 1) * 128], hts[hb][:, kc, :], kc == 0, kc == 7,
                       [d_W, d_hts[hb]], [d_qp[b]])
                normrope(qp[b][:, :], d_qp[b], aux[b][:, :], d_aux[b], 1, j, kTd[:, m, j * 512:(j + 1) * 512], d_kT[m][j],
                         alt=(None if b == 0 else alt1))
            for i4 in range(4):
                t = j * 4 + i4
                vb = t % 2
                for kc in range(8):
                    mm(vp[vb][:, :], hts[hb][:, kc, i4 * 128:(i4 + 1) * 128], Wv[:, kc, :], kc == 0, kc == 7,
                       [d_W, d_hts[hb]], [d_vp[vb]])
                vv = vp[vb][:, :].rearrange("p (a b) -> p a b", a=2)
                cp("scalar", Va[:, t, :, 0:64], vv, [d_vp[vb], d_Vinit], [d_V[t]])
                cp("vector", Vb[:, t, :, 64:128], vv, [d_vp[vb], d_Vinit], [d_V[t]])
        P.barrier()
        P.emit()

    with ExitStack() as ph:
        sb = lambda name, shape, dt: ph.enter_context(nc.sbuf_tensor(uq(name), list(shape), dt))
        ps = lambda name, shape, dt: ph.enter_context(nc.psum_tensor(uq(name), list(shape), dt))
        ones_f = sb("ones_f", [128, 128], F32); d_of = Dep("ones_f")
        memset("gpsimd", ones_f[:, :], 1.0, [d_of])
        Wq = sb("Wq", [128, 8, 512], BF16); d_Wq = Dep("Wq")
        stg = [sb("stg%d" % i, [128, 8, 256], F32) for i in range(2)]; d_stg = [Dep("stg%d" % i) for i in range(2)]
        load_w(stg, d_stg, w_view[:, :, C_Q:C_Q + 512], 8, 512, lambda c0, n: Wq[:, :, c0:c0 + n], d_Wq, nw[:, :], d_nw)
        hts = [sb("hts%d" % i, [128, 8, 512], BF16) for i in range(2)]; d_hts = [Dep("hts%d" % i) for i in range(2)]
        qm = [sb("qm%d" % i, [128, 512], BF16) for i in range(2)]; d_qm = [Dep("qm%d" % i) for i in range(2)]
        sc = [ps("sc%d" % i, [128, 512], F32) for i in range(4)]; d_sc = [Dep("sc%d" % i) for i in range(4)]
        ov = [ps("ov%d" % i, [128, 512], F32) for i in range(2)]; d_ov = [Dep("ov%d" % i) for i in range(2)]
        aux = [ps("aux%d" % i, [128, 512], F32) for i in range(2)]; d_aux = [Dep("aux%d" % i) for i in range(2)]
        pT = [sb("pT%d" % i, [128, 512], BF16) for i in range(4)]; d_pT = [Dep("pT%d" % i) for i in range(4)]
        den = [sb("den%d" % i, [128, 512], F32) for i in range(2)]; d_den = [Dep("den%d" % i) for i in range(2)]
        rb = [sb("rb%d" % i, [128, 512], F32) for i in range(2)]; d_rb = [Dep("rb%d" % i) for i in range(2)]
        A_M = int(os.environ.get("ATT_M", "4")); A_J = int(os.environ.get("ATT_J", str(NS)))
        iters = [(m, j) for m in range(A_M) for j in range(A_J)]
        N_FILL = int(os.environ.get("ATT_FILL", "0"))

        ovs = [sb("ovs%d" % i, [128, 512], F32) for i in range(2)]; d_ovs = [Dep("ovs%d" % i) for i in range(2)]

        def q_stages(n):
            m, j = iters[n]
            hb = n % 2
            js = slice(j * 512, (j + 1) * 512)
            qp_ap, d_qp, aux_ap, d_ax = aux[0][:, :], d_aux[0], aux[1][:, :], d_aux[1]
            P.dma(hts[hb][:, :, :], hT_d[j], writes=[d_hts[hb]], semkey=("hts", hb))
            yield
            for kc in range(8):
                mm(qp_ap, Wq[:, kc, m * 128:(m + 1) * 128], hts[hb][:, kc, :], kc == 0, kc == 7, [d_Wq, d_hts[hb]], [d_qp])
            yield
            act(sq[:, :], qp_ap, AF.Square, [d_qp], [d_sq])
            yield
            mm(aux_ap, onesblk[:, :], sq[:, :], True, True, [d_ob, d_sq], [d_ax])
            yield
            act(rq[:, :], aux_ap, AF.Sqrt, [d_ax, d_eps], [d_rq], bias=epsb[:, 0:1], scale=1.0 / 64)
            yield
            recip(rq[:, :], rq[:, :], [d_rq], [d_rq])
            yield
            stt(qn[:, :], qp_ap, qkw[:, 0:1], rq[:, :], ALU.mult, ALU.mult, [d_qp, d_rq, d_qkw], [d_qn])
            yield
            mm(aux_ap, rmat[:, :], qn[:, :], True, True, [d_rm, d_qn], [d_ax])
            yield
            tt("vector", t1[:, :], qn[:, :], cosT[:, js], ALU.mult, [d_qn, d_cos], [d_t1])
            yield
            tt("vector", t2[:, :], aux_ap, sinT[:, js], ALU.mult, [d_ax, d_sin], [d_t2])
            yield
            tt("gpsimd", qm[n % 2][:, :], t1[:, :], t2[:, :], ALU.add, [d_t1, d_t2], [d_qm[n % 2]])
            yield

        def norm_stages(n):
            m, j = iters[n]
            qs = slice(j * 512, (j + 1) * 512)
            for hh in range(2):
                dr = 64 if hh == 0 else 0
                lo = 0 if hh == 0 else 64
                if hh == 0:
                    mm(aux[hh][0:64, :], ones_f[dr:dr + 1, 0:64], ovs[hh][dr:dr + 1, :], True, True, [d_of, d_ovs[hh]], [d_aux[hh]])
                else:
                    mm(aux[hh][:, :], ones_f[dr:dr + 1, :], ovs[hh][dr:dr + 1, :], True, True, [d_of, d_ovs[hh]], [d_aux[hh]])
                yield
                recip(rb[hh][lo:lo + 64, :], aux[hh][lo:lo + 64, :], [d_aux[hh]], [d_rb[hh]])
                yield
                tt("vector", attnT[lo:lo + 64, m, qs], ovs[hh][lo:lo + 64, :], rb[hh][lo:lo + 64, :], ALU.mult,
                   [d_ovs[hh], d_rb[hh]], [d_attn])
                yield

        def drain(gen):
            for _ in gen:
                pass

        drain(q_stages(0))

        def scores(n, kt):
            m, j = iters[n]
            qb = n % 2
            it = n * NT + kt
            ks = slice(kt * 128, (kt + 1) * 128)
            for hh in range(2):
                kv = (2 * m + hh) // 4
                off = hh * 64
                sbk = (it % 2) * 2 + hh
                mm(sc[sbk][:, :], kTd[off:off + 64, kv, ks], qm[qb][off:off + 64, :], True, True,
                   [d_kT[kv][kt // 4], d_qm[qb]], [d_sc[sbk]])
                act(pT[sbk][:, :], sc[sbk][:, :], AF.Exp, [d_sc[sbk]], [d_pT[sbk]], scale=0.125)

        def pv(n, kt):
            m, j = iters[n]
            it = n * NT + kt
            for hh in range(2):
                kv = (2 * m + hh) // 4
                sbk = (it % 2) * 2 + hh
                if hh == 0:
                    mm(ov[0][0:65, :], Va[:, kt, kv, 0:65], pT[sbk][:, :], kt == 0, kt == NT - 1,
                       [d_V[kt], d_pT[sbk]], [d_ov[0]])
                else:
                    mm(ov[1][:, :], Vb[:, kt, kv, :], pT[sbk][:, :], kt == 0, kt == NT - 1,
                       [d_V[kt], d_pT[sbk]], [d_ov[1]])

        for n, (m, j) in enumerate(iters):
            gens = []
            if n >= 1:
                gens.append(norm_stages(n - 1))
            if n + 1 < len(iters):
                gens.append(q_stages(n + 1))
            scores(n, 0)
            for kt in range(NT):
                if kt + 1 < NT:
                    scores(n, kt + 1)
                pv(n, kt)
                while gens:
                    try:
                        next(gens[0])
                        break
                    except StopIteration:
                        gens.pop(0)
            for g_ in gens:
                drain(g_)
            cp("vector", ovs[0][0:65, :], ov[0][0:65, :], [d_ov[0]], [d_ovs[0]])
            cp("vector", ovs[1][:, :], ov[1][:, :], [d_ov[1]], [d_ovs[1]])
        drain(norm_stages(len(iters) - 1))
        if "attnT" in dbg:
            da = dbgout("attnT", [128, 4, S], BF16)
            P.dma(da, attnT[:, :, :], reads=[d_attn], semkey="d0")
        P.barrier()
        P.emit()
    es23.close()
    if stop_after == 3:
        es35.close()
        return finish()


    with ExitStack() as ph:
        sb = lambda name, shape, dt: ph.enter_context(nc.sbuf_tensor(uq(name), list(shape), dt))
        ps = lambda name, shape, dt: ph.enter_context(nc.psum_tensor(uq(name), list(shape), dt))
        nssd = sb("s_nssd", [128, 4], F32); d_nssd = Dep("nssd")
        bg = sb("s_bg", [128, 16], F32); d_bg = Dep("bg")
        P.dma(nssd[:, :], nssd_d[:, :], writes=[d_nssd], semkey="c0")
        P.dma(bg[:, :], bg_d[:, :], writes=[d_bg], semkey="c1")
        Wg = sb("Wg", [128, 8, 2048], BF16); Wao = sb("Wao", [128, 4, 1024], BF16)
        Wso = sb("Wso", [128, 4, 1024], BF16); Wout = sb("Wout", [128, 8, 1024], BF16)
        d_W = Dep("W5")
        stg = [sb("stg%d" % i, [128, 8, 128], F32) for i in range(2)]; d_stg = [Dep("stg%d" % i) for i in range(2)]
        load_w(stg, d_stg, w_view[:, :, C_G:C_G + 2048], 8, 2048, lambda c0, n: Wg[:, :, c0:c0 + n], d_W, nw[:, :], d_nw, piece=128)
        load_w(stg, d_stg, w_ao_d.rearrange("(c p) n -> p c n", p=128), 4, 1024, lambda c0, n: Wao[:, :, c0:c0 + n], d_W, piece=128)
        load_w(stg, d_stg, w_so_d.rearrange("(c p) n -> p c n", p=128), 4, 1024, lambda c0, n: Wso[:, :, c0:c0 + n], d_W,
               nssd[:, :], d_nssd, piece=128)
        load_w(stg, d_stg, w_out_d.rearrange("(c p) n -> p c n", p=128), 8, 1024, lambda c0, n: Wout[:, :, c0:c0 + n], d_W, piece=128)
        hts = [sb("hts%d" % i, [128, 8, 512], BF16) for i in range(2)]; d_hts = [Dep("hts%d" % i) for i in range(2)]
        mg = [sb("mg%d" % i, [128, 8, 512], BF16) for i in range(2)]; d_mg = [Dep("mg%d" % i) for i in range(2)]
        xr = [sb("xr%d" % i, [128, D], F32) for i in range(2)]; d_xr = [Dep("xr%d" % i) for i in range(2)]
        sgA = [sb("sgA%d" % i, [128, 512], F32) for i in range(2)]; d_sgA = [Dep("sgA%d" % i) for i in range(2)]
        sgS = [sb("sgS%d" % i, [128, 512], F32) for i in range(2)]; d_sgS = [Dep("sgS%d" % i) for i in range(2)]
        m1 = sb("m1", [128, 512], F32); d_m1 = Dep("m1")
        m2 = sb("m2", [128, 512], F32); d_m2 = Dep("m2")
        pgA = [ps("pgA%d" % i, [128, 512], F32) for i in range(2)]; d_pgA = [Dep("pgA%d" % i) for i in range(2)]
        pgS = [ps("pgS%d" % i, [128, 512], F32) for i in range(2)]; d_pgS = [Dep("pgS%d" % i) for i in range(2)]
        pA = ps("pA", [128, 512], F32); d_pA = Dep("pA")
        pS = ps("pS", [128, 512], F32); d_pS = Dep("pS")
        po = [ps("po%d" % i, [128, 512], F32) for i in range(2)]; d_po = [Dep("po%d" % i) for i in range(2)]
        u = 0
        for j in range(NS):
            hb = j % 2
            js = slice(j * 512, (j + 1) * 512)
            P.dma(hts[hb][:, :, :], hT_d[j], writes=[d_hts[hb]], semkey=("hts", hb))
            def gates(mo):
                b = mo % 2
                for kc in range(8):
                    mm(pgA[b][:, :], Wg[:, kc, mo * 128:(mo + 1) * 128], hts[hb][:, kc, :], kc == 0, kc == 7,
                       [d_W, d_hts[hb]], [d_pgA[b]])
                for kc in range(8):
                    mm(pgS[b][:, :], Wg[:, kc, 1024 + mo * 128:1024 + (mo + 1) * 128], hts[hb][:, kc, :], kc == 0, kc == 7,
                       [d_W, d_hts[hb]], [d_pgS[b]])
                act(sgA[b][:, :], pgA[b][:, :], AF.Sigmoid, [d_pgA[b], d_bg], [d_sgA[b]], bias=bg[:, mo:mo + 1])
                act(sgS[b][:, :], pgS[b][:, :], AF.Sigmoid, [d_pgS[b], d_bg], [d_sgS[b]], bias=bg[:, 8 + mo:9 + mo])

            gates(0)
            for mo in range(8):
                b = mo % 2
                if mo + 1 < 8:
                    gates(mo + 1)
                for c in range(4):
                    mm(pA[:, :], Wao[:, c, mo * 128:(mo + 1) * 128], attnT[:, c, js], c == 0, c == 3, [d_W, d_attn], [d_pA])
                for c in range(4):
                    mm(pS[:, :], Wso[:, c, mo * 128:(mo + 1) * 128], ssdT[:, c, js], c == 0, c == 3, [d_W, d_ssdT], [d_pS])
                tt("vector", m1[:, :], sgA[b][:, :], pA[:, :], ALU.mult, [d_sgA[b], d_pA], [d_m1])
                tt("vector", m2[:, :], sgS[b][:, :], pS[:, :], ALU.mult, [d_sgS[b], d_pS], [d_m2])
                tt("gpsimd", mg[hb][:, mo, :], m1[:, :], m2[:, :], ALU.add, [d_m1, d_m2], [d_mg[hb]])
            for i4 in range(4):
                t = j * 4 + i4
                xb = t % 2
                P.dma(xr[xb][:, :], x_d[t * 128:(t + 1) * 128, :], writes=[d_xr[xb]], semkey=("xr", xb))
                for nh in range(2):
                    for kc in range(8):
                        mm(po[nh][:, :], mg[hb][:, kc, i4 * 128:(i4 + 1) * 128], Wout[:, kc, nh * 512:(nh + 1) * 512],
                           kc == 0, kc == 7, [d_W, d_mg[hb]], [d_po[nh]])
                    tt("vector", xr[xb][:, nh * 512:(nh + 1) * 512], xr[xb][:, nh * 512:(nh + 1) * 512], po[nh][:, :], ALU.add,
                       [d_xr[xb], d_po[nh]], [d_xr[xb]])
                P.dma(x1_d[t * 128:(t + 1) * 128, :], xr[xb][:, :], reads=[d_xr[xb]], semkey=("x1s", xb))
                if "x1" in dbg:
                    if t == 0:
                        dbg_x1 = dbgout("x1", [S, D], F32)
                    P.dma(dbg_x1[t * 128:(t + 1) * 128, :], xr[xb][:, :], reads=[d_xr[xb]], semkey=("x1d", xb))
        P.barrier()
        P.emit()
    es35.close()
    es_ssd.close()
    if stop_after == 5:
        return finish()

    NSLOT = 11
    RW = 1032
    sorted_d = dscr("sorted_h2", [NSLOT * 512, RW], BF16)
    sout_d = dscr("sorted_out", [NSLOT * 512, D], F32)
    nfbc_d = din("nf_bc", [128, D])
    sconst_d = din("sconst", [128, 32])

    def dma_fn(eng, fn, reads, writes, semkey, grp=None):
        return P._rec(eng, fn, list(reads), list(writes), is_dma=True, semkey=semkey, grp=grp)

    es6 = ExitStack()
    sb6 = lambda name, shape, dt: es6.enter_context(nc.sbuf_tensor(uq(name), list(shape), dt))
    widx1 = sb6("widx1", [128, NSLOT, 8], mybir.dt.int32)
    d_widx = Dep("widx")
    wc_d = din("wconst", [128, 48])
    pos_i = sb6("pos_i", [128, NT], mybir.dt.int32); d_pos = Dep("pos_i")
    gs_i = sb6("gs_i", [128, 16], mybir.dt.int32); d_gs = Dep("gs_i")
    d_sorted = Dep("sorted_d")

    with ExitStack() as ph:
        sb = lambda name, shape, dt: ph.enter_context(nc.sbuf_tensor(uq(name), list(shape), dt))
        ps = lambda name, shape, dt: ph.enter_context(nc.psum_tensor(uq(name), list(shape), dt))
        nfbc = sb("s_nfbc", [128, D], F32); d_nfbc = Dep("nfbc")
        brt = sb("s_br", [128, 20], F32); d_br = Dep("br")
        sconst = sb("s_sconst", [128, 32], F32); d_sc0 = Dep("sconst")
        masks = sb("s_masks6", [128, 4, 128], F32); d_mk = Dep("masks6")
        Wr = sb("Wr", [128, 8, 20], BF16); d_Wr = Dep("Wr")
        P.dma(nfbc[:, :], nfbc_d[:, :], writes=[d_nfbc], semkey="c0")
        P.dma(brt[:, :], br_d[:, :], writes=[d_br], semkey="c1")
        P.dma(sconst[:, :], sconst_d[:, :], writes=[d_sc0], semkey="c2")
        P.dma(masks[:, :, :], masks_d[:, :, :], writes=[d_mk], semkey="c3")
        P.dma(Wr[:, :, :], wr_d.rearrange("(kc p) n -> p kc n", p=128), writes=[d_Wr], semkey="wr_cast", eng="gpsimd")
        ones_f = sb("ones_f6", [128, 128], F32); d_of = Dep("ones_f6")
        memset("gpsimd", ones_f[:, :], 1.0, [d_of])
        xn_all = sb("xn_all", [128, NT, RW], BF16); d_xn = [Dep("xn_all%d" % t) for t in range(NT)]
        oh_all = sb("oh_all", [128, NT, 4], F32); d_oh = Dep("oh_all")
        x1t = [sb("x1t%d" % i, [128, D], F32) for i in range(3)]; d_x1t = [Dep("x1t%d" % i) for i in range(3)]
        junk = sb("junk6", [128, D], BF16); d_junk = Dep("junk6")
        h2t = [sb("h2t%d" % i, [128, 8, 128], BF16) for i in range(2)]; d_h2t = [Dep("h2t%d" % i) for i in range(2)]
        rt = sb("rt", [128, 96], F32); d_rt = Dep("rt")
        rt2 = sb("rt2", [128, NT, 2], F32); d_rt2 = [Dep("rt2_%d" % i) for i in range(NT)]
        Lall = sb("Lall", [128, NT, 20], F32); d_L = Dep("Lall"); d_rz = Dep("rz")
        gmax = sb("gmax", [128, NT], F32); gsum = sb("gsum", [128, NT], F32); m1c = sb("m1c", [128, NT], F32)
        m2c = sb("m2c", [128, NT], F32); esum = sb("esum", [128, NT], F32)
        eg = sb("eg", [128, NT, 4], F32); fs = sb("fs", [128, NT, 4], F32); fs2 = sb("fs2", [128, NT, 4], F32)
        mk1 = sb("mk1", [128, NT, 4], F32); mk2 = sb("mk2", [128, NT, 4], F32); ef = sb("ef", [128, NT, 4], F32)
        tmp16 = sb("tmp16", [128, NT, 4, 4], F32)
        tpp = [ps("tpp%d" % i, [128, 8, 128], BF16) for i in range(2)]; d_tpp = [Dep("tpp%d" % i) for i in range(2)]
        plg = [ps("plg%d" % i, [128, 32], F32) for i in range(2)]; d_plg = [Dep("plg%d" % i) for i in range(2)]
        def p6_stage1(t):
            xb = t % 3
            P.dma(x1t[xb][:, :], x1_d[t * 128:(t + 1) * 128, :], writes=[d_x1t[xb]], semkey=("x1t", xb))
            R = [d_rt2[t]]
            act(junk[:, :], x1t[xb][:, :], AF.Square, [d_x1t[xb]], [d_junk] + R, accum_out=rt2[:, t, 0:1])
            act(rt2[:, t, 1:2], rt2[:, t, 0:1], AF.Sqrt, R + [d_eps], R, bias=epsb[:, 0:1], scale=1.0 / D)
            recip(rt2[:, t, 1:2], rt2[:, t, 1:2], R, R)
            stt(xn_all[:, t, 0:D], x1t[xb][:, :], rt2[:, t, 1:2], nfbc[:, :], ALU.mult, ALU.mult, [d_x1t[xb], d_nfbc] + R, [d_xn[t]])

        def p6_stage2(t):
            nb = t % 2
            for c in range(8):
                tr(tpp[nb][:, c, :], xn_all[:, t, c * 128:(c + 1) * 128], ident[:, :], [d_xn[t], d_ident], [d_tpp[nb]])
            cp("scalar", h2t[nb][:, :, :], tpp[nb][:, :, :], [d_tpp[nb]], [d_h2t[nb]])
            for kc in range(8):
                mm(plg[nb][:, 0:20], h2t[nb][:, kc, :], Wr[:, kc, :], kc == 0, kc == 7, [d_h2t[nb], d_Wr], [d_plg[nb]])
            tt("vector", Lall[:, t, :], plg[nb][:, 0:20], brt[:, :], ALU.add, [d_plg[nb], d_br], [d_L])

        p6_stage1(0)
        for t in range(NT):
            if t + 1 < NT:
                p6_stage1(t + 1)
            p6_stage2(t)
        B3 = lambda ap, n: ap.unsqueeze(2).to_broadcast([128, NT, n])
        Lg = Lall[:, :, 0:4]
        Fv = Lall[:, :, 4:20].rearrange("p t (g k) -> p t g k", g=4)
        Z = [d_rz]
        red(gmax[:, :], Lg, ALU.max, [d_L], Z)
        tt("vector", eg[:, :, :], Lg, B3(gmax[:, :], 4), ALU.subtract, [d_L] + Z, Z)
        ts("vector", oh_all[:, :, :], eg[:, :, :], 0.0, None, ALU.is_equal, None, Z, [d_oh])
        act(eg[:, :, :], eg[:, :, :], AF.Exp, Z, Z)
        red(gsum[:, :], eg[:, :, :], ALU.add, Z, Z)
        tt("vector", tmp16[:, :, :, :], Fv, oh_all[:, :, :].unsqueeze(3).to_broadcast([128, NT, 4, 4]), ALU.mult, [d_L, d_oh] + Z, Z)
        red(fs[:, :, :], tmp16[:, :, :, :].rearrange("p t g k -> p t k g"), ALU.add, Z, Z)
        red(m1c[:, :], fs[:, :, :], ALU.max, Z, Z)
        tt("vector", fs[:, :, :], fs[:, :, :], B3(m1c[:, :], 4), ALU.subtract, Z, Z)
        ts("vector", mk1[:, :, :], fs[:, :, :], 0.0, None, ALU.is_equal, None, Z, Z)
        stt(fs2[:, :, :], mk1[:, :, :], -1.0e30, fs[:, :, :], ALU.mult, ALU.add, Z, Z)
        red(m2c[:, :], fs2[:, :, :], ALU.max, Z, Z)
        tt("vector", mk2[:, :, :], fs2[:, :, :], B3(m2c[:, :], 4), ALU.is_equal, Z, Z)
        tt("vector", mk1[:, :, :], mk1[:, :, :], mk2[:, :, :], ALU.add, Z, Z)
        act(ef[:, :, :], fs[:, :, :], AF.Exp, Z, Z)
        tt("vector", ef[:, :, :], ef[:, :, :], mk1[:, :, :], ALU.mult, Z, Z)
        red(esum[:, :], ef[:, :, :], ALU.add, Z, Z)
        tt("vector", esum[:, :], esum[:, :], gsum[:, :], ALU.mult, Z, Z)
        recip(esum[:, :], esum[:, :], Z, Z)
        tt("vector", xn_all[:, :, D:RW].bitcast(F32), ef[:, :, :], B3(esum[:, :], 4), ALU.mult, Z, d_xn)
        pcw = ps("pcw", [128, 128], F32); d_pcw = Dep("pcw")
        ptot = ps("ptot", [128, 128], F32); d_ptot = Dep("ptot")
        ohf = oh_all[:, :, :].rearrange("p t g -> p (t g)")
        mm(pcw[:, :], masks[:, 0, :], ohf, True, True, [d_mk, d_oh], [d_pcw])
        mm(ptot[:, :], ones_f[:, :], ohf, True, True, [d_of, d_oh], [d_ptot])
        tot = sb("tot", [128, NT, 4], F32); pre = sb("pre", [128, NT, 4], F32); Aa = sb("Aa", [128, NT, 4], F32)
        sm6 = sb("sm6", [128, 64], F32)
        d_q = Dep("posq")
        Q = [d_q]
        cp("vector", tot[:, :, :], ptot[:, :].rearrange("p (t g) -> p t g", g=4), [d_ptot], Q)
        memset("vector", pre[:, 0, :], 0.0, Q)
        for t in range(1, NT):
            tt("vector", pre[:, t, :], pre[:, t - 1, :], tot[:, t - 1, :], ALU.add, Q, Q)
        ng = sm6[:, 0:4]; cnt = sm6[:, 4:8]; pn = sm6[:, 8:12]; st = sm6[:, 12:16]; en = sm6[:, 16:20]; stm1 = sm6[:, 20:24]
        cmp8 = sm6[:, 24:32]; posf = sb("posf", [128, NT], F32); gsf = sm6[:, 32:48]; cmp11 = sm6[:, 48:64]
        tt("vector", ng, pre[:, NT - 1, :], tot[:, NT - 1, :], ALU.add, Q, Q)
        for g in range(4):
            ts("vector", cmp8, sconst[:, 16:24], ng[:, g:g + 1], None, ALU.is_lt, None, Q + [d_sc0], Q)
            red(cnt[:, g:g + 1], cmp8, ALU.add, Q, Q)
        ts("vector", pn, cnt, 512.0, None, ALU.mult, None, Q, Q)
        memset("vector", st[:, 0:1], 0.0, Q)
        for g in range(1, 4):
            tt("vector", st[:, g:g + 1], st[:, g - 1:g], pn[:, g - 1:g], ALU.add, Q, Q)
        tt("vector", en, st, pn, ALU.add, Q, Q)
        ts("vector", stm1, st, -1.0, None, ALU.add, None, Q, Q)
        tt("vector", Aa[:, :, :], pcw[:, :].rearrange("p (t g) -> p t g", g=4), pre[:, :, :], ALU.add, Q + [d_pcw], Q)
        tt("vector", Aa[:, :, :], Aa[:, :, :], stm1.unsqueeze(1).to_broadcast([128, NT, 4]), ALU.add, Q, Q)
        tt("vector", Aa[:, :, :], Aa[:, :, :], oh_all[:, :, :], ALU.mult, Q + [d_oh], Q)
        red(posf[:, :], Aa[:, :, :], ALU.add, Q, Q)
        cp("vector", pos_i[:, :], posf[:, :], Q, [d_pos])
        memset("vector", gsf, 0.0, Q)
        for g in range(3):
            ts("vector", cmp11, sconst[:, 0:16], en[:, g:g + 1], None, ALU.is_ge, None, Q + [d_sc0], Q)
            tt("vector", gsf, gsf, cmp11, ALU.add, Q, Q)
        cp("vector", gs_i[:, :], gsf, Q, [d_gs])
        wcst = sb("s_wconst", [128, 48], F32); d_wc = Dep("wconst")
        P.dma(wcst[:, :], wc_d[:, :], writes=[d_wc], semkey="c5")
        wf1 = sb("wf1", [128, NSLOT, 8], F32)
        g1 = sm6[:, 48:64]
        ts("vector", g1, gsf, 1024.0, None, ALU.mult, None, Q, Q)
        for s in range(NSLOT):
            ts("vector", wf1[:, s, :], wcst[:, 0:8], g1[:, s:s + 1], None, ALU.add, None, Q + [d_wc], Q)
        same = sb("same6", [128, 16], F32)
        memset("vector", same[:, 0:1], 0.0, Q)
        tt("vector", same[:, 1:NSLOT], gsf[:, 1:NSLOT], gsf[:, 0:NSLOT - 1], ALU.is_equal, Q, Q)
        ts("vector", same[:, 0:NSLOT], same[:, 0:NSLOT], 8192.0, None, ALU.mult, None, Q, Q)
        tt("vector", wf1[:, :, :], wf1[:, :, :], same[:, 0:NSLOT].unsqueeze(2).to_broadcast([128, NSLOT, 8]), ALU.add, Q, Q)
        cp("vector", widx1[:, :, :], wf1[:, :, :], Q, [d_widx])
        for t in range(NT):
            dma_fn("gpsimd", lambda e, t=t: e.indirect_dma_start(
                out=sorted_d[:, :], out_offset=bass.IndirectOffsetOnAxis(ap=pos_i[:, t:t + 1], axis=0),
                in_=xn_all[:, t, :], in_offset=None, bounds_check=None, oob_is_err=False),
                [d_xn[t], d_pos], [d_sorted], ("scat", t % 4))
        if "sort" in dbg:
            o1 = dbgout("pos", [128, NT], mybir.dt.int32); o2 = dbgout("gs", [128, 16], mybir.dt.int32)
            o3 = dbgout("xn_all", [128, NT, RW], BF16)
            P.dma(o1, pos_i[:, :], reads=[d_pos], semkey="d0")
            P.dma(o2, gs_i[:, :], reads=[d_gs], semkey="d1")
            P.dma(o3, xn_all[:, :, :], reads=d_xn, semkey="d2")
        P.barrier()
        P.emit()
    if stop_after == 61:
        es6.close()
        return finish()

    with ExitStack() as ph:
        sb = lambda name, shape, dt: ph.enter_context(nc.sbuf_tensor(uq(name), list(shape), dt))
        ps = lambda name, shape, dt: ph.enter_context(nc.psum_tensor(uq(name), list(shape), dt))
        NWB = 4
        W1s = [sb("W1s%d" % i, [128, 8, DE], BF16) for i in range(NWB)]
        W3s = [sb("W3s%d" % i, [128, 8, DE], BF16) for i in range(NWB)]
        W2s = [sb("W2s%d" % i, [128, 4, D], BF16) for i in range(NWB)]
        d_W1 = [Dep("W1s%d" % i) for i in range(NWB)]; d_W3 = [Dep("W3s%d" % i) for i in range(NWB)]
        d_W2 = [Dep("W2s%d" % i) for i in range(NWB)]
        xs = [sb("xs%d" % i, [128, 4, RW], BF16) for i in range(2)]; d_xs6 = [Dep("xs6_%d" % i) for i in range(2)]
        h2s = [sb("h2s%d" % i, [128, 8, 512], BF16) for i in range(2)]; d_h2s = [Dep("h2s%d" % i) for i in range(2)]
        gT = [sb("gT%d" % i, [128, 4, 512], BF16) for i in range(2)]; d_gT = [Dep("gT%d" % i) for i in range(2)]
        s1 = [sb("s1_%d" % i, [128, 512], F32) for i in range(2)]; d_s1 = [Dep("s1_%d" % i) for i in range(2)]
        yacc = [sb("yacc%d" % i, [128, 4, D], F32) for i in range(2)]; d_ya6 = [Dep("yacc%d" % i) for i in range(2)]
        tpp = ps("tpp6", [128, 8, 128], BF16); d_tpp = Dep("tpp6")
        ph1 = [ps("ph1_%d" % i, [128, 512], F32) for i in range(2)]; d_ph1 = [Dep("ph1_%d" % i) for i in range(2)]
        ph3 = [ps("ph3_%d" % i, [128, 512], F32) for i in range(2)]; d_ph3 = [Dep("ph3_%d" % i) for i in range(2)]
        py = [ps("py%d" % i, [128, 512], F32) for i in range(2)]; d_py = [Dep("py%d" % i) for i in range(2)]
        d_sout = Dep("sout")

        grp_ctr = [0]
        bnd_cache = {}

        def wload(s, ee, wb):
            for (rows, dst, nchunk, ncol, dd, key) in ((w1_d, W1s[wb], 8, DE, d_W1[wb], "w1"),
                                                       (w3_d, W3s[wb], 8, DE, d_W3[wb], "w3"),
                                                       (w2_d, W2s[wb], 4, D, d_W2[wb], "w2")):
                grp_ctr[0] += 1
                gid = grp_ctr[0]
                hc = nchunk // 2
                for hf in range(2):
                    def fn(e, rows=rows, dst=dst, hf=hf, hc=hc, ncol=ncol):
                        if "bval" not in bnd_cache:
                            rg = e.alloc_register("wbound")
                            e.reg_mov(rg, NE * 128 * 2 - 1)
                            bnd_cache["bval"] = e.snap(rg)
                        return e.indirect_dma_start(
                            out=dst[:, hf * hc:(hf + 1) * hc, :].rearrange("p c n -> p (c n)"), out_offset=None,
                            in_=rows[:, :],
                            in_offset=bass.IndirectOffsetOnAxis(ap=widx1[:, s, ee * 2 + hf:ee * 2 + hf + 1], axis=0),
                            bounds_check=bnd_cache["bval"], oob_is_err=False)
                    dma_fn("gpsimd", fn, [d_widx], [dd], (key, wb), grp=gid)

        NSL = int(os.environ.get("MOE_NSLOT", str(NSLOT)))

        def slot_rows(s2):
            for i4 in range(4):
                r0 = (s2 * 4 + i4) * 128
                P.dma(xs[s2 % 2][:, i4, :], sorted_d[r0:r0 + 128, :], reads=[d_sorted], writes=[d_xs6[s2 % 2]], semkey=("xs6", i4))

        def slot_tr(s2, i4):
            for c in range(8):
                tr(tpp[:, c, :], xs[s2 % 2][:, i4, c * 128:(c + 1) * 128], ident[:, :], [d_xs6[s2 % 2], d_ident], [d_tpp])
            cp("scalar", h2s[s2 % 2][:, :, i4 * 128:(i4 + 1) * 128], tpp[:, :, :], [d_tpp], [d_h2s[s2 % 2]])

        work = [(s, ee) for s in range(NSL) for ee in range(4)]
        for ee0 in range(4):
            wload(0, ee0, ee0)
        uu = 0
        for n, (s, ee) in enumerate(work):
            if n >= 1:
                ps_, pe_ = work[n - 1]
                if ps_ + 1 < NSL:
                    wload(ps_ + 1, pe_, pe_)
            wb = ee
            sb_ = s % 2
            if n == 0:
                slot_rows(0)
                for i4 in range(4):
                    slot_tr(0, i4)
            if s + 1 < NSL:
                if ee == 0:
                    slot_rows(s + 1)
                else:
                    slot_tr(s + 1, ee - 1)
                    if ee == 3:
                        slot_tr(s + 1, 3)
            gb = n % 2
            for mc in range(4):
                b = uu % 2
                uu += 1
                for kc in range(8):
                    mm(ph1[b][:, :], W1s[wb][:, kc, mc * 128:(mc + 1) * 128], h2s[sb_][:, kc, :], kc == 0, kc == 7,
                       [d_W1[wb], d_h2s[sb_]], [d_ph1[b]])
                for kc in range(8):
                    mm(ph3[b][:, :], W3s[wb][:, kc, mc * 128:(mc + 1) * 128], h2s[sb_][:, kc, :], kc == 0, kc == 7,
                       [d_W3[wb], d_h2s[sb_]], [d_ph3[b]])
                act(s1[b][:, :], ph1[b][:, :], AF.Silu, [d_ph1[b]], [d_s1[b]])
                tt("vector", gT[gb][:, mc, :], s1[b][:, :], ph3[b][:, :], ALU.mult, [d_s1[b], d_ph3[b]], [d_gT[gb]])
            for i4 in range(4):
                wcol = xs[sb_][:, i4, D:RW].bitcast(F32)[:, ee:ee + 1]
                for nh in range(2):
                    for mc in range(4):
                        mm(py[nh][:, :], gT[gb][:, mc, i4 * 128:(i4 + 1) * 128], W2s[wb][:, mc, nh * 512:(nh + 1) * 512],
                           mc == 0, mc == 3, [d_gT[gb], d_W2[wb]], [d_py[nh]])
                    ysl = yacc[sb_][:, i4, nh * 512:(nh + 1) * 512]
                    if ee == 0:
                        ts("vector", ysl, py[nh][:, :], wcol, None, ALU.mult, None, [d_py[nh], d_xs6[sb_]], [d_ya6[sb_]])
                    else:
                        stt(ysl, py[nh][:, :], wcol, ysl, ALU.mult, ALU.add, [d_py[nh], d_xs6[sb_], d_ya6[sb_]], [d_ya6[sb_]])
            if ee == 3:
                for i4 in range(4):
                    r0 = (s * 4 + i4) * 128
                    P.dma(sout_d[r0:r0 + 128, :], yacc[sb_][:, i4, :], reads=[d_ya6[sb_]], writes=[d_sout], semkey=("ys6", i4))
        P.barrier()
        P.emit()

    with ExitStack() as ph:
        sb = lambda name, shape, dt: ph.enter_context(nc.sbuf_tensor(uq(name), list(shape), dt))
        NB6 = 6
        yt = [sb("yt%d" % i, [128, D], F32) for i in range(NB6)]; d_yt = [Dep("yt%d" % i) for i in range(NB6)]
        x1r = [sb("x1r%d" % i, [128, D], F32) for i in range(NB6)]; d_x1r = [Dep("x1r%d" % i) for i in range(NB6)]
        for t in range(NT):
            b = t % NB6
            dma_fn("gpsimd", lambda e, t=t, b=b: e.indirect_dma_start(
                out=yt[b][:, :], out_offset=None, in_=sout_d[:, :],
                in_offset=bass.IndirectOffsetOnAxis(ap=pos_i[:, t:t + 1], axis=0),
                bounds_check=None, oob_is_err=False), [d_pos], [d_yt[b]], ("gat", b))
            P.dma(x1r[b][:, :], x1_d[t * 128:(t + 1) * 128, :], writes=[d_x1r[b]], semkey=("x1r", b))
            tt("vector", x1r[b][:, :], x1r[b][:, :], yt[b][:, :], ALU.add, [d_x1r[b], d_yt[b]], [d_x1r[b]])
            P.dma(out_d[t * 128:(t + 1) * 128, :], x1r[b][:, :], reads=[d_x1r[b]], semkey=("outs", b), eng="scalar")
        P.barrier()
        P.emit()
    es6.close()

    return finish()


_CACHE = {}


def _consts():
    bf = ml_dtypes.bfloat16
    c = {}
    c["ident_bf"] = np.eye(128, dtype=np.float32).astype(bf)
    ob = np.zeros((128, 128), np.float32)
    ob[:64, :64] = 1.0
    ob[64:, 64:] = 1.0
    c["onesblk"] = ob.astype(bf)
    rm = np.zeros((128, 128), np.float32)
    for blk in range(4):
        base = blk * 32
        for i in range(16):
            rm[base + i + 16, base + i] = -1.0
            rm[base + i, base + i + 16] = 1.0
    c["rmat"] = rm.astype(bf)
    t = np.arange(S)
    row = (t // 64).astype(np.float32)
    col = (t % 64).astype(np.float32)
    inv = (np.float32(10000.0) ** (-(np.arange(0, 32, 2, dtype=np.float32)) / np.float32(32))).astype(np.float32)
    ar = row[:, None] * inv[None, :]
    ac = col[:, None] * inv[None, :]
    cos = np.concatenate([np.cos(ar), np.cos(ar), np.cos(ac), np.cos(ac)], -1).astype(np.float32)
    sin = np.concatenate([np.sin(ar), np.sin(ar), np.sin(ac), np.sin(ac)], -1).astype(np.float32)
    c["cosT"] = np.ascontiguousarray(np.concatenate([cos.T, cos.T], 0)).astype(bf)
    c["sinT"] = np.ascontiguousarray(np.concatenate([sin.T, sin.T], 0)).astype(bf)
    r = np.arange(128)[:, None]
    q = np.arange(128)[None, :]
    mk = np.stack([(r <= q) + 0 * q, (r > q) + 0 * q, (r >= q) + 0 * q, (r < q) + 0 * q], axis=1).astype(np.float32)
    c["masks"] = np.ascontiguousarray(mk)
    sc = np.zeros((128, 32), np.float32)
    sc[:, 0:16] = 512.0 * np.arange(16)[None, :]
    sc[:, 16:24] = 512.0 * np.arange(8)[None, :]
    c["sconst"] = sc
    wc = np.zeros((128, 48), np.float32)
    for ee in range(4):
        for hf in range(2):
            wc[:, ee * 2 + hf] = (ee * 128 + np.arange(128)) * 2 + hf
    c["wconst"] = wc
    return c


def _layout_inputs(inputs, b):
    f = lambda k: np.ascontiguousarray(np.asarray(inputs[k], dtype=np.float32)[0])
    bc = lambda v: np.ascontiguousarray(np.broadcast_to(v.reshape(1, -1), (128, v.size)))
    m = {}
    m["x"] = np.ascontiguousarray(np.asarray(inputs["x"], dtype=np.float32)[b])
    m["w_in"] = f("w_in")
    m["nw_mix"] = np.ascontiguousarray(f("norm_mix_w").reshape(8, 128).T)
    qw = f("q_norm_w")
    kw = f("k_norm_w")
    m["qkw"] = np.ascontiguousarray(np.stack([np.tile(qw, 2), np.tile(kw, 2)], axis=1))
    cw = f("conv_w")
    m["cw"] = np.ascontiguousarray(cw.T.reshape(6, 128, 7).transpose(1, 0, 2).reshape(128, 42))
    m["cbias"] = np.ascontiguousarray(f("conv_b").reshape(6, 128).T)
    m["dtb"] = bc(f("dt_bias").reshape(-1))
    m["alog"] = bc(f("a_log").reshape(-1))
    m["dsk"] = bc(f("d_skip").reshape(-1))
    m["nssd"] = np.ascontiguousarray(f("ssd_norm_w").reshape(4, 128).T)
    m["w_attn_o"] = f("w_attn_o")
    m["w_ssd_o"] = f("w_ssd_o")
    m["w_out"] = f("w_out")
    m["bgate"] = np.ascontiguousarray(f("b_gate").reshape(16, 128).T)
    m["nffn"] = np.ascontiguousarray(f("norm_ffn_w").reshape(8, 128).T)
    m["wr"] = np.ascontiguousarray(np.concatenate([f("w_router_group"), f("w_router_expert")], axis=1))
    m["br"] = bc(np.concatenate([f("b_router_group"), f("b_router_expert")]))
    m["nf_bc"] = bc(f("norm_ffn_w"))
    m["w1"] = np.ascontiguousarray(f("w1").reshape(NE, 8, 128, DE).transpose(0, 2, 1, 3)).reshape(NE * 128 * 2, 2048)
    m["w3"] = np.ascontiguousarray(f("w3").reshape(NE, 8, 128, DE).transpose(0, 2, 1, 3)).reshape(NE * 128 * 2, 2048)
    m["w2"] = np.ascontiguousarray(f("w2").reshape(NE, 4, 128, D).transpose(0, 2, 1, 3)).reshape(NE * 128 * 2, 2048)
    return m


def kernel(**inputs):
    B = np.asarray(inputs["x"]).shape[0]
    if "nc" not in _CACHE:
        _CACHE["nc"] = build_program()
    nc = _CACHE["nc"]
    cst = _consts()
    in_maps = []
    for b in range(B):
        m = _layout_inputs(inputs, b)
        m.update(cst)
        in_maps.append(m)
    res = run_bass_kernel_spmd(nc, in_maps, core_ids=list(range(B)))
    return np.stack([np.asarray(r["out"]) for r in res.results], axis=0)
```
